# Optimizing a Trainium2 kernel written in Bass

```python
import math
import jax, jax.numpy as jnp
from jax import lax
import numpy as np

D_MODEL = 1024
BATCH = 16
SEQ = 2048
DEPTH = 1

MOBA_HEADS = 8
MOBA_HEAD_DIM = 64
MOBA_BLOCK = 256
MOBA_TOPK = 3
MOBA_Q_CHUNK = 16
MOBA_WIDTH = MOBA_HEADS * MOBA_HEAD_DIM
DIFF_HEADS = 4
DIFF_HEAD_DIM = 64
DIFF_Q_BLOCK = 128
DIFF_WIDTH = DIFF_HEADS * 2 * DIFF_HEAD_DIM
N_BRANCHES = 2
IN_COLS = 3 * MOBA_WIDTH + 3 * DIFF_WIDTH + N_BRANCHES * D_MODEL
D_FF = -(-8 * D_MODEL // (3 * 256)) * 256
NORM_EPS = 1e-6
SUBLN_EPS = 1e-5
ALIBI_MAX_BIAS = 8.0
NEG = -1e30

kernel_name = 'hybrid_moba_diffattn_gated_sandwich_block'


def rms_norm(x, g, eps=NORM_EPS):
    x32 = x.astype(jnp.float32)
    y = x32 * lax.rsqrt(jnp.mean(x32 * x32, axis=-1, keepdims=True) + eps)
    return (y * g.astype(jnp.float32)).astype(x.dtype)


def alibi_slopes():
    n = MOBA_HEADS + DIFF_HEADS
    slopes = 2.0 ** (-ALIBI_MAX_BIAS * np.arange(1, n + 1) / n)
    stride = n // DIFF_HEADS
    diff_idx = np.arange(DIFF_HEADS) * stride + (stride - 1)
    moba_idx = np.setdiff1d(np.arange(n), diff_idx)
    return (jnp.asarray(slopes[moba_idx], dtype=jnp.float32),
            jnp.asarray(slopes[diff_idx], dtype=jnp.float32))


def moba_attention(q, k, v, slopes):
    B, H, S, Dh = q.shape
    BS = MOBA_BLOCK
    nb = -(-S // BS)
    s_pad = nb * BS
    scale = Dh ** -0.5
    pad = ((0, 0), (0, 0), (0, s_pad - S), (0, 0))
    k_pad = jnp.pad(k, pad)
    v_pad = jnp.pad(v, pad)
    k_blocks = k_pad.reshape(B, H, nb, BS, Dh)
    v_blocks = v_pad.reshape(B, H, nb, BS, Dh)
    k_mean = jnp.mean(k_blocks.astype(jnp.float32), axis=3).astype(q.dtype)
    pos = jnp.arange(S)
    q_blk = pos // BS
    gate = jnp.einsum('bhtd,bhnd->bhtn', q, k_mean).astype(jnp.float32)
    is_past = jnp.arange(nb)[None, :] < q_blk[:, None]
    gate = jnp.where(is_past, gate, NEG)
    n_sel = max(1, min(MOBA_TOPK, nb - 1))
    _, sel = lax.top_k(gate, n_sel)
    sel_valid = jnp.arange(n_sel)[None, :] < q_blk[:, None]
    b_ix = jnp.arange(B)[:, None, None, None]
    h_ix = jnp.arange(H)[None, :, None, None]
    C = MOBA_Q_CHUNK
    sl = slopes[None, :, None, None, None]

    def chunk(c):
        t0 = c * C
        qc = lax.dynamic_slice_in_dim(q, t0, C, axis=2)
        sc = lax.dynamic_slice_in_dim(sel, t0, C, axis=2)
        vc = lax.dynamic_slice_in_dim(sel_valid, t0, C, axis=0)
        tq = t0 + jnp.arange(C)
        kg = k_blocks[b_ix, h_ix, sc]
        s_pos = sc[..., None] * BS + jnp.arange(BS)
        lp = jnp.einsum('bhcd,bhcnkd->bhcnk', qc, kg).astype(jnp.float32) * scale
        lp = lp - sl * (tq[:, None, None] - s_pos).astype(jnp.float32)
        lp = jnp.where(vc[:, :, None], lp, NEG).reshape(B, H, C, n_sel * BS)
        own0 = (t0 // BS) * BS
        ko = lax.dynamic_slice_in_dim(k_pad, own0, BS, axis=2)
        vo = lax.dynamic_slice_in_dim(v_pad, own0, BS, axis=2)
        so = own0 + jnp.arange(BS)
        dist_o = (tq[:, None] - so[None, :]).astype(jnp.float32)
        lo = jnp.einsum('bhcd,bhkd->bhck', qc, ko).astype(jnp.float32) * scale
        lo = lo - slopes[None, :, None, None] * dist_o
        lo = jnp.where(so[None, :] <= tq[:, None], lo, NEG)
        p = jax.nn.softmax(jnp.concatenate([lp, lo], axis=-1), axis=-1).astype(v.dtype)
        pp = p[..., :n_sel * BS].reshape(B, H, C, n_sel, BS)
        po = p[..., n_sel * BS:]
        vg = v_blocks[b_ix, h_ix, sc]
        return (jnp.einsum('bhcnk,bhcnkd->bhcd', pp, vg)
                + jnp.einsum('bhck,bhkd->bhcd', po, vo))

    outs = lax.map(chunk, jnp.arange(S // C))
    return outs.transpose(1, 2, 0, 3, 4).reshape(B, H, S, Dh)


def diff_attention(q1, q2, k1, k2, v, lam, slopes):
    B, H, S, d = q1.shape
    scale = d ** -0.5
    QB = DIFF_Q_BLOCK
    kpos = jnp.arange(S)

    def block(c):
        t0 = c * QB
        tq = t0 + jnp.arange(QB)
        bias = -slopes[:, None, None] * (tq[:, None] - kpos[None, :]).astype(jnp.float32)
        causal = kpos[None, :] <= tq[:, None]

        def probs(q, k):
            qc = lax.dynamic_slice_in_dim(q, t0, QB, axis=2)
            logits = jnp.einsum('bhqd,bhkd->bhqk', qc, k).astype(jnp.float32) * scale + bias[None]
            return jax.nn.softmax(jnp.where(causal, logits, NEG), axis=-1)

        a = probs(q1, k1) - lam * probs(q2, k2)
        return jnp.einsum('bhqk,bhke->bhqe', a.astype(v.dtype), v)

    outs = lax.map(block, jnp.arange(S // QB))
    return outs.transpose(1, 2, 0, 3, 4).reshape(B, H, S, 2 * d)


def setup_inputs(seed: int = 0) -> dict:
    key = jax.random.key(seed)
    ks = jax.random.split(key, 17)
    L, D = DEPTH, D_MODEL

    def normal(k, shape, scale):
        return jax.random.normal(k, shape, dtype=jnp.float32) * scale

    def gain(k, n):
        return 1.0 + normal(k, (L, n), 0.05)

    return {
        'x': normal(ks[0], (BATCH, SEQ, D), 1.0),
        'norm_mix_pre_g': gain(ks[1], D),
        'w_in': normal(ks[2], (L, D, IN_COLS), D ** -0.5),
        'w_branch_a': normal(ks[3], (L, MOBA_WIDTH, D), MOBA_WIDTH ** -0.5),
        'w_branch_b': normal(ks[4], (L, DIFF_WIDTH, D), DIFF_WIDTH ** -0.5),
        'lam_q1': normal(ks[5], (L, DIFF_HEAD_DIM), 0.1),
        'lam_k1': normal(ks[6], (L, DIFF_HEAD_DIM), 0.1),
        'lam_q2': normal(ks[7], (L, DIFF_HEAD_DIM), 0.1),
        'lam_k2': normal(ks[8], (L, DIFF_HEAD_DIM), 0.1),
        'diff_subln_g': gain(ks[9], 2 * DIFF_HEAD_DIM),
        'w_out': normal(ks[10], (L, D, D), D ** -0.5),
        'norm_mix_post_g': gain(ks[11], D),
        'norm_ffn_pre_g': gain(ks[12], D),
        'w_gate': normal(ks[13], (L, D, D_FF), D ** -0.5),
        'w_up': normal(ks[14], (L, D, D_FF), D ** -0.5),
        'w_down': normal(ks[15], (L, D_FF, D), D_FF ** -0.5),
        'norm_ffn_post_g': gain(ks[16], D),
    }


def reference(x, norm_mix_pre_g, w_in, w_branch_a, w_branch_b, lam_q1, lam_k1, lam_q2, lam_k2,
              diff_subln_g, w_out, norm_mix_post_g, norm_ffn_pre_g, w_gate, w_up, w_down,
              norm_ffn_post_g):
    B, S, D = x.shape
    moba_slopes, diff_slopes = alibi_slopes()
    sizes = [MOBA_WIDTH] * 3 + [DIFF_WIDTH] * 3 + [D_MODEL] * N_BRANCHES
    split_at = [int(v) for v in np.cumsum(sizes)[:-1]]
    for l in range(DEPTH):
        lam_init = 0.8 - 0.6 * math.exp(-0.3 * l)
        h = rms_norm(x, norm_mix_pre_g[l])
        proj = h @ w_in[l]
        mq, mk, mv, dq, dk, dv, ga, gb = jnp.split(proj, split_at, axis=-1)

        def moba_heads(t):
            return t.reshape(B, S, MOBA_HEADS, MOBA_HEAD_DIM).transpose(0, 2, 1, 3)

        ya = moba_attention(moba_heads(mq), moba_heads(mk), moba_heads(mv), moba_slopes)
        ya = ya.transpose(0, 2, 1, 3).reshape(B, S, MOBA_WIDTH)

        dq = dq.reshape(B, S, DIFF_HEADS, 2, DIFF_HEAD_DIM).transpose(0, 2, 3, 1, 4)
        dk = dk.reshape(B, S, DIFF_HEADS, 2, DIFF_HEAD_DIM).transpose(0, 2, 3, 1, 4)
        dv = dv.reshape(B, S, DIFF_HEADS, 2 * DIFF_HEAD_DIM).transpose(0, 2, 1, 3)
        lam = (jnp.exp(jnp.sum(lam_q1[l].astype(jnp.float32) * lam_k1[l].astype(jnp.float32)))
               - jnp.exp(jnp.sum(lam_q2[l].astype(jnp.float32) * lam_k2[l].astype(jnp.float32)))
               + lam_init)
        yb = diff_attention(dq[:, :, 0], dq[:, :, 1], dk[:, :, 0], dk[:, :, 1], dv, lam, diff_slopes)
        yb = rms_norm(yb, diff_subln_g[l], SUBLN_EPS) * (1.0 - lam_init)
        yb = yb.transpose(0, 2, 1, 3).reshape(B, S, DIFF_WIDTH)

        merged = (jax.nn.sigmoid(ga) * (ya @ w_branch_a[l])
                  + jax.nn.sigmoid(gb) * (yb @ w_branch_b[l]))
        x = x + rms_norm(merged @ w_out[l], norm_mix_post_g[l])
        h = rms_norm(x, norm_ffn_pre_g[l])
        f = (jax.nn.silu(h @ w_gate[l]) * (h @ w_up[l])) @ w_down[l]
        x = x + rms_norm(f, norm_ffn_post_g[l])
    return x
```

```python
import numpy as np
import ml_dtypes
import concourse.bass as bass
import concourse.mybir as mybir
from concourse.bass_utils import run_bass_kernel_spmd

F32 = mybir.dt.float32
BF16 = mybir.dt.bfloat16
AF = mybir.ActivationFunctionType
ALU = mybir.AluOpType
AX = mybir.AxisListType

T = 2048
D = 1024
DFF = 2816
NF = DFF // 128
NEGM = -30000.0


class Buf:
    __slots__ = ("name", "w", "r")

    def __init__(self, name):
        self.name = name
        self.w = None
        self.r = {}


class EngState:
    def __init__(self, name, eng, sem):
        self.name = name
        self.eng = eng
        self.sem = sem
        self.cnt = 0
        self.seen = {}


class Sync:
    def __init__(self, nc, ndma=20):
        self.nc = nc
        self.E = {}
        for name, e in (("pe", nc.tensor), ("act", nc.scalar), ("dve", nc.vector),
                        ("pool", nc.gpsimd), ("sp", nc.sync)):
            self.E[name] = EngState(name, e, nc.alloc_semaphore("s_" + name))
        self.dsem = {}
        self.dnext = {}
        for q in ("sp", "pool"):
            self.dsem[q] = [[nc.alloc_semaphore("d_%s%d" % (q, i)), 0] for i in range(ndma)]
            self.dnext[q] = 0
        self.semof = {}
        for name, st in self.E.items():
            self.semof[("c", name)] = st.sem
        for q in ("sp", "pool"):
            for i, (s, _) in enumerate(self.dsem[q]):
                self.semof[("d", q, i)] = s

    def _wait(self, E, deps):
        for key, n in deps.items():
            if key == ("c", "pe") and E.name == "pe":
                continue
            if E.seen.get(key, 0) >= n:
                continue
            E.eng.wait_ge(self.semof[key], n)
            E.seen[key] = n

    @staticmethod
    def _deps(reads, writes):
        deps = {}

        def add(key, n):
            if deps.get(key, 0) < n:
                deps[key] = n
        for b in reads:
            if b.w is not None:
                add(*b.w)
        for b in writes:
            if b.w is not None:
                add(*b.w)
            for key, n in b.r.items():
                add(key, n)
        return deps

    @staticmethod
    def _mark(ev, reads, writes):
        key, n = ev
        for b in reads:
            if b.r.get(key, 0) < n:
                b.r[key] = n
        for b in writes:
            b.w = ev
            b.r = {}

    def op(self, engname, fn, reads=(), writes=()):
        E = self.E[engname]
        self._wait(E, self._deps(reads, writes))
        ins = fn(E.eng)
        E.cnt += 1
        ins.then_inc(E.sem, 1)
        ev = (("c", engname), E.cnt)
        self._mark(ev, reads, writes)
        return ev

    def group(self, engname, fns, reads=(), writes=()):
        E = self.E[engname]
        self._wait(E, self._deps(reads, writes))
        ins = None
        for fn in fns:
            ins = fn(E.eng)
        E.cnt += 1
        ins.then_inc(E.sem, 1)
        ev = (("c", engname), E.cnt)
        self._mark(ev, reads, writes)
        return ev

    def split_group(self, engname, fns, reads, writes, per):
        E = self.E[engname]
        self._wait(E, self._deps(reads, writes))
        n = len(fns)
        for i, fn in enumerate(fns):
            ins = fn(E.eng)
            if i == n - 1:
                E.cnt += 1
                ins.then_inc(E.sem, 1)
                self._mark((("c", engname), E.cnt), reads, writes)
            elif (i + 1) % per == 0:
                yield

    def dma(self, q, out, in_, reads=(), writes=()):
        E = self.E[q]
        i = self.dnext[q]
        self.dnext[q] = (i + 1) % len(self.dsem[q])
        slot = self.dsem[q][i]
        key = ("d", q, i)
        deps = self._deps(reads, writes)
        if slot[1] > 0 and deps.get(key, 0) < slot[1]:
            deps[key] = slot[1]
        self._wait(E, deps)
        E.eng.dma_start(out=out, in_=in_).then_inc(slot[0], 16)
        slot[1] += 16
        ev = (key, slot[1])
        self._mark(ev, reads, writes)
        return ev

    def barrier(self, engines=("pe", "act", "dve", "pool", "sp")):
        deps = {}
        for name, st in self.E.items():
            if st.cnt > 0:
                deps[("c", name)] = st.cnt
        for q in ("sp", "pool"):
            for i, (s, n) in enumerate(self.dsem[q]):
                if n > 0:
                    deps[("d", q, i)] = n
        for name in engines:
            self._wait(self.E[name], deps)


class Arena:
    def __init__(self, nc, base, top):
        self.nc = nc
        self.ptr = (base + 31) // 32 * 32
        self.top = top
        self.n = 0

    def alloc(self, name, shape, dtype):
        nbytes = 2 if dtype == BF16 else 4
        sz = 1
        for s in shape[1:]:
            sz *= s
        sz *= nbytes
        off = self.ptr
        self.ptr = (off + sz + 31) // 32 * 32
        assert self.ptr <= self.top, ("SBUF overflow", name, self.ptr, self.top)
        self.n += 1
        return self.nc.alloc_sbuf_tensor_at("%s_%d" % (name, self.n), list(shape), dtype, offset=off)

    def mark(self):
        return self.ptr

    def reset(self, m):
        self.ptr = m


def _alibi_slopes():
    n = 12
    slopes = 2.0 ** (-8.0 * np.arange(1, n + 1) / n)
    diff_idx = np.arange(4) * 3 + 2
    moba_idx = np.setdiff1d(np.arange(n), diff_idx)
    return slopes[moba_idx].astype(np.float32), slopes[diff_idx].astype(np.float32)


def _host_consts():
    bf = ml_dtypes.bfloat16
    ms, ds = _alibi_slopes()
    slopes = np.concatenate([ms, ds]).astype(np.float32)
    pos = np.arange(T)
    hi = (256 * (pos // 256)).astype(np.float32)
    lo = (pos % 256).astype(np.float32)
    qx = np.zeros((12, 16, T), np.float32)
    kx = np.zeros((12, 16, T), np.float32)
    for h in range(12):
        c1 = np.float32(slopes[h]).astype(bf).astype(np.float32)
        c2 = np.float32(slopes[h] - c1).astype(bf).astype(np.float32)
        qx[h, 8], qx[h, 9], qx[h, 10], qx[h, 11] = hi, lo, hi, lo
        qx[h, 12], qx[h, 13], qx[h, 14], qx[h, 15] = c1, c1, c2, c2
        kx[h, 8], kx[h, 9], kx[h, 10], kx[h, 11] = -c1, -c1, -c2, -c2
        kx[h, 12], kx[h, 13], kx[h, 14], kx[h, 15] = hi, lo, hi, lo
        if h < 8:
            for n in range(8):
                kx[h, n] = (pos // 256 == n).astype(np.float32)
    ident = np.eye(128, dtype=np.float32)
    kk = np.arange(128)[:, None]
    qq = np.arange(128)[None, :]
    negtri = np.where(kk <= qq, 0.0, NEGM).astype(np.float32)
    pastmask = np.zeros((64, 8), np.float32)
    pairsum = np.zeros((64, 72), np.float32)
    for n in range(8):
        for m in range(8):
            p = n * 8 + m
            pairsum[p, 64 + n] = 1.0
            for qb in range(8):
                pastmask[p, qb] = 1.0 if m < qb else 0.0
    notown = np.zeros((128, 8), np.float32)
    for n in range(8):
        for qb in range(8):
            notown[64 + n, qb] = NEGM if n < qb else 0.0
    return {
        "c_qx": qx.astype(bf), "c_kx": kx.astype(bf), "c_ident": ident.astype(bf),
        "c_negtri": negtri.astype(bf), "c_pastmask": pastmask, "c_pairsum": pairsum.astype(bf),
        "c_notown": notown,
    }


def build(nseq=2, debug=False):
    nc = bass.Bass("TRN2", target_bir_lowering=False)
    S = Sync(nc)

    def din(name, shape, dt=F32):
        return nc.dram_tensor(name, list(shape), dt, kind="ExternalInput").ap()

    x = din("x", [nseq, T, D])
    w_in = din("w_in", [D, 5120])
    w_a = din("w_branch_a", [512, D])
    w_b = din("w_branch_b", [512, D])
    w_out = din("w_out", [D, D])
    w_gate = din("w_gate", [D, DFF])
    w_up = din("w_up", [D, DFF])
    w_down = din("w_down", [DFF, D])
    g_pre = din("norm_mix_pre_g", [1, D])
    g_post = din("norm_mix_post_g", [1, D])
    g_fpre = din("norm_ffn_pre_g", [1, D])
    g_fpost = din("norm_ffn_post_g", [1, D])
    lamv = din("lamv", [1, 256])
    g_sub = din("diff_subln_g", [128, 1])
    c_qx = din("c_qx", [12, 16, T], BF16)
    c_kx = din("c_kx", [12, 16, T], BF16)
    c_ident = din("c_ident", [128, 128], BF16)
    c_negtri = din("c_negtri", [128, 128], BF16)
    c_pastmask = din("c_pastmask", [64, 8])
    c_pairsum = din("c_pairsum", [64, 72], BF16)
    c_notown = din("c_notown", [128, 8])
    y = nc.dram_tensor("y", [nseq, T, D], F32, kind="ExternalOutput").ap()
    x1s = nc.dram_tensor("x1s", [nseq, T, D], F32).ap()
    dbg = {}
    if debug:
        dbg["hT"] = nc.dram_tensor("dbg_hT", [128, 8, T], BF16, kind="ExternalOutput").ap()
        dbg["yT"] = nc.dram_tensor("dbg_yT", [128, 8, T], BF16, kind="ExternalOutput").ap()
        dbg["mT"] = nc.dram_tensor("dbg_mT", [128, 8, T], BF16, kind="ExternalOutput").ap()

    w_in_r = w_in.rearrange("(kc p) n -> p kc n", p=128)
    w_in_r4 = w_in.rearrange("(kc p) (a n) -> p kc a n", p=128, n=128)
    w_a_r = w_a.rearrange("(kc p) n -> p kc n", p=128)
    w_b_r = w_b.rearrange("(kc p) n -> p kc n", p=128)
    w_out_r = w_out.rearrange("(kc p) n -> p kc n", p=128)
    w_gate_r = w_gate.rearrange("(kc p) n -> p kc n", p=128)
    w_up_r = w_up.rearrange("(kc p) n -> p kc n", p=128)
    w_down_r = w_down.rearrange("(f p) n -> p f n", p=128)

    ar = Arena(nc, nc.sbuf_base, nc.sbuf_top)
    ps = nc.alloc_psum_tensor("ps", [128, 8, 512], F32)
    PB = [Buf("bank%d" % i) for i in range(8)]

    ident = ar.alloc("ident", [128, 128], BF16)
    negtri = ar.alloc("negtri", [128, 128], BF16)
    ones_bf = ar.alloc("ones", [128, 128], BF16)
    pastmask = ar.alloc("pastmask", [64, 8], F32)
    pairsum = ar.alloc("pairsum", [64, 72], BF16)
    notown = ar.alloc("notown", [128, 8], F32)
    gsub = ar.alloc("gsub", [128, 1], F32)
    neglam = ar.alloc("neglam", [128, 1], F32)
    lamt = ar.alloc("lamt", [128, 256], F32)
    lamp = ar.alloc("lamp", [128, 128], F32)
    lams = ar.alloc("lams", [128, 4], F32)
    XIN = [ar.alloc("xin", [128, D], F32) for _ in range(2)]
    XINb = [Buf("xin%d" % i) for i in range(2)]
    HN = [ar.alloc("hn", [128, D], BF16) for _ in range(2)]
    HNb = [Buf("hn%d" % i) for i in range(2)]
    junk = ar.alloc("junk", [128, D], BF16)
    junkb = Buf("junk")
    SS = [ar.alloc("ss", [128, 4], F32) for _ in range(2)]
    SSb = [Buf("ss%d" % i) for i in range(2)]
    TST = [ar.alloc("tst", [128, D], F32) for _ in range(2)]
    TSTb = [Buf("tst%d" % i) for i in range(2)]
    cb = Buf("consts")

    S.dma("sp", ident[:], c_ident, writes=[cb])
    S.dma("sp", negtri[:], c_negtri, writes=[cb])
    S.dma("sp", pastmask[:], c_pastmask, writes=[cb])
    S.dma("sp", pairsum[:], c_pairsum, writes=[cb])
    S.dma("sp", notown[:], c_notown, writes=[cb])
    S.dma("sp", gsub[:], g_sub, writes=[cb])
    S.dma("sp", lamt[:], lamv.partition_broadcast(128), writes=[cb])
    S.op("dve", lambda e: e.memset(ones_bf[:], 1.0), writes=[cb])
    lamtv = lamt[:].rearrange("p (a b d) -> p a b d", a=2, b=2)
    S.op("dve", lambda e: e.tensor_tensor(out=lamp[:].rearrange("p (a d) -> p a d", a=2),
                                          in0=lamtv[:, :, 0, :], in1=lamtv[:, :, 1, :], op=ALU.mult),
         reads=[cb], writes=[cb])
    S.op("dve", lambda e: e.tensor_reduce(out=lams[:, 0:2], in_=lamp[:].rearrange("p (a d) -> p a d", a=2),
                                          axis=AX.X, op=ALU.add), reads=[cb], writes=[cb])
    S.op("act", lambda e: e.activation(out=lams[:, 2:4], in_=lams[:, 0:2], func=AF.Exp), reads=[cb], writes=[cb])
    S.op("dve", lambda e: e.scalar_tensor_tensor(out=neglam[:], in0=lams[:, 3:4], scalar=-0.2, in1=lams[:, 2:3],
                                                 op0=ALU.add, op1=ALU.subtract), reads=[cb], writes=[cb])
    S.op("dve", lambda e: e.tensor_scalar(out=gsub[:], in0=gsub[:], scalar1=0.8, scalar2=None, op0=ALU.mult),
         reads=[cb], writes=[cb])

    base_mark = ar.mark()

    def rms_rstd(ssap, rstd_ap, n_inv, eps, sb):
        S.op("dve", lambda e: e.tensor_scalar(out=rstd_ap, in0=ssap, scalar1=n_inv, scalar2=eps,
                                              op0=ALU.mult, op1=ALU.add), reads=[sb], writes=[sb])
        S.op("act", lambda e: e.activation(out=rstd_ap, in_=rstd_ap, func=AF.Ln), reads=[sb], writes=[sb])
        S.op("act", lambda e: e.activation(out=rstd_ap, in_=rstd_ap, func=AF.Exp, scale=-0.5),
             reads=[sb], writes=[sb])

    def norm_transpose(src_ap, srcb, gt, gtb, dstT, dstb, tcol, k, bank):
        ss, ssb = SS[k % 2], SSb[k % 2]
        hn, hnb = HN[k % 2], HNb[k % 2]
        S.op("dve", lambda e: e.memset(ss[:], 0.0), writes=[ssb])
        S.op("act", lambda e: e.activation(out=junk[:], in_=src_ap, func=AF.Square, accum_out=ss[:, 0:1]),
             reads=[srcb], writes=[junkb, ssb])
        rms_rstd(ss[:, 0:1], ss[:, 1:2], 1.0 / D, 1e-6, ssb)
        S.op("dve", lambda e: e.scalar_tensor_tensor(out=hn[:], in0=src_ap, scalar=ss[:, 1:2], in1=gt[:],
                                                     op0=ALU.mult, op1=ALU.mult),
             reads=[srcb, ssb, gtb], writes=[hnb])
        psb = ps[:, bank, :].bitcast(BF16)
        S.group("pe", [(lambda e, kc=kc: e.transpose(out=psb[:, kc * 128:(kc + 1) * 128],
                                                      in_=hn[:, kc * 128:(kc + 1) * 128], identity=ident[:]))
                       for kc in range(8)], reads=[hnb, cb], writes=[PB[bank]])
        S.op("act", lambda e: e.activation(out=dstT[:, :, tcol:tcol + 128],
                                           in_=psb.rearrange("p (k t) -> p k t", k=8), func=AF.Copy),
             reads=[PB[bank]], writes=[dstb])

    def post_norm_resid(banks, gt, gtb, resid_ap, residb, k, out_dram):
        ss, ssb = SS[k % 2], SSb[k % 2]
        tst, tstb = TST[k % 2], TSTb[k % 2]
        S.op("dve", lambda e: e.memset(ss[:], 0.0), writes=[ssb])
        for hf in range(2):
            S.op("act", lambda e, hf=hf: e.activation(out=junk[:, 0:512], in_=ps[:, banks[hf], :], func=AF.Square,
                                                      accum_out=ss[:, hf:hf + 1]),
                 reads=[PB[banks[hf]]], writes=[junkb, ssb])
        S.op("dve", lambda e: e.tensor_tensor(out=ss[:, 2:3], in0=ss[:, 0:1], in1=ss[:, 1:2], op=ALU.add),
             reads=[ssb], writes=[ssb])
        rms_rstd(ss[:, 2:3], ss[:, 3:4], 1.0 / D, 1e-6, ssb)
        for hf in range(2):
            S.op("dve", lambda e, hf=hf: e.scalar_tensor_tensor(
                out=tst[:, hf * 512:(hf + 1) * 512], in0=ps[:, banks[hf], :], scalar=ss[:, 3:4],
                in1=gt[:, hf * 512:(hf + 1) * 512], op0=ALU.mult, op1=ALU.mult),
                reads=[PB[banks[hf]], ssb, gtb], writes=[tstb])
        S.op("dve", lambda e: e.tensor_tensor(out=tst[:], in0=tst[:], in1=resid_ap, op=ALU.add),
             reads=[tstb, residb], writes=[tstb])
        S.dma("sp", out_dram, tst[:], reads=[tstb])

    for seq in range(nseq):
        ar.reset(base_mark)
        hT = ar.alloc("hT", [128, 8, T], BF16)
        hTb = Buf("hT")
        yT = ar.alloc("yT", [128, 8, T], BF16)
        yTb = Buf("yT")
        a_mark = ar.mark()
        gpre = ar.alloc("gpre", [128, D], F32)
        gpreb = Buf("gpre")
        QK = [[ar.alloc("qk", [128, T], BF16) for _ in range(4)] for _ in range(2)]
        QKb = [[Buf("qk") for _ in range(4)] for _ in range(2)]
        VV = [ar.alloc("vv", [128, 16, 320], BF16) for _ in range(2)]
        VVb = [Buf("vv") for _ in range(2)]
        WSL = [ar.alloc("wsl", [128, 8, 384], BF16) for _ in range(2)]
        WSLb = [Buf("wsl") for _ in range(2)]
        NPB = 4
        PBF = [ar.alloc("pbf", [128, 512], BF16) for _ in range(NPB)]
        PBFb = [Buf("pbf") for _ in range(NPB)]
        ksum = [ar.alloc("ksum", [64, 8], F32) for _ in range(2)]
        diffw = [ar.alloc("diffw", [64, 64], BF16) for _ in range(2)]
        ind = [ar.alloc("ind", [64, 512], BF16) for _ in range(2)]
        gateb = [Buf("gate") for _ in range(2)]
        indb = [Buf("ind") for _ in range(2)]
        rscr = [ar.alloc("rscr", [128, 512], F32) for _ in range(2)]
        rbuf = [ar.alloc("rbuf", [128, 512], F32) for _ in range(2)]
        rbufb = [Buf("rbuf") for _ in range(2)]
        dL1 = [ar.alloc("dL1", [128, 512], F32) for _ in range(2)]
        dL2 = [ar.alloc("dL2", [128, 512], F32) for _ in range(2)]
        dO1 = [ar.alloc("dO1", [128, 512], F32) for _ in range(2)]
        dO2 = [ar.alloc("dO2", [128, 512], F32) for _ in range(2)]
        dSQ = [ar.alloc("dSQ", [128, 512], BF16) for _ in range(2)]
        dRS = [ar.alloc("dRS", [128, 512], F32) for _ in range(2)]
        dfb = [Buf("dfin") for _ in range(2)]

        S.dma("sp", gpre[:], g_pre.partition_broadcast(128), writes=[gpreb])
        for sl in range(2):
            for i in range(4):
                S.op("dve", lambda e, sl=sl, i=i: e.memset(QK[sl][i][:], 0.0), writes=[QKb[sl][i]])
            S.op("dve", lambda e, sl=sl: e.memset(VV[sl][:], 1.0), writes=[VVb[sl]])

        for tt in range(16):
            xin, xinb = XIN[tt % 2], XINb[tt % 2]
            S.dma("sp", xin[:], x[seq, tt * 128:(tt + 1) * 128, :], writes=[xinb])
            norm_transpose(xin[:], xinb, gpre, gpreb, hT, hTb, tt * 128, tt, 7)
        if debug and seq == 0:
            S.dma("sp", dbg["hT"], hT[:], reads=[hTb])

        def load_group_weights(g):
            sl = g % 2
            a0 = g if g < 4 else 12 + (g - 4)
            for i3 in range(3):
                S.dma("pool", WSL[sl][:, :, i3 * 128:(i3 + 1) * 128], w_in_r4[:, :, a0 + 4 * i3, :],
                      writes=[WSLb[sl]])
            hA, hB = (2 * g, 2 * g + 1) if g < 4 else (8 + g - 4, 8 + g - 4)
            q = QK[sl]
            qb_ = QKb[sl]
            S.dma("sp", q[0][64:80, :], c_qx[hA], writes=[qb_[0]])
            S.dma("sp", q[1][64:80, :], c_qx[hB], writes=[qb_[1]])
            S.dma("sp", q[2][64:80, :], c_kx[hA], writes=[qb_[2]])
            S.dma("sp", q[3][64:80, :], c_kx[hB], writes=[qb_[3]])

        pbk = {"i": 0}

        pbk["set"] = (2, 7, 5, 6)

        def pbank():
            pbk["i"] += 1
            st = pbk["set"]
            return st[pbk["i"] % len(st)]

        def prep_units(g):
            sl = g % 2
            q, qb_ = QK[sl], QKb[sl]
            w, wb = WSL[sl], WSLb[sl]
            load_group_weights(g)
            yield
            for c in range(4):
                cs = slice(c * 512, (c + 1) * 512)
                bank = pbank()
                yield from S.split_group(
                    "pe", [(lambda e, kc=kc: e.matmul(ps[:, bank, :], lhsT=w[:, kc, 0:128], rhs=hT[:, kc, cs],
                                                      start=(kc == 0), stop=(kc == 7))) for kc in range(8)],
                    [wb, hTb], [PB[bank]], 2)
                S.op("dve", lambda e: e.tensor_scalar(out=q[0][0:64, cs], in0=ps[0:64, bank, :], scalar1=0.125,
                                                      scalar2=None, op0=ALU.mult),
                     reads=[PB[bank]], writes=[qb_[0]])
                S.op("dve", lambda e: e.tensor_scalar(out=q[1][0:64, cs], in0=ps[64:128, bank, :], scalar1=0.125,
                                                      scalar2=None, op0=ALU.mult),
                     reads=[PB[bank]], writes=[qb_[1]])
                yield
                bank = pbank()
                yield from S.split_group(
                    "pe", [(lambda e, kc=kc: e.matmul(ps[:, bank, :], lhsT=w[:, kc, 128:256], rhs=hT[:, kc, cs],
                                                      start=(kc == 0), stop=(kc == 7))) for kc in range(8)],
                    [wb, hTb], [PB[bank]], 2)
                S.op("dve", lambda e: e.tensor_copy(out=q[2][0:64, cs], in_=ps[0:64, bank, :]),
                     reads=[PB[bank]], writes=[qb_[2]])
                S.op("dve", lambda e: e.tensor_copy(out=q[3][0:64, cs], in_=ps[64:128, bank, :]),
                     reads=[PB[bank]], writes=[qb_[3]])
                yield
            vv, vvb = VV[sl], VVb[sl]
            for tg in range(4):
                bank = pbank()
                fns = []
                for i in range(4):
                    tt = 4 * tg + i
                    for kc in range(8):
                        fns.append(lambda e, i=i, tt=tt, kc=kc: e.matmul(
                            ps[:, bank, i * 128:(i + 1) * 128], lhsT=hT[:, kc, tt * 128:(tt + 1) * 128],
                            rhs=w[:, kc, 256:384], start=(kc == 0), stop=(kc == 7)))
                yield from S.split_group("pe", fns, [wb, hTb], [PB[bank]], 8)
                if g < 4:
                    vq = vv[:].rearrange("p t (a d) -> p t a d", d=64)
                    S.op("dve", lambda e: e.tensor_copy(
                        out=vq[:, 4 * tg:4 * tg + 4, 0:3:2, :],
                        in_=ps[:, bank, :].rearrange("p (t h d) -> p t h d", t=4, h=2)),
                        reads=[PB[bank]], writes=[vvb])
                else:
                    S.op("dve", lambda e: e.tensor_copy(
                        out=vv[:, 4 * tg:4 * tg + 4, 192:320],
                        in_=ps[:, bank, :].rearrange("p (t d) -> p t d", t=4)),
                        reads=[PB[bank]], writes=[vvb])
                yield
            if g < 4:
                for hx in range(2):
                    for _ in moba_gate(g, hx):
                        yield

        def moba_gate(g, hx):
            sl = g % 2
            qt, qtb = QK[sl][hx], QKb[sl][hx]
            kt_, ktb = QK[sl][2 + hx], QKb[sl][2 + hx]
            ks, dw, gb_ = ksum[hx], diffw[hx], gateb[hx]
            S.op("dve", lambda e: e.tensor_reduce(out=ks[:], in_=kt_[0:64, :].rearrange("p (n k) -> p n k", k=256),
                                                  axis=AX.X, op=ALU.add), reads=[ktb], writes=[gb_])
            for n in range(8):
                S.op("dve", lambda e, n=n: e.tensor_scalar(out=dw[:, n * 8:(n + 1) * 8], in0=ks[:, 0:8],
                                                           scalar1=ks[:, n:n + 1], scalar2=None,
                                                           op0=ALU.subtract), reads=[gb_], writes=[gb_])
            yield
            for c in (2, 3):
                cs = slice(c * 512, (c + 1) * 512)
                bank = pbank()
                S.group("pe", [lambda e: e.matmul(ps[0:64, bank, :], lhsT=dw[:, :], rhs=qt[0:64, cs],
                                                  start=True, stop=True)],
                        reads=[gb_, qtb], writes=[PB[bank]])
                for hh in range(2):
                    qblk = 2 * c + hh
                    hs = slice(hh * 256, (hh + 1) * 256)
                    S.op("dve", lambda e, hs=hs, qblk=qblk: e.tensor_scalar(
                        out=ind[hx][:, hs], in0=ps[0:64, bank, hs], scalar1=0.0,
                        scalar2=pastmask[:, qblk:qblk + 1], op0=ALU.is_gt, op1=ALU.mult),
                        reads=[PB[bank], cb], writes=[indb[hx]])
                yield
                bank = pbank()
                S.group("pe", [lambda e: e.matmul(ps[0:72, bank, :], lhsT=pairsum[:, :], rhs=ind[hx][:, :],
                                                  start=True, stop=True)],
                        reads=[indb[hx], cb], writes=[PB[bank]])
                for hh in range(2):
                    qblk = 2 * c + hh
                    hs = slice(hh * 256, (hh + 1) * 256)
                    S.op("dve", lambda e, hs=hs, qblk=qblk: e.tensor_scalar(
                        out=qt[64:72, c * 512 + hh * 256:c * 512 + (hh + 1) * 256], in0=ps[64:72, bank, hs],
                        scalar1=2.5, scalar2=notown[64:72, qblk:qblk + 1], op0=ALU.is_gt, op1=ALU.mult),
                        reads=[PB[bank], cb], writes=[qtb])
                yield

        state = {"s": 0, "p": 0}
        pending = []

        def defer(n, fn):
            pending.append([n, fn])

        def run_pending(flush=False):
            while True:
                ready = None
                for ent in pending:
                    if flush or ent[0] <= 0:
                        ready = ent
                        break
                if ready is None:
                    break
                pending.remove(ready)
                ready[1]()
            for ent in pending:
                ent[0] -= 1

        def attention(items, finalize, filler, every):
            n = len(items)
            sb_of = {}

            def qk(i):
                it = items[i]
                sbank = state["s"] % 2
                state["s"] += 1
                sb_of[i] = sbank
                c, kt = it["c"], it["kt"]
                j = kt - 4 * c
                col0 = 128 * j if j > 0 else 0
                it["col0"] = col0
                fns = [lambda e: e.matmul(ps[:, sbank, col0:512], lhsT=it["k"][0:80, kt * 128:(kt + 1) * 128],
                                          rhs=it["q"][0:80, c * 512 + col0:(c + 1) * 512],
                                          start=True, stop=(j < 0))]
                if j >= 0:
                    fns.append(lambda e: e.matmul(ps[:, sbank, col0:col0 + 128], lhsT=ident[:], rhs=negtri[:],
                                                  start=False, stop=True))
                S.group("pe", fns, reads=[it["kb"], it["qb"], cb], writes=[PB[sbank]])

            def ex_pv(i):
                it = items[i]
                sbank = sb_of[i]
                pi = state["p"] % NPB
                state["p"] += 1
                col0 = it["col0"]
                S.op("act", lambda e: e.activation(out=PBF[pi][:, col0:512], in_=ps[:, sbank, col0:512], func=AF.Exp),
                     reads=[PB[sbank]], writes=[PBFb[pi]])
                fns = []
                banks = []
                rd = [PBFb[pi]]
                for (bank, lap, lbuf) in it["pv"]:
                    fns.append(lambda e, bank=bank, lap=lap: e.matmul(
                        ps[:, bank, col0:512], lhsT=lap, rhs=PBF[pi][:, col0:512],
                        start=(it["kt"] == 0), stop=it["last"]))
                    banks.append(PB[bank])
                    rd.append(lbuf)
                S.group("pe", fns, reads=rd, writes=banks)
                if it["last"]:
                    finalize(it)

            qk(0)
            for i in range(n):
                if i + 1 < n:
                    qk(i + 1)
                ex_pv(i)
                run_pending()
                if filler is not None and i % every == every - 1:
                    next(filler, None)

        def recip(out_ap, in_ap, scr_ap, reads, wb_, on_act=False):
            if on_act:
                S.op("act", lambda e: e.activation(out=scr_ap, in_=in_ap, func=AF.Ln), reads=reads, writes=[wb_])
                S.op("act", lambda e: e.activation(out=out_ap, in_=scr_ap, func=AF.Exp, scale=-1.0),
                     reads=[wb_], writes=[wb_])
            else:
                S.op("dve", lambda e: e.reciprocal(out=out_ap, in_=in_ap), reads=reads, writes=[wb_])

        def run_moba(g, filler):
            sl = g % 2
            for hx in range(2):
                items = []
                for c in range(4):
                    nk = 4 * c + 4
                    bank = 3 + ((hx * 4 + c) % 2)
                    lap_of = (lambda kt: VV[sl][:, kt, 0:128]) if hx == 0 else (lambda kt: VV[sl][:, kt, 64:192])
                    for kt in range(nk):
                        items.append(dict(q=QK[sl][hx], qb=QKb[sl][hx], k=QK[sl][2 + hx], kb=QKb[sl][2 + hx],
                                          c=c, kt=kt, pv=[(bank, lap_of(kt), VVb[sl])], last=(kt == nk - 1),
                                          bank=bank))

                def fin(it, hx=hx):
                    bank, c = it["bank"], it["c"]
                    cs = slice(c * 512, (c + 1) * 512)
                    rb, rbb = rbuf[c % 2], rbufb[c % 2]
                    rs_ = rscr[c % 2]
                    if hx == 0:
                        recip(rb[0:64, :], ps[64:128, bank, :], rs_[0:64, :], [PB[bank]], rbb, on_act=True)
                        S.op("dve", lambda e: e.tensor_tensor(out=yT[0:64, g, cs], in0=ps[0:64, bank, :],
                                                              in1=rb[0:64, :], op=ALU.mult),
                             reads=[PB[bank], rbb], writes=[yTb])
                    else:
                        recip(rb[64:128, :], ps[0:64, bank, :], rs_[64:128, :], [PB[bank]], rbb, on_act=True)
                        S.op("dve", lambda e: e.tensor_tensor(out=yT[64:128, g, cs], in0=ps[64:128, bank, :],
                                                              in1=rb[64:128, :], op=ALU.mult),
                             reads=[PB[bank], rbb], writes=[yTb])
                attention(items, fin, filler, 1)

        def run_diff(g, filler):
            sl = g % 2
            j = g - 4
            items = []
            for c in range(4):
                nk = 4 * c + 4
                for kt in range(nk):
                    for mp in range(2):
                        items.append(dict(q=QK[sl][mp], qb=QKb[sl][mp], k=QK[sl][2 + mp], kb=QKb[sl][2 + mp],
                                          c=c, kt=kt,
                                          pv=[(3 + mp, VV[sl][:, kt, 192:320], VVb[sl]),
                                              (5 + mp, ones_bf[:], cb)],
                                          last=(kt == nk - 1), mp=mp))

            def fin(it):
                if it["mp"] == 0:
                    return
                run_pending(flush=True)
                c = it["c"]
                cs = slice(c * 512, (c + 1) * 512)
                k2 = c % 2
                L1, L2, O1, O2, SQ, RS, fb = dL1[k2], dL2[k2], dO1[k2], dO2[k2], dSQ[k2], dRS[k2], dfb[k2]
                S.op("act", lambda e: e.activation(out=L1[:], in_=ps[:, 5, :], func=AF.Ln), reads=[PB[5]], writes=[fb])
                S.op("dve", lambda e: e.tensor_copy(out=O1[:], in_=ps[:, 3, :]), reads=[PB[3]], writes=[fb])
                S.op("act", lambda e: e.activation(out=L2[:], in_=ps[:, 6, :], func=AF.Ln), reads=[PB[6]], writes=[fb])
                S.op("dve", lambda e: e.tensor_copy(out=O2[:], in_=ps[:, 4, :]), reads=[PB[4]], writes=[fb])

                def stage_b():
                    S.op("act", lambda e: e.activation(out=L1[:], in_=L1[:], func=AF.Exp, scale=-1.0),
                         reads=[fb], writes=[fb])
                    S.op("act", lambda e: e.activation(out=L2[:], in_=L2[:], func=AF.Exp, scale=-1.0),
                         reads=[fb], writes=[fb])
                    S.op("dve", lambda e: e.tensor_tensor(out=O1[:], in0=O1[:], in1=L1[:], op=ALU.mult),
                         reads=[fb], writes=[fb])
                    S.op("dve", lambda e: e.tensor_tensor(out=O2[:], in0=O2[:], in1=L2[:], op=ALU.mult),
                         reads=[fb], writes=[fb])
                    S.op("dve", lambda e: e.scalar_tensor_tensor(out=O1[:], in0=O2[:], scalar=neglam[:, 0:1],
                                                                 in1=O1[:], op0=ALU.mult, op1=ALU.add),
                         reads=[fb, cb], writes=[fb])
                    S.op("act", lambda e: e.activation(out=SQ[:], in_=O1[:], func=AF.Square), reads=[fb], writes=[fb])
                    defer(2, stage_c)

                def stage_c():
                    bank = pbank()
                    S.group("pe", [lambda e: e.matmul(ps[:, bank, :], lhsT=ones_bf[:], rhs=SQ[:], start=True,
                                                      stop=True)], reads=[fb, cb], writes=[PB[bank]])
                    S.op("dve", lambda e: e.tensor_scalar(out=RS[:], in0=ps[:, bank, :], scalar1=1.0 / 128,
                                                          scalar2=1e-5, op0=ALU.mult, op1=ALU.add),
                         reads=[PB[bank]], writes=[fb])
                    S.op("act", lambda e: e.activation(out=RS[:], in_=RS[:], func=AF.Ln), reads=[fb], writes=[fb])
                    S.op("act", lambda e: e.activation(out=RS[:], in_=RS[:], func=AF.Exp, scale=-0.5),
                         reads=[fb], writes=[fb])
                    S.op("dve", lambda e: e.scalar_tensor_tensor(out=yT[:, 4 + j, cs], in0=O1[:], scalar=gsub[:, 0:1],
                                                                 in1=RS[:], op0=ALU.mult, op1=ALU.mult),
                         reads=[fb, cb], writes=[yTb])
                defer(2, stage_b)
            attention(items, fin, filler, 2)

        for _ in prep_units(0):
            pass
        for g in range(8):
            filler = prep_units(g + 1) if g + 1 < 8 else None
            if g < 4:
                pbk["set"] = (2, 7, 5, 6)
                run_moba(g, filler)
            else:
                pbk["set"] = (2, 7)
                run_diff(g, filler)
            if filler is not None:
                for _ in filler:
                    pass
            run_pending(flush=True)
        if debug and seq == 0:
            S.dma("sp", dbg["yT"], yT[:], reads=[yTb])

        S.barrier()
        ar.reset(a_mark)
        mT = ar.alloc("mT", [128, 8, T], BF16)
        mTb = Buf("mT")
        woT = ar.alloc("woT", [128, 8, D], BF16)
        woTb = Buf("woT")
        WM = [ar.alloc("wm", [128, 24, 128], BF16) for _ in range(2)]
        WMb = [Buf("wm") for _ in range(2)]
        gpost = ar.alloc("gpost", [128, D], F32)
        gpostb = Buf("gpost")
        SA = [ar.alloc("sa", [128, 512], F32) for _ in range(2)]
        SB_ = [ar.alloc("sb", [128, 512], F32) for _ in range(2)]
        TA = [ar.alloc("ta", [128, 512], F32) for _ in range(2)]
        TB = [ar.alloc("tb", [128, 512], F32) for _ in range(2)]
        MSb = [Buf("ms") for _ in range(2)]

        def load_merge_weights(j):
            sl = j % 2
            js = slice(128 * j, 128 * j + 128)
            S.dma("pool", WM[sl][:, 0:4, :], w_a_r[:, :, js], writes=[WMb[sl]])
            S.dma("pool", WM[sl][:, 4:8, :], w_b_r[:, :, js], writes=[WMb[sl]])
            S.dma("pool", WM[sl][:, 8:16, :], w_in_r[:, :, 3072 + 128 * j:3072 + 128 * j + 128], writes=[WMb[sl]])
            S.dma("pool", WM[sl][:, 16:24, :], w_in_r[:, :, 4096 + 128 * j:4096 + 128 * j + 128], writes=[WMb[sl]])

        S.dma("sp", gpost[:], g_post.partition_broadcast(128), writes=[gpostb])
        load_merge_weights(0)
        load_merge_weights(1)
        for hf in range(2):
            S.dma("pool", woT[:, 4 * hf:4 * hf + 4, :], w_out_r[:, 4 * hf:4 * hf + 4, :], writes=[woTb])
        it_ = 0
        for j in range(8):
            sl = j % 2
            wm, wmb = WM[sl], WMb[sl]
            for c in range(4):
                cs = slice(c * 512, (c + 1) * 512)
                b0 = 4 * (it_ % 2)
                k2 = it_ % 2
                it_ += 1
                S.group("pe", [(lambda e, kc=kc: e.matmul(ps[:, b0, :], lhsT=wm[:, kc, :], rhs=yT[:, kc, cs],
                                                           start=(kc == 0), stop=(kc == 3))) for kc in range(4)],
                        reads=[wmb, yTb], writes=[PB[b0]])
                S.group("pe", [(lambda e, kc=kc: e.matmul(ps[:, b0 + 1, :], lhsT=wm[:, 4 + kc, :],
                                                           rhs=yT[:, 4 + kc, cs],
                                                           start=(kc == 0), stop=(kc == 3))) for kc in range(4)],
                        reads=[wmb, yTb], writes=[PB[b0 + 1]])
                S.group("pe", [(lambda e, kc=kc: e.matmul(ps[:, b0 + 2, :], lhsT=wm[:, 8 + kc, :], rhs=hT[:, kc, cs],
                                                           start=(kc == 0), stop=(kc == 7))) for kc in range(8)],
                        reads=[wmb, hTb], writes=[PB[b0 + 2]])
                S.group("pe", [(lambda e, kc=kc: e.matmul(ps[:, b0 + 3, :], lhsT=wm[:, 16 + kc, :], rhs=hT[:, kc, cs],
                                                           start=(kc == 0), stop=(kc == 7))) for kc in range(8)],
                        reads=[wmb, hTb], writes=[PB[b0 + 3]])
                S.op("act", lambda e: e.activation(out=SA[k2][:], in_=ps[:, b0 + 2, :], func=AF.Sigmoid),
                     reads=[PB[b0 + 2]], writes=[MSb[k2]])
                S.op("act", lambda e: e.activation(out=SB_[k2][:], in_=ps[:, b0 + 3, :], func=AF.Sigmoid),
                     reads=[PB[b0 + 3]], writes=[MSb[k2]])
                S.op("dve", lambda e: e.tensor_tensor(out=TA[k2][:], in0=ps[:, b0, :], in1=SA[k2][:], op=ALU.mult),
                     reads=[PB[b0], MSb[k2]], writes=[MSb[k2]])
                S.op("dve", lambda e: e.tensor_tensor(out=TB[k2][:], in0=ps[:, b0 + 1, :], in1=SB_[k2][:],
                                                      op=ALU.mult),
                     reads=[PB[b0 + 1], MSb[k2]], writes=[MSb[k2]])
                S.op("dve", lambda e: e.tensor_tensor(out=mT[:, j, cs], in0=TA[k2][:], in1=TB[k2][:], op=ALU.add),
                     reads=[MSb[k2]], writes=[mTb])
            if j + 2 < 8:
                load_merge_weights(j + 2)
        if debug and seq == 0:
            S.dma("sp", dbg["mT"], mT[:], reads=[mTb])
        for tt in range(16):
            ts_ = slice(tt * 128, (tt + 1) * 128)
            banks = (2 * (tt % 2), 2 * (tt % 2) + 1)
            xin, xinb = XIN[tt % 2], XINb[tt % 2]
            S.dma("sp", xin[:], x[seq, ts_, :], writes=[xinb])
            for hf in range(2):
                S.group("pe", [(lambda e, kc=kc: e.matmul(ps[:, banks[hf], :], lhsT=mT[:, kc, ts_],
                                                           rhs=woT[:, kc, hf * 512:(hf + 1) * 512],
                                                           start=(kc == 0), stop=(kc == 7))) for kc in range(8)],
                        reads=[mTb, woTb], writes=[PB[banks[hf]]])
            post_norm_resid(banks, gpost, gpostb, xin[:], xinb, tt, x1s[seq, ts_, :])

        S.barrier()
        ar.reset(base_mark)
        X1 = ar.alloc("X1", [128, 8, D], F32)
        X1b = [Buf("x1_%d" % i) for i in range(8)]
        h2T = ar.alloc("h2T", [128, 8, 1024], BF16)
        h2Tb = Buf("h2T")
        actT = ar.alloc("actT", [128, NF, 1024], BF16)
        actTb = Buf("actT")
        Wd = ar.alloc("Wd", [128, NF, D], BF16)
        Wdb = Buf("Wd")
        WGU = [ar.alloc("wgu", [128, 8, 512], BF16) for _ in range(2)]
        WGUb = [Buf("wgu") for _ in range(2)]
        gfpre = ar.alloc("gfpre", [128, D], F32)
        gfpost = ar.alloc("gfpost", [128, D], F32)
        gfb = Buf("gf")
        SG = [ar.alloc("sg", [128, 512], F32) for _ in range(2)]
        SGb = [Buf("sg") for _ in range(2)]

        S.dma("sp", gfpre[:], g_fpre.partition_broadcast(128), writes=[gfb])
        S.dma("sp", gfpost[:], g_fpost.partition_broadcast(128), writes=[gfb])

        def load_gu(fp):
            sl = fp % 2
            S.dma("pool", WGU[sl][:, :, 0:256], w_gate_r[:, :, 256 * fp:256 * fp + 256], writes=[WGUb[sl]])
            S.dma("pool", WGU[sl][:, :, 256:512], w_up_r[:, :, 256 * fp:256 * fp + 256], writes=[WGUb[sl]])

        nfp = NF // 2
        gu_loaded = [0]
        for half in range(2):
            if half == 0:
                load_gu(0)
                load_gu(1)
                for f2 in range(nfp):
                    S.dma("pool", Wd[:, 2 * f2:2 * f2 + 2, :], w_down_r[:, 2 * f2:2 * f2 + 2, :], writes=[Wdb])
            for t8 in range(8):
                tt = half * 8 + t8
                S.dma("sp", X1[:, t8, :], x1s[seq, tt * 128:(tt + 1) * 128, :], writes=[X1b[t8]])
                norm_transpose(X1[:, t8, :], X1b[t8], gfpre, gfb, h2T, h2Tb, t8 * 128, t8, 7)
            it_ = 0
            for fp in range(nfp):
                sl = fp % 2
                wg, wgb = WGU[sl], WGUb[sl]
                if half == 1 and fp == 0:
                    load_gu(0)
                    load_gu(1)
                for fi in range(2):
                    f = 2 * fp + fi
                    for c2 in range(2):
                        cs = slice(c2 * 512, (c2 + 1) * 512)
                        b0 = 2 * (it_ % 3)
                        k2 = it_ % 2
                        it_ += 1
                        S.group("pe", [(lambda e, kc=kc: e.matmul(ps[:, b0, :], lhsT=wg[:, kc, fi * 128:(fi + 1) * 128],
                                                                   rhs=h2T[:, kc, cs], start=(kc == 0), stop=(kc == 7)))
                                       for kc in range(8)], reads=[wgb, h2Tb], writes=[PB[b0]])
                        S.group("pe", [(lambda e, kc=kc: e.matmul(ps[:, b0 + 1, :],
                                                                   lhsT=wg[:, kc, 256 + fi * 128:256 + (fi + 1) * 128],
                                                                   rhs=h2T[:, kc, cs], start=(kc == 0), stop=(kc == 7)))
                                       for kc in range(8)], reads=[wgb, h2Tb], writes=[PB[b0 + 1]])
                        S.op("act", lambda e: e.activation(out=SG[k2][:], in_=ps[:, b0, :], func=AF.Silu),
                             reads=[PB[b0]], writes=[SGb[k2]])
                        S.op("dve", lambda e: e.tensor_tensor(out=actT[:, f, cs], in0=ps[:, b0 + 1, :], in1=SG[k2][:],
                                                              op=ALU.mult),
                             reads=[PB[b0 + 1], SGb[k2]], writes=[actTb])
                if fp + 2 < nfp:
                    load_gu(fp + 2)
            for t8 in range(8):
                tt = half * 8 + t8
                banks = (2 * (t8 % 2), 2 * (t8 % 2) + 1)
                for hf in range(2):
                    S.group("pe", [(lambda e, f=f: e.matmul(ps[:, banks[hf], :], lhsT=actT[:, f, t8 * 128:(t8 + 1) * 128],
                                                             rhs=Wd[:, f, hf * 512:(hf + 1) * 512],
                                                             start=(f == 0), stop=(f == NF - 1))) for f in range(NF)],
                            reads=[actTb, Wdb], writes=[PB[banks[hf]]])
                post_norm_resid(banks, gfpost, gfb, X1[:, t8, :], X1b[t8], t8, y[seq, tt * 128:(tt + 1) * 128, :])
        S.barrier()
    return nc


_NC_CACHE = {}


def kernel(x, norm_mix_pre_g, w_in, w_branch_a, w_branch_b, lam_q1, lam_k1, lam_q2, lam_k2,
           diff_subln_g, w_out, norm_mix_post_g, norm_ffn_pre_g, w_gate, w_up, w_down, norm_ffn_post_g):
    n = 8
    f32 = lambda a: np.ascontiguousarray(np.asarray(a, dtype=np.float32))
    x = f32(x)
    consts = _host_consts()
    shared = {
        "w_in": f32(w_in)[0], "w_branch_a": f32(w_branch_a)[0], "w_branch_b": f32(w_branch_b)[0],
        "w_out": f32(w_out)[0], "w_gate": f32(w_gate)[0], "w_up": f32(w_up)[0], "w_down": f32(w_down)[0],
        "norm_mix_pre_g": f32(norm_mix_pre_g), "norm_mix_post_g": f32(norm_mix_post_g),
        "norm_ffn_pre_g": f32(norm_ffn_pre_g), "norm_ffn_post_g": f32(norm_ffn_post_g),
        "lamv": np.concatenate([f32(lam_q1)[0], f32(lam_k1)[0], f32(lam_q2)[0], f32(lam_k2)[0]])[None, :],
        "diff_subln_g": f32(diff_subln_g)[0][:, None],
    }
    shared.update(consts)
    nc = build(2)
    in_maps = []
    for c in range(n):
        m = dict(shared)
        m["x"] = np.ascontiguousarray(x[2 * c:2 * c + 2])
        in_maps.append(m)
    res = run_bass_kernel_spmd(nc, in_maps, core_ids=list(range(n)))
    return np.concatenate([r["y"] for r in res.results], axis=0).astype(np.float32)
```

```python
import numpy as np
import ml_dtypes
import concourse.bass as bass
import concourse.mybir as mybir
from concourse.bass_utils import run_bass_kernel_spmd

F32 = mybir.dt.float32
BF16 = mybir.dt.bfloat16
AF = mybir.ActivationFunctionType
ALU = mybir.AluOpType
AX = mybir.AxisListType

T = 2048
D = 1024
DFF = 2816
NF = DFF // 128
NEGM = -30000.0


class Buf:
    __slots__ = ("name", "w", "r")

    def __init__(self, name):
        self.name = name
        self.w = None
        self.r = {}


class EngState:
    def __init__(self, name, eng, sem):
        self.name = name
        self.eng = eng
        self.sem = sem
        self.cnt = 0
        self.seen = {}


class Sync:
    def __init__(self, nc, ndma=20):
        self.nc = nc
        self.E = {}
        for name, e in (("pe", nc.tensor), ("act", nc.scalar), ("dve", nc.vector),
                        ("pool", nc.gpsimd), ("sp", nc.sync)):
            self.E[name] = EngState(name, e, nc.alloc_semaphore("s_" + name))
        self.dsem = {}
        self.dnext = {}
        for q in ("sp", "pool"):
            self.dsem[q] = [[nc.alloc_semaphore("d_%s%d" % (q, i)), 0] for i in range(ndma)]
            self.dnext[q] = 0
        self.semof = {}
        for name, st in self.E.items():
            self.semof[("c", name)] = st.sem
        for q in ("sp", "pool"):
            for i, (s, _) in enumerate(self.dsem[q]):
                self.semof[("d", q, i)] = s

    def _wait(self, E, deps):
        for key, n in deps.items():
            if key == ("c", "pe") and E.name == "pe":
                continue
            if E.seen.get(key, 0) >= n:
                continue
            E.eng.wait_ge(self.semof[key], n)
            E.seen[key] = n

    @staticmethod
    def _deps(reads, writes):
        deps = {}

        def add(key, n):
            if deps.get(key, 0) < n:
                deps[key] = n
        for b in reads:
            if b.w is not None:
                add(*b.w)
        for b in writes:
            if b.w is not None:
                add(*b.w)
            for key, n in b.r.items():
                add(key, n)
        return deps

    @staticmethod
    def _mark(ev, reads, writes):
        key, n = ev
        for b in reads:
            if b.r.get(key, 0) < n:
                b.r[key] = n
        for b in writes:
            b.w = ev
            b.r = {}

    def op(self, engname, fn, reads=(), writes=()):
        E = self.E[engname]
        self._wait(E, self._deps(reads, writes))
        ins = fn(E.eng)
        E.cnt += 1
        ins.then_inc(E.sem, 1)
        ev = (("c", engname), E.cnt)
        self._mark(ev, reads, writes)
        return ev

    def group(self, engname, fns, reads=(), writes=()):
        E = self.E[engname]
        self._wait(E, self._deps(reads, writes))
        ins = None
        for fn in fns:
            ins = fn(E.eng)
        E.cnt += 1
        ins.then_inc(E.sem, 1)
        ev = (("c", engname), E.cnt)
        self._mark(ev, reads, writes)
        return ev

    def split_group(self, engname, fns, reads, writes, per):
        E = self.E[engname]
        self._wait(E, self._deps(reads, writes))
        n = len(fns)
        for i, fn in enumerate(fns):
            ins = fn(E.eng)
            if i == n - 1:
                E.cnt += 1
                ins.then_inc(E.sem, 1)
                self._mark((("c", engname), E.cnt), reads, writes)
            elif (i + 1) % per == 0:
                yield

    def dma(self, q, out, in_, reads=(), writes=()):
        E = self.E[q]
        i = self.dnext[q]
        self.dnext[q] = (i + 1) % len(self.dsem[q])
        slot = self.dsem[q][i]
        key = ("d", q, i)
        deps = self._deps(reads, writes)
        if slot[1] > 0 and deps.get(key, 0) < slot[1]:
            deps[key] = slot[1]
        self._wait(E, deps)
        E.eng.dma_start(out=out, in_=in_).then_inc(slot[0], 16)
        slot[1] += 16
        ev = (key, slot[1])
        self._mark(ev, reads, writes)
        return ev

    def barrier(self, engines=("pe", "act", "dve", "pool", "sp")):
        deps = {}
        for name, st in self.E.items():
            if st.cnt > 0:
                deps[("c", name)] = st.cnt
        for q in ("sp", "pool"):
            for i, (s, n) in enumerate(self.dsem[q]):
                if n > 0:
                    deps[("d", q, i)] = n
        for name in engines:
            self._wait(self.E[name], deps)


class Arena:
    def __init__(self, nc, base, top):
        self.nc = nc
        self.ptr = (base + 31) // 32 * 32
        self.top = top
        self.n = 0

    def alloc(self, name, shape, dtype):
        nbytes = 2 if dtype == BF16 else 4
        sz = 1
        for s in shape[1:]:
            sz *= s
        sz *= nbytes
        off = self.ptr
        self.ptr = (off + sz + 31) // 32 * 32
        assert self.ptr <= self.top, ("SBUF overflow", name, self.ptr, self.top)
        self.n += 1
        return self.nc.alloc_sbuf_tensor_at("%s_%d" % (name, self.n), list(shape), dtype, offset=off)

    def mark(self):
        return self.ptr

    def reset(self, m):
        self.ptr = m


def _alibi_slopes():
    n = 12
    slopes = 2.0 ** (-8.0 * np.arange(1, n + 1) / n)
    diff_idx = np.arange(4) * 3 + 2
    moba_idx = np.setdiff1d(np.arange(n), diff_idx)
    return slopes[moba_idx].astype(np.float32), slopes[diff_idx].astype(np.float32)


def _host_consts():
    bf = ml_dtypes.bfloat16
    ms, ds = _alibi_slopes()
    slopes = np.concatenate([ms, ds]).astype(np.float32)
    pos = np.arange(T)
    hi = (256 * (pos // 256)).astype(np.float32)
    lo = (pos % 256).astype(np.float32)
    qx = np.zeros((12, 16, T), np.float32)
    kx = np.zeros((12, 16, T), np.float32)
    for h in range(12):
        c1 = np.float32(slopes[h]).astype(bf).astype(np.float32)
        c2 = np.float32(slopes[h] - c1).astype(bf).astype(np.float32)
        qx[h, 8], qx[h, 9], qx[h, 10], qx[h, 11] = hi, lo, hi, lo
        qx[h, 12], qx[h, 13], qx[h, 14], qx[h, 15] = c1, c1, c2, c2
        kx[h, 8], kx[h, 9], kx[h, 10], kx[h, 11] = -c1, -c1, -c2, -c2
        kx[h, 12], kx[h, 13], kx[h, 14], kx[h, 15] = hi, lo, hi, lo
        if h < 8:
            for n in range(8):
                kx[h, n] = (pos // 256 == n).astype(np.float32)
    ident = np.eye(128, dtype=np.float32)
    kk = np.arange(128)[:, None]
    qq = np.arange(128)[None, :]
    negtri = np.where(kk <= qq, 0.0, NEGM).astype(np.float32)
    pastmask = np.zeros((64, 8), np.float32)
    pairsum = np.zeros((64, 72), np.float32)
    for n in range(8):
        for m in range(8):
            p = n * 8 + m
            pairsum[p, 64 + n] = 1.0
            for qb in range(8):
                pastmask[p, qb] = 1.0 if m < qb else 0.0
    notown = np.zeros((128, 8), np.float32)
    for n in range(8):
        for qb in range(8):
            notown[64 + n, qb] = NEGM if n < qb else 0.0
    return {
        "c_qx": qx.astype(bf), "c_kx": kx.astype(bf), "c_ident": ident.astype(bf),
        "c_negtri": negtri.astype(bf), "c_pastmask": pastmask, "c_pairsum": pairsum.astype(bf),
        "c_notown": notown,
    }


def build(nseq=2, debug=False):
    nc = bass.Bass("TRN2", target_bir_lowering=False)
    S = Sync(nc)

    def din(name, shape, dt=F32):
        return nc.dram_tensor(name, list(shape), dt, kind="ExternalInput").ap()

    x = din("x", [nseq, T, D])
    w_in = din("w_in", [D, 5120])
    w_a = din("w_branch_a", [512, D])
    w_b = din("w_branch_b", [512, D])
    w_out = din("w_out", [D, D])
    w_gate = din("w_gate", [D, DFF])
    w_up = din("w_up", [D, DFF])
    w_down = din("w_down", [DFF, D])
    g_pre = din("norm_mix_pre_g", [1, D])
    g_post = din("norm_mix_post_g", [1, D])
    g_fpre = din("norm_ffn_pre_g", [1, D])
    g_fpost = din("norm_ffn_post_g", [1, D])
    lamv = din("lamv", [1, 256])
    g_sub = din("diff_subln_g", [128, 1])
    c_qx = din("c_qx", [12, 16, T], BF16)
    c_kx = din("c_kx", [12, 16, T], BF16)
    c_ident = din("c_ident", [128, 128], BF16)
    c_negtri = din("c_negtri", [128, 128], BF16)
    c_pastmask = din("c_pastmask", [64, 8])
    c_pairsum = din("c_pairsum", [64, 72], BF16)
    c_notown = din("c_notown", [128, 8])
    y = nc.dram_tensor("y", [nseq, T, D], F32, kind="ExternalOutput").ap()
    x1s = nc.dram_tensor("x1s", [nseq, T, D], F32).ap()
    dbg = {}
    if debug:
        dbg["hT"] = nc.dram_tensor("dbg_hT", [128, 8, T], BF16, kind="ExternalOutput").ap()
        dbg["yT"] = nc.dram_tensor("dbg_yT", [128, 8, T], BF16, kind="ExternalOutput").ap()
        dbg["mT"] = nc.dram_tensor("dbg_mT", [128, 8, T], BF16, kind="ExternalOutput").ap()

    w_in_r = w_in.rearrange("(kc p) n -> p kc n", p=128)
    w_in_r4 = w_in.rearrange("(kc p) (a n) -> p kc a n", p=128, n=128)
    w_a_r = w_a.rearrange("(kc p) n -> p kc n", p=128)
    w_b_r = w_b.rearrange("(kc p) n -> p kc n", p=128)
    w_out_r = w_out.rearrange("(kc p) n -> p kc n", p=128)
    w_gate_r = w_gate.rearrange("(kc p) n -> p kc n", p=128)
    w_up_r = w_up.rearrange("(kc p) n -> p kc n", p=128)
    w_down_r = w_down.rearrange("(f p) n -> p f n", p=128)

    ar = Arena(nc, nc.sbuf_base, nc.sbuf_top)
    ps = nc.alloc_psum_tensor("ps", [128, 8, 512], F32)
    PB = [Buf("bank%d" % i) for i in range(8)]

    ident = ar.alloc("ident", [128, 128], BF16)
    negtri = ar.alloc("negtri", [128, 128], BF16)
    ones_bf = ar.alloc("ones", [128, 128], BF16)
    pastmask = ar.alloc("pastmask", [64, 8], F32)
    pairsum = ar.alloc("pairsum", [64, 72], BF16)
    notown = ar.alloc("notown", [128, 8], F32)
    gsub = ar.alloc("gsub", [128, 1], F32)
    neglam = ar.alloc("neglam", [128, 1], F32)
    lamt = ar.alloc("lamt", [128, 256], F32)
    lamp = ar.alloc("lamp", [128, 128], F32)
    lams = ar.alloc("lams", [128, 4], F32)
    XIN = [ar.alloc("xin", [128, D], F32) for _ in range(2)]
    XINb = [Buf("xin%d" % i) for i in range(2)]
    HN = [ar.alloc("hn", [128, D], BF16) for _ in range(2)]
    HNb = [Buf("hn%d" % i) for i in range(2)]
    junk = ar.alloc("junk", [128, D], BF16)
    junkb = Buf("junk")
    SS = [ar.alloc("ss", [128, 4], F32) for _ in range(2)]
    SSb = [Buf("ss%d" % i) for i in range(2)]
    TST = [ar.alloc("tst", [128, D], F32) for _ in range(2)]
    TSTb = [Buf("tst%d" % i) for i in range(2)]
    cb = Buf("consts")

    S.dma("sp", ident[:], c_ident, writes=[cb])
    S.dma("sp", negtri[:], c_negtri, writes=[cb])
    S.dma("sp", pastmask[:], c_pastmask, writes=[cb])
    S.dma("sp", pairsum[:], c_pairsum, writes=[cb])
    S.dma("sp", notown[:], c_notown, writes=[cb])
    S.dma("sp", gsub[:], g_sub, writes=[cb])
    S.dma("sp", lamt[:], lamv.partition_broadcast(128), writes=[cb])
    S.op("dve", lambda e: e.memset(ones_bf[:], 1.0), writes=[cb])
    lamtv = lamt[:].rearrange("p (a b d) -> p a b d", a=2, b=2)
    S.op("dve", lambda e: e.tensor_tensor(out=lamp[:].rearrange("p (a d) -> p a d", a=2),
                                          in0=lamtv[:, :, 0, :], in1=lamtv[:, :, 1, :], op=ALU.mult),
         reads=[cb], writes=[cb])
    S.op("dve", lambda e: e.tensor_reduce(out=lams[:, 0:2], in_=lamp[:].rearrange("p (a d) -> p a d", a=2),
                                          axis=AX.X, op=ALU.add), reads=[cb], writes=[cb])
    S.op("act", lambda e: e.activation(out=lams[:, 2:4], in_=lams[:, 0:2], func=AF.Exp), reads=[cb], writes=[cb])
    S.op("dve", lambda e: e.scalar_tensor_tensor(out=neglam[:], in0=lams[:, 3:4], scalar=-0.2, in1=lams[:, 2:3],
                                                 op0=ALU.add, op1=ALU.subtract), reads=[cb], writes=[cb])
    S.op("dve", lambda e: e.tensor_scalar(out=gsub[:], in0=gsub[:], scalar1=0.8, scalar2=None, op0=ALU.mult),
         reads=[cb], writes=[cb])

    base_mark = ar.mark()

    def rms_rstd(ssap, rstd_ap, n_inv, eps, sb):
        S.op("dve", lambda e: e.tensor_scalar(out=rstd_ap, in0=ssap, scalar1=n_inv, scalar2=eps,
                                              op0=ALU.mult, op1=ALU.add), reads=[sb], writes=[sb])
        S.op("act", lambda e: e.activation(out=rstd_ap, in_=rstd_ap, func=AF.Ln), reads=[sb], writes=[sb])
        S.op("act", lambda e: e.activation(out=rstd_ap, in_=rstd_ap, func=AF.Exp, scale=-0.5),
             reads=[sb], writes=[sb])

    def norm_transpose(src_ap, srcb, gt, gtb, dstT, dstb, tcol, k, bank):
        ss, ssb = SS[k % 2], SSb[k % 2]
        hn, hnb = HN[k % 2], HNb[k % 2]
        S.op("dve", lambda e: e.memset(ss[:], 0.0), writes=[ssb])
        S.op("act", lambda e: e.activation(out=junk[:], in_=src_ap, func=AF.Square, accum_out=ss[:, 0:1]),
             reads=[srcb], writes=[junkb, ssb])
        rms_rstd(ss[:, 0:1], ss[:, 1:2], 1.0 / D, 1e-6, ssb)
        S.op("dve", lambda e: e.scalar_tensor_tensor(out=hn[:], in0=src_ap, scalar=ss[:, 1:2], in1=gt[:],
                                                     op0=ALU.mult, op1=ALU.mult),
             reads=[srcb, ssb, gtb], writes=[hnb])
        psb = ps[:, bank, :].bitcast(BF16)
        S.group("pe", [(lambda e, kc=kc: e.transpose(out=psb[:, kc * 128:(kc + 1) * 128],
                                                      in_=hn[:, kc * 128:(kc + 1) * 128], identity=ident[:]))
                       for kc in range(8)], reads=[hnb, cb], writes=[PB[bank]])
        S.op("act", lambda e: e.activation(out=dstT[:, :, tcol:tcol + 128],
                                           in_=psb.rearrange("p (k t) -> p k t", k=8), func=AF.Copy),
             reads=[PB[bank]], writes=[dstb])

    def post_norm_resid(banks, gt, gtb, resid_ap, residb, k, out_dram):
        ss, ssb = SS[k % 2], SSb[k % 2]
        tst, tstb = TST[k % 2], TSTb[k % 2]
        S.op("dve", lambda e: e.memset(ss[:], 0.0), writes=[ssb])
        for hf in range(2):
            S.op("act", lambda e, hf=hf: e.activation(out=junk[:, 0:512], in_=ps[:, banks[hf], :], func=AF.Square,
                                                      accum_out=ss[:, hf:hf + 1]),
                 reads=[PB[banks[hf]]], writes=[junkb, ssb])
        S.op("dve", lambda e: e.tensor_tensor(out=ss[:, 2:3], in0=ss[:, 0:1], in1=ss[:, 1:2], op=ALU.add),
             reads=[ssb], writes=[ssb])
        rms_rstd(ss[:, 2:3], ss[:, 3:4], 1.0 / D, 1e-6, ssb)
        for hf in range(2):
            S.op("dve", lambda e, hf=hf: e.scalar_tensor_tensor(
                out=tst[:, hf * 512:(hf + 1) * 512], in0=ps[:, banks[hf], :], scalar=ss[:, 3:4],
                in1=gt[:, hf * 512:(hf + 1) * 512], op0=ALU.mult, op1=ALU.mult),
                reads=[PB[banks[hf]], ssb, gtb], writes=[tstb])
        S.op("dve", lambda e: e.tensor_tensor(out=tst[:], in0=tst[:], in1=resid_ap, op=ALU.add),
             reads=[tstb, residb], writes=[tstb])
        S.dma("sp", out_dram, tst[:], reads=[tstb])

    for seq in range(nseq):
        ar.reset(base_mark)
        hT = ar.alloc("hT", [128, 8, T], BF16)
        hTb = Buf("hT")
        yT = ar.alloc("yT", [128, 8, T], BF16)
        yTb = Buf("yT")
        a_mark = ar.mark()
        gpre = ar.alloc("gpre", [128, D], F32)
        gpreb = Buf("gpre")
        QK = [[ar.alloc("qk", [128, T], BF16) for _ in range(4)] for _ in range(2)]
        QKb = [[Buf("qk") for _ in range(4)] for _ in range(2)]
        VV = [ar.alloc("vv", [128, 16, 320], BF16) for _ in range(2)]
        VVb = [Buf("vv") for _ in range(2)]
        WSL = [ar.alloc("wsl", [128, 8, 384], BF16) for _ in range(2)]
        WSLb = [Buf("wsl") for _ in range(2)]
        NPB = 5
        PBF = [ar.alloc("pbf", [128, 512], BF16) for _ in range(NPB)]
        PBFb = [Buf("pbf") for _ in range(NPB)]
        ksum = [ar.alloc("ksum", [64, 8], F32) for _ in range(2)]
        diffw = [ar.alloc("diffw", [64, 64], BF16) for _ in range(2)]
        ind = [ar.alloc("ind", [64, 512], BF16) for _ in range(2)]
        gateb = [Buf("gate") for _ in range(2)]
        indb = [Buf("ind") for _ in range(2)]
        rscr = [ar.alloc("rscr", [128, 512], F32) for _ in range(2)]
        rbuf = [ar.alloc("rbuf", [128, 512], F32) for _ in range(2)]
        rbufb = [Buf("rbuf") for _ in range(2)]
        dL1 = [ar.alloc("dL1", [128, 512], F32) for _ in range(2)]
        dL2 = [ar.alloc("dL2", [128, 512], F32) for _ in range(2)]
        dO1 = [ar.alloc("dO1", [128, 512], F32) for _ in range(2)]
        dO2 = [ar.alloc("dO2", [128, 512], F32) for _ in range(2)]
        dSQ = [ar.alloc("dSQ", [128, 512], BF16) for _ in range(2)]
        dRS = [ar.alloc("dRS", [128, 512], F32) for _ in range(2)]
        dfb = [Buf("dfin") for _ in range(2)]

        S.dma("sp", gpre[:], g_pre.partition_broadcast(128), writes=[gpreb])
        for sl in range(2):
            for i in range(4):
                S.op("dve", lambda e, sl=sl, i=i: e.memset(QK[sl][i][:], 0.0), writes=[QKb[sl][i]])
            S.op("dve", lambda e, sl=sl: e.memset(VV[sl][:], 1.0), writes=[VVb[sl]])

        for tt in range(16):
            xin, xinb = XIN[tt % 2], XINb[tt % 2]
            S.dma("sp", xin[:], x[seq, tt * 128:(tt + 1) * 128, :], writes=[xinb])
            norm_transpose(xin[:], xinb, gpre, gpreb, hT, hTb, tt * 128, tt, 7)
        if debug and seq == 0:
            S.dma("sp", dbg["hT"], hT[:], reads=[hTb])

        def load_group_weights(g):
            sl = g % 2
            a0 = g if g < 4 else 12 + (g - 4)
            for i3 in range(3):
                S.dma("pool", WSL[sl][:, :, i3 * 128:(i3 + 1) * 128], w_in_r4[:, :, a0 + 4 * i3, :],
                      writes=[WSLb[sl]])
            hA, hB = (2 * g, 2 * g + 1) if g < 4 else (8 + g - 4, 8 + g - 4)
            q = QK[sl]
            qb_ = QKb[sl]
            S.dma("sp", q[0][64:80, :], c_qx[hA], writes=[qb_[0]])
            S.dma("sp", q[1][64:80, :], c_qx[hB], writes=[qb_[1]])
            S.dma("sp", q[2][64:80, :], c_kx[hA], writes=[qb_[2]])
            S.dma("sp", q[3][64:80, :], c_kx[hB], writes=[qb_[3]])

        pbk = {"i": 0}

        pbk["set"] = (7, 5, 6)

        def pbank():
            pbk["i"] += 1
            st = pbk["set"]
            return st[pbk["i"] % len(st)]

        def prep_units(g):
            sl = g % 2
            q, qb_ = QK[sl], QKb[sl]
            w, wb = WSL[sl], WSLb[sl]
            load_group_weights(g)
            yield
            for c in range(4):
                cs = slice(c * 512, (c + 1) * 512)
                bank = pbank()
                yield from S.split_group(
                    "pe", [(lambda e, kc=kc: e.matmul(ps[:, bank, :], lhsT=w[:, kc, 0:128], rhs=hT[:, kc, cs],
                                                      start=(kc == 0), stop=(kc == 7))) for kc in range(8)],
                    [wb, hTb], [PB[bank]], 2)
                S.op("dve", lambda e: e.tensor_scalar(out=q[0][0:64, cs], in0=ps[0:64, bank, :], scalar1=0.125,
                                                      scalar2=None, op0=ALU.mult),
                     reads=[PB[bank]], writes=[qb_[0]])
                S.op("dve", lambda e: e.tensor_scalar(out=q[1][0:64, cs], in0=ps[64:128, bank, :], scalar1=0.125,
                                                      scalar2=None, op0=ALU.mult),
                     reads=[PB[bank]], writes=[qb_[1]])
                yield
                bank = pbank()
                yield from S.split_group(
                    "pe", [(lambda e, kc=kc: e.matmul(ps[:, bank, :], lhsT=w[:, kc, 128:256], rhs=hT[:, kc, cs],
                                                      start=(kc == 0), stop=(kc == 7))) for kc in range(8)],
                    [wb, hTb], [PB[bank]], 2)
                S.op("dve", lambda e: e.tensor_copy(out=q[2][0:64, cs], in_=ps[0:64, bank, :]),
                     reads=[PB[bank]], writes=[qb_[2]])
                S.op("dve", lambda e: e.tensor_copy(out=q[3][0:64, cs], in_=ps[64:128, bank, :]),
                     reads=[PB[bank]], writes=[qb_[3]])
                yield
            vv, vvb = VV[sl], VVb[sl]
            for tg in range(4):
                bank = pbank()
                fns = []
                for i in range(4):
                    tt = 4 * tg + i
                    for kc in range(8):
                        fns.append(lambda e, i=i, tt=tt, kc=kc: e.matmul(
                            ps[:, bank, i * 128:(i + 1) * 128], lhsT=hT[:, kc, tt * 128:(tt + 1) * 128],
                            rhs=w[:, kc, 256:384], start=(kc == 0), stop=(kc == 7)))
                yield from S.split_group("pe", fns, [wb, hTb], [PB[bank]], 4)
                if g < 4:
                    vq = vv[:].rearrange("p t (a d) -> p t a d", d=64)
                    S.op("dve", lambda e: e.tensor_copy(
                        out=vq[:, 4 * tg:4 * tg + 4, 0:3:2, :],
                        in_=ps[:, bank, :].rearrange("p (t h d) -> p t h d", t=4, h=2)),
                        reads=[PB[bank]], writes=[vvb])
                else:
                    S.op("dve", lambda e: e.tensor_copy(
                        out=vv[:, 4 * tg:4 * tg + 4, 192:320],
                        in_=ps[:, bank, :].rearrange("p (t d) -> p t d", t=4)),
                        reads=[PB[bank]], writes=[vvb])
                yield
            if g < 4:
                for hx in range(2):
                    for _ in moba_gate(g, hx):
                        yield

        def moba_gate(g, hx):
            sl = g % 2
            qt, qtb = QK[sl][hx], QKb[sl][hx]
            kt_, ktb = QK[sl][2 + hx], QKb[sl][2 + hx]
            ks, dw, gb_ = ksum[hx], diffw[hx], gateb[hx]
            S.op("dve", lambda e: e.tensor_reduce(out=ks[:], in_=kt_[0:64, :].rearrange("p (n k) -> p n k", k=256),
                                                  axis=AX.X, op=ALU.add), reads=[ktb], writes=[gb_])
            for n in range(8):
                S.op("dve", lambda e, n=n: e.tensor_scalar(out=dw[:, n * 8:(n + 1) * 8], in0=ks[:, 0:8],
                                                           scalar1=ks[:, n:n + 1], scalar2=None,
                                                           op0=ALU.subtract), reads=[gb_], writes=[gb_])
            yield
            for c in (2, 3):
                cs = slice(c * 512, (c + 1) * 512)
                bank = pbank()
                S.group("pe", [lambda e: e.matmul(ps[0:64, bank, :], lhsT=dw[:, :], rhs=qt[0:64, cs],
                                                  start=True, stop=True)],
                        reads=[gb_, qtb], writes=[PB[bank]])
                for hh in range(2):
                    qblk = 2 * c + hh
                    hs = slice(hh * 256, (hh + 1) * 256)
                    S.op("dve", lambda e, hs=hs, qblk=qblk: e.tensor_scalar(
                        out=ind[hx][:, hs], in0=ps[0:64, bank, hs], scalar1=0.0,
                        scalar2=pastmask[:, qblk:qblk + 1], op0=ALU.is_gt, op1=ALU.mult),
                        reads=[PB[bank], cb], writes=[indb[hx]])
                yield
                bank = pbank()
                S.group("pe", [lambda e: e.matmul(ps[0:72, bank, :], lhsT=pairsum[:, :], rhs=ind[hx][:, :],
                                                  start=True, stop=True)],
                        reads=[indb[hx], cb], writes=[PB[bank]])
                for hh in range(2):
                    qblk = 2 * c + hh
                    hs = slice(hh * 256, (hh + 1) * 256)
                    S.op("dve", lambda e, hs=hs, qblk=qblk: e.tensor_scalar(
                        out=qt[64:72, c * 512 + hh * 256:c * 512 + (hh + 1) * 256], in0=ps[64:72, bank, hs],
                        scalar1=2.5, scalar2=notown[64:72, qblk:qblk + 1], op0=ALU.is_gt, op1=ALU.mult),
                        reads=[PB[bank], cb], writes=[qtb])
                yield

        state = {"s": 0, "p": 0, "la": 2, "sb": (0, 1, 2)}
        pending = []

        def defer(n, fn):
            pending.append([n, fn])

        def run_pending(flush=False):
            while True:
                ready = None
                for ent in pending:
                    if flush or ent[0] <= 0:
                        ready = ent
                        break
                if ready is None:
                    break
                pending.remove(ready)
                ready[1]()
            for ent in pending:
                ent[0] -= 1

        def attention(items, finalize, filler, every):
            n = len(items)
            sb_of = {}

            def qk(i):
                it = items[i]
                sbank = state["sb"][state["s"] % len(state["sb"])]
                state["s"] += 1
                sb_of[i] = sbank
                c, kt = it["c"], it["kt"]
                j = kt - 4 * c
                col0 = 128 * j if j > 0 else 0
                it["col0"] = col0
                fns = [lambda e: e.matmul(ps[:, sbank, col0:512], lhsT=it["k"][0:80, kt * 128:(kt + 1) * 128],
                                          rhs=it["q"][0:80, c * 512 + col0:(c + 1) * 512],
                                          start=True, stop=(j < 0))]
                if j >= 0:
                    fns.append(lambda e: e.matmul(ps[:, sbank, col0:col0 + 128], lhsT=ident[:], rhs=negtri[:],
                                                  start=False, stop=True))
                S.group("pe", fns, reads=[it["kb"], it["qb"], cb], writes=[PB[sbank]])

            def ex_pv(i):
                it = items[i]
                sbank = sb_of[i]
                pi = state["p"] % NPB
                state["p"] += 1
                col0 = it["col0"]
                S.op("act", lambda e: e.activation(out=PBF[pi][:, col0:512], in_=ps[:, sbank, col0:512], func=AF.Exp),
                     reads=[PB[sbank]], writes=[PBFb[pi]])
                fns = []
                banks = []
                rd = [PBFb[pi]]
                for (bank, lap, lbuf) in it["pv"]:
                    fns.append(lambda e, bank=bank, lap=lap: e.matmul(
                        ps[:, bank, col0:512], lhsT=lap, rhs=PBF[pi][:, col0:512],
                        start=(it["kt"] == 0), stop=it["last"]))
                    banks.append(PB[bank])
                    rd.append(lbuf)
                S.group("pe", fns, reads=rd, writes=banks)
                if it["last"]:
                    finalize(it)

            la = state["la"]
            for i in range(min(la, n)):
                qk(i)
            for i in range(n):
                if i + la < n:
                    qk(i + la)
                ex_pv(i)
                run_pending()
                if filler is not None and i % every == every - 1:
                    next(filler, None)

        def recip(out_ap, in_ap, scr_ap, reads, wb_, on_act=False):
            if on_act:
                S.op("act", lambda e: e.activation(out=scr_ap, in_=in_ap, func=AF.Ln), reads=reads, writes=[wb_])
                S.op("act", lambda e: e.activation(out=out_ap, in_=scr_ap, func=AF.Exp, scale=-1.0),
                     reads=[wb_], writes=[wb_])
            else:
                S.op("dve", lambda e: e.reciprocal(out=out_ap, in_=in_ap), reads=reads, writes=[wb_])

        def run_moba(g, filler):
            sl = g % 2
            for hx in range(2):
                items = []
                for c in range(4):
                    nk = 4 * c + 4
                    bank = 3 + ((hx * 4 + c) % 2)
                    lap_of = (lambda kt: VV[sl][:, kt, 0:128]) if hx == 0 else (lambda kt: VV[sl][:, kt, 64:192])
                    for kt in range(nk):
                        items.append(dict(q=QK[sl][hx], qb=QKb[sl][hx], k=QK[sl][2 + hx], kb=QKb[sl][2 + hx],
                                          c=c, kt=kt, pv=[(bank, lap_of(kt), VVb[sl])], last=(kt == nk - 1),
                                          bank=bank))

                def fin(it, hx=hx):
                    bank, c = it["bank"], it["c"]
                    cs = slice(c * 512, (c + 1) * 512)
                    rb, rbb = rbuf[c % 2], rbufb[c % 2]
                    rs_ = rscr[c % 2]
                    if hx == 0:
                        recip(rb[0:64, :], ps[64:128, bank, :], rs_[0:64, :], [PB[bank]], rbb, on_act=True)
                        S.op("dve", lambda e: e.tensor_tensor(out=yT[0:64, g, cs], in0=ps[0:64, bank, :],
                                                              in1=rb[0:64, :], op=ALU.mult),
                             reads=[PB[bank], rbb], writes=[yTb])
                    else:
                        recip(rb[64:128, :], ps[0:64, bank, :], rs_[64:128, :], [PB[bank]], rbb, on_act=True)
                        S.op("dve", lambda e: e.tensor_tensor(out=yT[64:128, g, cs], in0=ps[64:128, bank, :],
                                                              in1=rb[64:128, :], op=ALU.mult),
                             reads=[PB[bank], rbb], writes=[yTb])
                attention(items, fin, filler, 1)

        def run_diff(g, filler):
            sl = g % 2
            j = g - 4
            items = []
            for c in range(4):
                nk = 4 * c + 4
                for kt in range(nk):
                    for mp in range(2):
                        items.append(dict(q=QK[sl][mp], qb=QKb[sl][mp], k=QK[sl][2 + mp], kb=QKb[sl][2 + mp],
                                          c=c, kt=kt,
                                          pv=[(3 + mp, VV[sl][:, kt, 192:320], VVb[sl]),
                                              (5 + mp, ones_bf[:], cb)],
                                          last=(kt == nk - 1), mp=mp))

            def fin(it):
                if it["mp"] == 0:
                    return
                run_pending(flush=True)
                c = it["c"]
                cs = slice(c * 512, (c + 1) * 512)
                k2 = c % 2
                L1, L2, O1, O2, SQ, RS, fb = dL1[k2], dL2[k2], dO1[k2], dO2[k2], dSQ[k2], dRS[k2], dfb[k2]
                S.op("act", lambda e: e.activation(out=L1[:], in_=ps[:, 5, :], func=AF.Ln), reads=[PB[5]], writes=[fb])
                S.op("dve", lambda e: e.tensor_copy(out=O1[:], in_=ps[:, 3, :]), reads=[PB[3]], writes=[fb])
                S.op("act", lambda e: e.activation(out=L2[:], in_=ps[:, 6, :], func=AF.Ln), reads=[PB[6]], writes=[fb])
                S.op("dve", lambda e: e.tensor_copy(out=O2[:], in_=ps[:, 4, :]), reads=[PB[4]], writes=[fb])

                def stage_b():
                    S.op("act", lambda e: e.activation(out=L1[:], in_=L1[:], func=AF.Exp, scale=-1.0),
                         reads=[fb], writes=[fb])
                    S.op("act", lambda e: e.activation(out=L2[:], in_=L2[:], func=AF.Exp, scale=-1.0),
                         reads=[fb], writes=[fb])
                    S.op("dve", lambda e: e.tensor_tensor(out=O1[:], in0=O1[:], in1=L1[:], op=ALU.mult),
                         reads=[fb], writes=[fb])
                    S.op("dve", lambda e: e.tensor_tensor(out=O2[:], in0=O2[:], in1=L2[:], op=ALU.mult),
                         reads=[fb], writes=[fb])
                    S.op("dve", lambda e: e.scalar_tensor_tensor(out=O1[:], in0=O2[:], scalar=neglam[:, 0:1],
                                                                 in1=O1[:], op0=ALU.mult, op1=ALU.add),
                         reads=[fb, cb], writes=[fb])
                    S.op("act", lambda e: e.activation(out=SQ[:], in_=O1[:], func=AF.Square), reads=[fb], writes=[fb])
                    defer(2, stage_c)

                def stage_c():
                    bank = pbank()
                    S.group("pe", [lambda e: e.matmul(ps[:, bank, :], lhsT=ones_bf[:], rhs=SQ[:], start=True,
                                                      stop=True)], reads=[fb, cb], writes=[PB[bank]])
                    S.op("dve", lambda e: e.tensor_scalar(out=RS[:], in0=ps[:, bank, :], scalar1=1.0 / 128,
                                                          scalar2=1e-5, op0=ALU.mult, op1=ALU.add),
                         reads=[PB[bank]], writes=[fb])
                    S.op("act", lambda e: e.activation(out=RS[:], in_=RS[:], func=AF.Ln), reads=[fb], writes=[fb])
                    S.op("act", lambda e: e.activation(out=RS[:], in_=RS[:], func=AF.Exp, scale=-0.5),
                         reads=[fb], writes=[fb])
                    S.op("dve", lambda e: e.scalar_tensor_tensor(out=yT[:, 4 + j, cs], in0=O1[:], scalar=gsub[:, 0:1],
                                                                 in1=RS[:], op0=ALU.mult, op1=ALU.mult),
                         reads=[fb, cb], writes=[yTb])
                defer(2, stage_b)
            attention(items, fin, filler, 2)

        for _ in prep_units(0):
            pass
        for g in range(8):
            filler = prep_units(g + 1) if g + 1 < 8 else None
            if g < 4:
                pbk["set"] = (7, 5, 6)
                state["la"], state["sb"] = 2, (0, 1, 2)
                run_moba(g, filler)
            else:
                pbk["set"] = (2, 7)
                state["la"], state["sb"] = 1, (0, 1)
                run_diff(g, filler)
            if filler is not None:
                for _ in filler:
                    pass
            run_pending(flush=True)
        if debug and seq == 0:
            S.dma("sp", dbg["yT"], yT[:], reads=[yTb])

        S.barrier()
        ar.reset(a_mark)
        mT = ar.alloc("mT", [128, 8, T], BF16)
        mTb = Buf("mT")
        woT = ar.alloc("woT", [128, 8, D], BF16)
        woTb = Buf("woT")
        WM = [ar.alloc("wm", [128, 24, 128], BF16) for _ in range(2)]
        WMb = [Buf("wm") for _ in range(2)]
        gpost = ar.alloc("gpost", [128, D], F32)
        gpostb = Buf("gpost")
        SA = [ar.alloc("sa", [128, 512], F32) for _ in range(2)]
        SB_ = [ar.alloc("sb", [128, 512], F32) for _ in range(2)]
        TA = [ar.alloc("ta", [128, 512], F32) for _ in range(2)]
        TB = [ar.alloc("tb", [128, 512], F32) for _ in range(2)]
        MSb = [Buf("ms") for _ in range(2)]

        def load_merge_weights(j):
            sl = j % 2
            js = slice(128 * j, 128 * j + 128)
            S.dma("pool", WM[sl][:, 0:4, :], w_a_r[:, :, js], writes=[WMb[sl]])
            S.dma("pool", WM[sl][:, 4:8, :], w_b_r[:, :, js], writes=[WMb[sl]])
            S.dma("pool", WM[sl][:, 8:16, :], w_in_r[:, :, 3072 + 128 * j:3072 + 128 * j + 128], writes=[WMb[sl]])
            S.dma("pool", WM[sl][:, 16:24, :], w_in_r[:, :, 4096 + 128 * j:4096 + 128 * j + 128], writes=[WMb[sl]])

        S.dma("sp", gpost[:], g_post.partition_broadcast(128), writes=[gpostb])
        load_merge_weights(0)
        load_merge_weights(1)
        for hf in range(2):
            S.dma("pool", woT[:, 4 * hf:4 * hf + 4, :], w_out_r[:, 4 * hf:4 * hf + 4, :], writes=[woTb])
        it_ = 0
        for j in range(8):
            sl = j % 2
            wm, wmb = WM[sl], WMb[sl]
            for c in range(4):
                cs = slice(c * 512, (c + 1) * 512)
                b0 = 4 * (it_ % 2)
                k2 = it_ % 2
                it_ += 1
                S.group("pe", [(lambda e, kc=kc: e.matmul(ps[:, b0, :], lhsT=wm[:, kc, :], rhs=yT[:, kc, cs],
                                                           start=(kc == 0), stop=(kc == 3))) for kc in range(4)],
                        reads=[wmb, yTb], writes=[PB[b0]])
                S.group("pe", [(lambda e, kc=kc: e.matmul(ps[:, b0 + 1, :], lhsT=wm[:, 4 + kc, :],
                                                           rhs=yT[:, 4 + kc, cs],
                                                           start=(kc == 0), stop=(kc == 3))) for kc in range(4)],
                        reads=[wmb, yTb], writes=[PB[b0 + 1]])
                S.group("pe", [(lambda e, kc=kc: e.matmul(ps[:, b0 + 2, :], lhsT=wm[:, 8 + kc, :], rhs=hT[:, kc, cs],
                                                           start=(kc == 0), stop=(kc == 7))) for kc in range(8)],
                        reads=[wmb, hTb], writes=[PB[b0 + 2]])
                S.group("pe", [(lambda e, kc=kc: e.matmul(ps[:, b0 + 3, :], lhsT=wm[:, 16 + kc, :], rhs=hT[:, kc, cs],
                                                           start=(kc == 0), stop=(kc == 7))) for kc in range(8)],
                        reads=[wmb, hTb], writes=[PB[b0 + 3]])
                S.op("act", lambda e: e.activation(out=SA[k2][:], in_=ps[:, b0 + 2, :], func=AF.Sigmoid),
                     reads=[PB[b0 + 2]], writes=[MSb[k2]])
                S.op("act", lambda e: e.activation(out=SB_[k2][:], in_=ps[:, b0 + 3, :], func=AF.Sigmoid),
                     reads=[PB[b0 + 3]], writes=[MSb[k2]])
                S.op("dve", lambda e: e.tensor_tensor(out=TA[k2][:], in0=ps[:, b0, :], in1=SA[k2][:], op=ALU.mult),
                     reads=[PB[b0], MSb[k2]], writes=[MSb[k2]])
                S.op("dve", lambda e: e.tensor_tensor(out=TB[k2][:], in0=ps[:, b0 + 1, :], in1=SB_[k2][:],
                                                      op=ALU.mult),
                     reads=[PB[b0 + 1], MSb[k2]], writes=[MSb[k2]])
                S.op("dve", lambda e: e.tensor_tensor(out=mT[:, j, cs], in0=TA[k2][:], in1=TB[k2][:], op=ALU.add),
                     reads=[MSb[k2]], writes=[mTb])
            if j + 2 < 8:
                load_merge_weights(j + 2)
        if debug and seq == 0:
            S.dma("sp", dbg["mT"], mT[:], reads=[mTb])
        for tt in range(16):
            ts_ = slice(tt * 128, (tt + 1) * 128)
            banks = (2 * (tt % 2), 2 * (tt % 2) + 1)
            xin, xinb = XIN[tt % 2], XINb[tt % 2]
            S.dma("sp", xin[:], x[seq, ts_, :], writes=[xinb])
            for hf in range(2):
                S.group("pe", [(lambda e, kc=kc: e.matmul(ps[:, banks[hf], :], lhsT=mT[:, kc, ts_],
                                                           rhs=woT[:, kc, hf * 512:(hf + 1) * 512],
                                                           start=(kc == 0), stop=(kc == 7))) for kc in range(8)],
                        reads=[mTb, woTb], writes=[PB[banks[hf]]])
            post_norm_resid(banks, gpost, gpostb, xin[:], xinb, tt, x1s[seq, ts_, :])

        S.barrier()
        ar.reset(base_mark)
        X1 = ar.alloc("X1", [128, 8, D], F32)
        X1b = [Buf("x1_%d" % i) for i in range(8)]
        h2T = ar.alloc("h2T", [128, 8, 1024], BF16)
        h2Tb = Buf("h2T")
        actT = ar.alloc("actT", [128, NF, 1024], BF16)
        actTb = Buf("actT")
        Wd = ar.alloc("Wd", [128, NF, D], BF16)
        Wdb = Buf("Wd")
        WGU = [ar.alloc("wgu", [128, 8, 512], BF16) for _ in range(2)]
        WGUb = [Buf("wgu") for _ in range(2)]
        gfpre = ar.alloc("gfpre", [128, D], F32)
        gfpost = ar.alloc("gfpost", [128, D], F32)
        gfb = Buf("gf")
        SG = [ar.alloc("sg", [128, 512], F32) for _ in range(2)]
        SGb = [Buf("sg") for _ in range(2)]

        S.dma("sp", gfpre[:], g_fpre.partition_broadcast(128), writes=[gfb])
        S.dma("sp", gfpost[:], g_fpost.partition_broadcast(128), writes=[gfb])

        def load_gu(fp):
            sl = fp % 2
            S.dma("pool", WGU[sl][:, :, 0:256], w_gate_r[:, :, 256 * fp:256 * fp + 256], writes=[WGUb[sl]])
            S.dma("pool", WGU[sl][:, :, 256:512], w_up_r[:, :, 256 * fp:256 * fp + 256], writes=[WGUb[sl]])

        nfp = NF // 2
        gu_loaded = [0]
        for half in range(2):
            if half == 0:
                load_gu(0)
                load_gu(1)
                for f2 in range(nfp):
                    S.dma("pool", Wd[:, 2 * f2:2 * f2 + 2, :], w_down_r[:, 2 * f2:2 * f2 + 2, :], writes=[Wdb])
            for t8 in range(8):
                tt = half * 8 + t8
                S.dma("sp", X1[:, t8, :], x1s[seq, tt * 128:(tt + 1) * 128, :], writes=[X1b[t8]])
                norm_transpose(X1[:, t8, :], X1b[t8], gfpre, gfb, h2T, h2Tb, t8 * 128, t8, 7)
            it_ = 0
            for fp in range(nfp):
                sl = fp % 2
                wg, wgb = WGU[sl], WGUb[sl]
                if half == 1 and fp == 0:
                    load_gu(0)
                    load_gu(1)
                for fi in range(2):
                    f = 2 * fp + fi
                    for c2 in range(2):
                        cs = slice(c2 * 512, (c2 + 1) * 512)
                        b0 = 2 * (it_ % 3)
                        k2 = it_ % 2
                        it_ += 1
                        S.group("pe", [(lambda e, kc=kc: e.matmul(ps[:, b0, :], lhsT=wg[:, kc, fi * 128:(fi + 1) * 128],
                                                                   rhs=h2T[:, kc, cs], start=(kc == 0), stop=(kc == 7)))
                                       for kc in range(8)], reads=[wgb, h2Tb], writes=[PB[b0]])
                        S.group("pe", [(lambda e, kc=kc: e.matmul(ps[:, b0 + 1, :],
                                                                   lhsT=wg[:, kc, 256 + fi * 128:256 + (fi + 1) * 128],
                                                                   rhs=h2T[:, kc, cs], start=(kc == 0), stop=(kc == 7)))
                                       for kc in range(8)], reads=[wgb, h2Tb], writes=[PB[b0 + 1]])
                        S.op("act", lambda e: e.activation(out=SG[k2][:], in_=ps[:, b0, :], func=AF.Silu),
                             reads=[PB[b0]], writes=[SGb[k2]])
                        S.op("dve", lambda e: e.tensor_tensor(out=actT[:, f, cs], in0=ps[:, b0 + 1, :], in1=SG[k2][:],
                                                              op=ALU.mult),
                             reads=[PB[b0 + 1], SGb[k2]], writes=[actTb])
                if fp + 2 < nfp:
                    load_gu(fp + 2)
            for t8 in range(8):
                tt = half * 8 + t8
                banks = (2 * (t8 % 2), 2 * (t8 % 2) + 1)
                for hf in range(2):
                    S.group("pe", [(lambda e, f=f: e.matmul(ps[:, banks[hf], :], lhsT=actT[:, f, t8 * 128:(t8 + 1) * 128],
                                                             rhs=Wd[:, f, hf * 512:(hf + 1) * 512],
                                                             start=(f == 0), stop=(f == NF - 1))) for f in range(NF)],
                            reads=[actTb, Wdb], writes=[PB[banks[hf]]])
                post_norm_resid(banks, gfpost, gfb, X1[:, t8, :], X1b[t8], t8, y[seq, tt * 128:(tt + 1) * 128, :])
        S.barrier()
    return nc


_NC_CACHE = {}


def kernel(x, norm_mix_pre_g, w_in, w_branch_a, w_branch_b, lam_q1, lam_k1, lam_q2, lam_k2,
           diff_subln_g, w_out, norm_mix_post_g, norm_ffn_pre_g, w_gate, w_up, w_down, norm_ffn_post_g):
    n = 8
    f32 = lambda a: np.ascontiguousarray(np.asarray(a, dtype=np.float32))
    x = f32(x)
    consts = _host_consts()
    shared = {
        "w_in": f32(w_in)[0], "w_branch_a": f32(w_branch_a)[0], "w_branch_b": f32(w_branch_b)[0],
        "w_out": f32(w_out)[0], "w_gate": f32(w_gate)[0], "w_up": f32(w_up)[0], "w_down": f32(w_down)[0],
        "norm_mix_pre_g": f32(norm_mix_pre_g), "norm_mix_post_g": f32(norm_mix_post_g),
        "norm_ffn_pre_g": f32(norm_ffn_pre_g), "norm_ffn_post_g": f32(norm_ffn_post_g),
        "lamv": np.concatenate([f32(lam_q1)[0], f32(lam_k1)[0], f32(lam_q2)[0], f32(lam_k2)[0]])[None, :],
        "diff_subln_g": f32(diff_subln_g)[0][:, None],
    }
    shared.update(consts)
    nc = build(2)
    in_maps = []
    for c in range(n):
        m = dict(shared)
        m["x"] = np.ascontiguousarray(x[2 * c:2 * c + 2])
        in_maps.append(m)
    res = run_bass_kernel_spmd(nc, in_maps, core_ids=list(range(n)))
    return np.concatenate([r["y"] for r in res.results], axis=0).astype(np.float32)
```

```python
import numpy as np
import ml_dtypes
import concourse.bass as bass
import concourse.mybir as mybir
from concourse.bass_utils import run_bass_kernel_spmd

F32 = mybir.dt.float32
BF16 = mybir.dt.bfloat16
AF = mybir.ActivationFunctionType
ALU = mybir.AluOpType
AX = mybir.AxisListType

T = 2048
D = 1024
DFF = 2816
NF = DFF // 128
NEGM = -30000.0


class Buf:
    __slots__ = ("name", "w", "r")

    def __init__(self, name):
        self.name = name
        self.w = None
        self.r = {}


class EngState:
    def __init__(self, name, eng, sem):
        self.name = name
        self.eng = eng
        self.sem = sem
        self.cnt = 0
        self.seen = {}


class Sync:
    def __init__(self, nc, ndma=20):
        self.nc = nc
        self.E = {}
        for name, e in (("pe", nc.tensor), ("act", nc.scalar), ("dve", nc.vector),
                        ("pool", nc.gpsimd), ("sp", nc.sync)):
            self.E[name] = EngState(name, e, nc.alloc_semaphore("s_" + name))
        self.dsem = {}
        self.dnext = {}
        for q in ("sp", "pool"):
            self.dsem[q] = [[nc.alloc_semaphore("d_%s%d" % (q, i)), 0] for i in range(ndma)]
            self.dnext[q] = 0
        self.semof = {}
        for name, st in self.E.items():
            self.semof[("c", name)] = st.sem
        for q in ("sp", "pool"):
            for i, (s, _) in enumerate(self.dsem[q]):
                self.semof[("d", q, i)] = s

    def _wait(self, E, deps):
        for key, n in deps.items():
            if key == ("c", "pe") and E.name == "pe":
                continue
            if E.seen.get(key, 0) >= n:
                continue
            E.eng.wait_ge(self.semof[key], n)
            E.seen[key] = n

    @staticmethod
    def _deps(reads, writes):
        deps = {}

        def add(key, n):
            if deps.get(key, 0) < n:
                deps[key] = n
        for b in reads:
            if b.w is not None:
                add(*b.w)
        for b in writes:
            if b.w is not None:
                add(*b.w)
            for key, n in b.r.items():
                add(key, n)
        return deps

    @staticmethod
    def _mark(ev, reads, writes):
        key, n = ev
        for b in reads:
            if b.r.get(key, 0) < n:
                b.r[key] = n
        for b in writes:
            b.w = ev
            b.r = {}

    def op(self, engname, fn, reads=(), writes=()):
        E = self.E[engname]
        self._wait(E, self._deps(reads, writes))
        ins = fn(E.eng)
        E.cnt += 1
        ins.then_inc(E.sem, 1)
        ev = (("c", engname), E.cnt)
        self._mark(ev, reads, writes)
        return ev

    def group(self, engname, fns, reads=(), writes=()):
        E = self.E[engname]
        self._wait(E, self._deps(reads, writes))
        ins = None
        for fn in fns:
            ins = fn(E.eng)
        E.cnt += 1
        ins.then_inc(E.sem, 1)
        ev = (("c", engname), E.cnt)
        self._mark(ev, reads, writes)
        return ev

    def split_group(self, engname, fns, reads, writes, per):
        E = self.E[engname]
        self._wait(E, self._deps(reads, writes))
        n = len(fns)
        for i, fn in enumerate(fns):
            ins = fn(E.eng)
            if i == n - 1:
                E.cnt += 1
                ins.then_inc(E.sem, 1)
                self._mark((("c", engname), E.cnt), reads, writes)
            elif (i + 1) % per == 0:
                yield

    def dma(self, q, out, in_, reads=(), writes=()):
        E = self.E[q]
        i = self.dnext[q]
        self.dnext[q] = (i + 1) % len(self.dsem[q])
        slot = self.dsem[q][i]
        key = ("d", q, i)
        deps = self._deps(reads, writes)
        if slot[1] > 0 and deps.get(key, 0) < slot[1]:
            deps[key] = slot[1]
        self._wait(E, deps)
        E.eng.dma_start(out=out, in_=in_).then_inc(slot[0], 16)
        slot[1] += 16
        ev = (key, slot[1])
        self._mark(ev, reads, writes)
        return ev

    def barrier(self, engines=("pe", "act", "dve", "pool", "sp")):
        deps = {}
        for name, st in self.E.items():
            if st.cnt > 0:
                deps[("c", name)] = st.cnt
        for q in ("sp", "pool"):
            for i, (s, n) in enumerate(self.dsem[q]):
                if n > 0:
                    deps[("d", q, i)] = n
        for name in engines:
            self._wait(self.E[name], deps)


class Arena:
    def __init__(self, nc, base, top):
        self.nc = nc
        self.ptr = (base + 31) // 32 * 32
        self.top = top
        self.n = 0

    def alloc(self, name, shape, dtype):
        nbytes = 2 if dtype == BF16 else 4
        sz = 1
        for s in shape[1:]:
            sz *= s
        sz *= nbytes
        off = self.ptr
        self.ptr = (off + sz + 31) // 32 * 32
        assert self.ptr <= self.top, ("SBUF overflow", name, self.ptr, self.top)
        self.n += 1
        return self.nc.alloc_sbuf_tensor_at("%s_%d" % (name, self.n), list(shape), dtype, offset=off)

    def mark(self):
        return self.ptr

    def reset(self, m):
        self.ptr = m


def _alibi_slopes():
    n = 12
    slopes = 2.0 ** (-8.0 * np.arange(1, n + 1) / n)
    diff_idx = np.arange(4) * 3 + 2
    moba_idx = np.setdiff1d(np.arange(n), diff_idx)
    return slopes[moba_idx].astype(np.float32), slopes[diff_idx].astype(np.float32)


def _host_consts():
    bf = ml_dtypes.bfloat16
    ms, ds = _alibi_slopes()
    slopes = np.concatenate([ms, ds]).astype(np.float32)
    pos = np.arange(T)
    hi = (256 * (pos // 256)).astype(np.float32)
    lo = (pos % 256).astype(np.float32)
    qx = np.zeros((12, 16, T), np.float32)
    kx = np.zeros((12, 16, T), np.float32)
    for h in range(12):
        c1 = np.float32(slopes[h]).astype(bf).astype(np.float32)
        c2 = np.float32(slopes[h] - c1).astype(bf).astype(np.float32)
        qx[h, 8], qx[h, 9], qx[h, 10], qx[h, 11] = hi, lo, hi, lo
        qx[h, 12], qx[h, 13], qx[h, 14], qx[h, 15] = c1, c1, c2, c2
        kx[h, 8], kx[h, 9], kx[h, 10], kx[h, 11] = -c1, -c1, -c2, -c2
        kx[h, 12], kx[h, 13], kx[h, 14], kx[h, 15] = hi, lo, hi, lo
        if h < 8:
            for n in range(8):
                kx[h, n] = (pos // 256 == n).astype(np.float32)
    ident = np.eye(128, dtype=np.float32)
    kk = np.arange(128)[:, None]
    qq = np.arange(128)[None, :]
    negtri = np.where(kk <= qq, 0.0, NEGM).astype(np.float32)
    pastmask = np.zeros((64, 8), np.float32)
    pairsum = np.zeros((64, 72), np.float32)
    for n in range(8):
        for m in range(8):
            p = n * 8 + m
            pairsum[p, 64 + n] = 1.0
            for qb in range(8):
                pastmask[p, qb] = 1.0 if m < qb else 0.0
    notown = np.zeros((128, 8), np.float32)
    for n in range(8):
        for qb in range(8):
            notown[64 + n, qb] = NEGM if n < qb else 0.0
    return {
        "c_qx": qx.astype(bf), "c_kx": kx.astype(bf), "c_ident": ident.astype(bf),
        "c_negtri": negtri.astype(bf), "c_pastmask": pastmask, "c_pairsum": pairsum.astype(bf),
        "c_notown": notown,
    }


def build(nseq=2, debug=False):
    nc = bass.Bass("TRN2", target_bir_lowering=False)
    S = Sync(nc)

    def din(name, shape, dt=F32):
        return nc.dram_tensor(name, list(shape), dt, kind="ExternalInput").ap()

    x = din("x", [nseq, T, D])
    w_in = din("w_in", [D, 5120])
    w_a = din("w_branch_a", [512, D])
    w_b = din("w_branch_b", [512, D])
    w_out = din("w_out", [D, D])
    w_gate = din("w_gate", [D, DFF])
    w_up = din("w_up", [D, DFF])
    w_down = din("w_down", [DFF, D])
    g_pre = din("norm_mix_pre_g", [1, D])
    g_post = din("norm_mix_post_g", [1, D])
    g_fpre = din("norm_ffn_pre_g", [1, D])
    g_fpost = din("norm_ffn_post_g", [1, D])
    lamv = din("lamv", [1, 256])
    g_sub = din("diff_subln_g", [128, 1])
    c_qx = din("c_qx", [12, 16, T], BF16)
    c_kx = din("c_kx", [12, 16, T], BF16)
    c_ident = din("c_ident", [128, 128], BF16)
    c_negtri = din("c_negtri", [128, 128], BF16)
    c_pastmask = din("c_pastmask", [64, 8])
    c_pairsum = din("c_pairsum", [64, 72], BF16)
    c_notown = din("c_notown", [128, 8])
    y = nc.dram_tensor("y", [nseq, T, D], F32, kind="ExternalOutput").ap()
    x1s = nc.dram_tensor("x1s", [nseq, T, D], F32).ap()
    dbg = {}
    if debug:
        dbg["hT"] = nc.dram_tensor("dbg_hT", [128, 8, T], BF16, kind="ExternalOutput").ap()
        dbg["yT"] = nc.dram_tensor("dbg_yT", [128, 8, T], BF16, kind="ExternalOutput").ap()
        dbg["mT"] = nc.dram_tensor("dbg_mT", [128, 8, T], BF16, kind="ExternalOutput").ap()

    w_in_r = w_in.rearrange("(kc p) n -> p kc n", p=128)
    w_in_r4 = w_in.rearrange("(kc p) (a n) -> p kc a n", p=128, n=128)
    w_a_r = w_a.rearrange("(kc p) n -> p kc n", p=128)
    w_b_r = w_b.rearrange("(kc p) n -> p kc n", p=128)
    w_out_r = w_out.rearrange("(kc p) n -> p kc n", p=128)
    w_gate_r = w_gate.rearrange("(kc p) n -> p kc n", p=128)
    w_up_r = w_up.rearrange("(kc p) n -> p kc n", p=128)
    w_down_r = w_down.rearrange("(f p) n -> p f n", p=128)

    ar = Arena(nc, nc.sbuf_base, nc.sbuf_top)
    ps = nc.alloc_psum_tensor("ps", [128, 8, 512], F32)
    PB = [Buf("bank%d" % i) for i in range(8)]

    ident = ar.alloc("ident", [128, 128], BF16)
    negtri = ar.alloc("negtri", [128, 128], BF16)
    ones_bf = ar.alloc("ones", [128, 128], BF16)
    pastmask = ar.alloc("pastmask", [64, 8], F32)
    pairsum = ar.alloc("pairsum", [64, 72], BF16)
    notown = ar.alloc("notown", [128, 8], F32)
    gsub = ar.alloc("gsub", [128, 1], F32)
    neglam = ar.alloc("neglam", [128, 1], F32)
    lamt = ar.alloc("lamt", [128, 256], F32)
    lamp = ar.alloc("lamp", [128, 128], F32)
    lams = ar.alloc("lams", [128, 4], F32)
    XIN = [ar.alloc("xin", [128, D], F32) for _ in range(2)]
    XINb = [Buf("xin%d" % i) for i in range(2)]
    HN = [ar.alloc("hn", [128, D], BF16) for _ in range(2)]
    HNb = [Buf("hn%d" % i) for i in range(2)]
    junk = ar.alloc("junk", [128, D], BF16)
    junkb = Buf("junk")
    SS = [ar.alloc("ss", [128, 4], F32) for _ in range(4)]
    SSb = [Buf("ss%d" % i) for i in range(4)]
    epsD = ar.alloc("epsD", [128, 1], F32)
    TST = [ar.alloc("tst", [128, D], F32) for _ in range(2)]
    TSTb = [Buf("tst%d" % i) for i in range(2)]
    cb = Buf("consts")

    S.dma("sp", ident[:], c_ident, writes=[cb])
    S.dma("sp", negtri[:], c_negtri, writes=[cb])
    S.dma("sp", pastmask[:], c_pastmask, writes=[cb])
    S.dma("sp", pairsum[:], c_pairsum, writes=[cb])
    S.dma("sp", notown[:], c_notown, writes=[cb])
    S.dma("sp", gsub[:], g_sub, writes=[cb])
    S.dma("sp", lamt[:], lamv.partition_broadcast(128), writes=[cb])
    S.op("dve", lambda e: e.memset(ones_bf[:], 1.0), writes=[cb])
    S.op("dve", lambda e: e.memset(epsD[:], 1e-6), writes=[cb])
    lamtv = lamt[:].rearrange("p (a b d) -> p a b d", a=2, b=2)
    S.op("dve", lambda e: e.tensor_tensor(out=lamp[:].rearrange("p (a d) -> p a d", a=2),
                                          in0=lamtv[:, :, 0, :], in1=lamtv[:, :, 1, :], op=ALU.mult),
         reads=[cb], writes=[cb])
    S.op("dve", lambda e: e.tensor_reduce(out=lams[:, 0:2], in_=lamp[:].rearrange("p (a d) -> p a d", a=2),
                                          axis=AX.X, op=ALU.add), reads=[cb], writes=[cb])
    S.op("act", lambda e: e.activation(out=lams[:, 2:4], in_=lams[:, 0:2], func=AF.Exp), reads=[cb], writes=[cb])
    S.op("dve", lambda e: e.scalar_tensor_tensor(out=neglam[:], in0=lams[:, 3:4], scalar=-0.2, in1=lams[:, 2:3],
                                                 op0=ALU.add, op1=ALU.subtract), reads=[cb], writes=[cb])
    S.op("dve", lambda e: e.tensor_scalar(out=gsub[:], in0=gsub[:], scalar1=0.8, scalar2=None, op0=ALU.mult),
         reads=[cb], writes=[cb])

    base_mark = ar.mark()

    NSS = 4

    def norm_A(src_ap, srcb, k):
        ss, ssb = SS[k % NSS], SSb[k % NSS]
        S.op("act", lambda e: e.memzero(ss[:]), writes=[ssb])
        S.op("act", lambda e: e.activation(out=junk[:], in_=src_ap, func=AF.Square, accum_out=ss[:, 0:1]),
             reads=srcb, writes=[junkb, ssb])
        S.op("act", lambda e: e.activation(out=ss[:, 1:2], in_=ss[:, 0:1], func=AF.Ln, scale=1.0 / D,
                                           bias=epsD[:, 0:1]), reads=[ssb, cb], writes=[ssb])
        S.op("act", lambda e: e.activation(out=ss[:, 1:2], in_=ss[:, 1:2], func=AF.Exp, scale=-0.5),
             reads=[ssb], writes=[ssb])

    def norm_B(src_ap, srcb, gt, gtb, k, bank):
        ss, ssb = SS[k % NSS], SSb[k % NSS]
        hn, hnb = HN[k % 2], HNb[k % 2]
        S.op("dve", lambda e: e.scalar_tensor_tensor(out=hn[:], in0=src_ap, scalar=ss[:, 1:2], in1=gt[:],
                                                     op0=ALU.mult, op1=ALU.mult),
             reads=list(srcb) + [ssb, gtb], writes=[hnb])
        psb = ps[:, bank, :].bitcast(BF16)
        S.group("pe", [(lambda e, kc=kc: e.transpose(out=psb[:, kc * 128:(kc + 1) * 128],
                                                      in_=hn[:, kc * 128:(kc + 1) * 128], identity=ident[:]))
                       for kc in range(8)], reads=[hnb, cb], writes=[PB[bank]])

    def norm_C(dstT, dstb, tcol, bank):
        psb = ps[:, bank, :].bitcast(BF16)
        S.op("act", lambda e: e.activation(out=dstT[:, :, tcol:tcol + 128],
                                           in_=psb.rearrange("p (k t) -> p k t", k=8), func=AF.Copy),
             reads=[PB[bank]], writes=[dstb])

    def norm_transpose_pipeline(n, src_of, gt, gtb, dstT, dstb, banks):
        srcs = {}

        def A(k):
            srcs[k] = src_of(k)
            norm_A(srcs[k][0], srcs[k][1], k)

        def B(k):
            norm_B(srcs[k][0], srcs[k][1], gt, gtb, k, banks[k % len(banks)])

        def C(k):
            norm_C(dstT, dstb, k * 128, banks[k % len(banks)])
        A(0)
        for k in range(n):
            if k + 1 < n:
                A(k + 1)
            B(k)
            if k >= 1:
                C(k - 1)
        C(n - 1)

    def post_A(banks, k):
        ss, ssb = SS[k % NSS], SSb[k % NSS]
        S.op("act", lambda e: e.memzero(ss[:]), writes=[ssb])
        S.op("act", lambda e: e.activation(out=junk[:], in_=ps[:, banks[0]:banks[0] + 2, :].rearrange("p b n -> p (b n)"),
                                           func=AF.Square, accum_out=ss[:, 0:1]),
             reads=[PB[banks[0]], PB[banks[1]]], writes=[junkb, ssb])
        S.op("act", lambda e: e.activation(out=ss[:, 1:2], in_=ss[:, 0:1], func=AF.Ln, scale=1.0 / D,
                                           bias=epsD[:, 0:1]), reads=[ssb, cb], writes=[ssb])
        S.op("act", lambda e: e.activation(out=ss[:, 1:2], in_=ss[:, 1:2], func=AF.Exp, scale=-0.5),
             reads=[ssb], writes=[ssb])

    def post_B(banks, gt, gtb, resid_ap, residb, k, out_dram):
        ss, ssb = SS[k % NSS], SSb[k % NSS]
        tst, tstb = TST[k % 2], TSTb[k % 2]
        for hf in range(2):
            S.op("dve", lambda e, hf=hf: e.scalar_tensor_tensor(
                out=tst[:, hf * 512:(hf + 1) * 512], in0=ps[:, banks[hf], :], scalar=ss[:, 1:2],
                in1=gt[:, hf * 512:(hf + 1) * 512], op0=ALU.mult, op1=ALU.mult),
                reads=[PB[banks[hf]], ssb, gtb], writes=[tstb])
        S.op("dve", lambda e: e.tensor_tensor(out=tst[:], in0=tst[:], in1=resid_ap, op=ALU.add),
             reads=[tstb] + list(residb), writes=[tstb])
        S.dma("sp", out_dram, tst[:], reads=[tstb])

    for seq in range(nseq):
        ar.reset(base_mark)
        hT = ar.alloc("hT", [128, 8, T], BF16)
        hTb = Buf("hT")
        yT = ar.alloc("yT", [128, 8, T], BF16)
        yTb = Buf("yT")
        a_mark = ar.mark()
        gpre = ar.alloc("gpre", [128, D], F32)
        gpreb = Buf("gpre")
        QK = [[ar.alloc("qk", [128, T], BF16) for _ in range(4)] for _ in range(2)]
        QKb = [[Buf("qk") for _ in range(4)] for _ in range(2)]
        VV = [ar.alloc("vv", [128, 16, 320], BF16) for _ in range(2)]
        VVb = [Buf("vv") for _ in range(2)]
        WSL = [ar.alloc("wsl", [128, 8, 384], BF16) for _ in range(2)]
        WSLb = [Buf("wsl") for _ in range(2)]
        NPB = 5
        PBF = [ar.alloc("pbf", [128, 512], BF16) for _ in range(NPB)]
        PBFb = [Buf("pbf") for _ in range(NPB)]
        ksum = [ar.alloc("ksum", [64, 8], F32) for _ in range(2)]
        diffw = [ar.alloc("diffw", [64, 64], BF16) for _ in range(2)]
        ind = [ar.alloc("ind", [64, 512], BF16) for _ in range(2)]
        gateb = [Buf("gate") for _ in range(2)]
        indb = [Buf("ind") for _ in range(2)]
        rscr = [ar.alloc("rscr", [128, 512], F32) for _ in range(2)]
        rbuf = [ar.alloc("rbuf", [128, 512], F32) for _ in range(2)]
        rbufb = [Buf("rbuf") for _ in range(2)]
        dL1 = [ar.alloc("dL1", [128, 512], F32) for _ in range(2)]
        dL2 = [ar.alloc("dL2", [128, 512], F32) for _ in range(2)]
        dO1 = [ar.alloc("dO1", [128, 512], F32) for _ in range(2)]
        dO2 = [ar.alloc("dO2", [128, 512], F32) for _ in range(2)]
        dSQ = [ar.alloc("dSQ", [128, 512], BF16) for _ in range(2)]
        dRS = [ar.alloc("dRS", [128, 512], F32) for _ in range(2)]
        dfb = [Buf("dfin") for _ in range(2)]

        S.dma("sp", gpre[:], g_pre.partition_broadcast(128), writes=[gpreb])
        for sl in range(2):
            for i in range(4):
                S.op("dve", lambda e, sl=sl, i=i: e.memset(QK[sl][i][:], 0.0), writes=[QKb[sl][i]])
            S.op("dve", lambda e, sl=sl: e.memset(VV[sl][:], 1.0), writes=[VVb[sl]])

        def p1_src(tt):
            xin, xinb = XIN[tt % 2], XINb[tt % 2]
            S.dma("sp", xin[:], x[seq, tt * 128:(tt + 1) * 128, :], writes=[xinb])
            return xin[:], [xinb]
        norm_transpose_pipeline(16, p1_src, gpre, gpreb, hT, hTb, (6, 7))
        if debug and seq == 0:
            S.dma("sp", dbg["hT"], hT[:], reads=[hTb])

        def load_group_weights(g):
            sl = g % 2
            a0 = g if g < 4 else 12 + (g - 4)
            for i3 in range(3):
                S.dma("pool", WSL[sl][:, :, i3 * 128:(i3 + 1) * 128], w_in_r4[:, :, a0 + 4 * i3, :],
                      writes=[WSLb[sl]])
            hA, hB = (2 * g, 2 * g + 1) if g < 4 else (8 + g - 4, 8 + g - 4)
            q = QK[sl]
            qb_ = QKb[sl]
            S.dma("sp", q[0][64:80, :], c_qx[hA], writes=[qb_[0]])
            S.dma("sp", q[1][64:80, :], c_qx[hB], writes=[qb_[1]])
            S.dma("sp", q[2][64:80, :], c_kx[hA], writes=[qb_[2]])
            S.dma("sp", q[3][64:80, :], c_kx[hB], writes=[qb_[3]])

        pbk = {"i": 0}

        pbk["set"] = (7, 5, 6)

        def pbank():
            pbk["i"] += 1
            st = pbk["set"]
            return st[pbk["i"] % len(st)]

        def prep_units(g):
            sl = g % 2
            q, qb_ = QK[sl], QKb[sl]
            w, wb = WSL[sl], WSLb[sl]
            load_group_weights(g)
            yield
            for c in range(4):
                cs = slice(c * 512, (c + 1) * 512)
                bank = pbank()
                yield from S.split_group(
                    "pe", [(lambda e, kc=kc: e.matmul(ps[:, bank, :], lhsT=w[:, kc, 0:128], rhs=hT[:, kc, cs],
                                                      start=(kc == 0), stop=(kc == 7))) for kc in range(8)],
                    [wb, hTb], [PB[bank]], 2)
                S.op("dve", lambda e: e.tensor_scalar(out=q[0][0:64, cs], in0=ps[0:64, bank, :], scalar1=0.125,
                                                      scalar2=None, op0=ALU.mult),
                     reads=[PB[bank]], writes=[qb_[0]])
                S.op("dve", lambda e: e.tensor_scalar(out=q[1][0:64, cs], in0=ps[64:128, bank, :], scalar1=0.125,
                                                      scalar2=None, op0=ALU.mult),
                     reads=[PB[bank]], writes=[qb_[1]])
                yield
                bank = pbank()
                yield from S.split_group(
                    "pe", [(lambda e, kc=kc: e.matmul(ps[:, bank, :], lhsT=w[:, kc, 128:256], rhs=hT[:, kc, cs],
                                                      start=(kc == 0), stop=(kc == 7))) for kc in range(8)],
                    [wb, hTb], [PB[bank]], 2)
                S.op("dve", lambda e: e.tensor_copy(out=q[2][0:64, cs], in_=ps[0:64, bank, :]),
                     reads=[PB[bank]], writes=[qb_[2]])
                S.op("dve", lambda e: e.tensor_copy(out=q[3][0:64, cs], in_=ps[64:128, bank, :]),
                     reads=[PB[bank]], writes=[qb_[3]])
                yield
            vv, vvb = VV[sl], VVb[sl]
            for tg in range(4):
                bank = pbank()
                fns = []
                for i in range(4):
                    tt = 4 * tg + i
                    for kc in range(8):
                        fns.append(lambda e, i=i, tt=tt, kc=kc: e.matmul(
                            ps[:, bank, i * 128:(i + 1) * 128], lhsT=hT[:, kc, tt * 128:(tt + 1) * 128],
                            rhs=w[:, kc, 256:384], start=(kc == 0), stop=(kc == 7)))
                yield from S.split_group("pe", fns, [wb, hTb], [PB[bank]], 4)
                if g < 4:
                    vq = vv[:].rearrange("p t (a d) -> p t a d", d=64)
                    S.op("dve", lambda e: e.tensor_copy(
                        out=vq[:, 4 * tg:4 * tg + 4, 0:3:2, :],
                        in_=ps[:, bank, :].rearrange("p (t h d) -> p t h d", t=4, h=2)),
                        reads=[PB[bank]], writes=[vvb])
                else:
                    S.op("dve", lambda e: e.tensor_copy(
                        out=vv[:, 4 * tg:4 * tg + 4, 192:320],
                        in_=ps[:, bank, :].rearrange("p (t d) -> p t d", t=4)),
                        reads=[PB[bank]], writes=[vvb])
                yield
            if g < 4:
                for hx in range(2):
                    for _ in moba_gate(g, hx):
                        yield

        def moba_gate(g, hx):
            sl = g % 2
            qt, qtb = QK[sl][hx], QKb[sl][hx]
            kt_, ktb = QK[sl][2 + hx], QKb[sl][2 + hx]
            ks, dw, gb_ = ksum[hx], diffw[hx], gateb[hx]
            S.op("dve", lambda e: e.tensor_reduce(out=ks[:], in_=kt_[0:64, :].rearrange("p (n k) -> p n k", k=256),
                                                  axis=AX.X, op=ALU.add), reads=[ktb], writes=[gb_])
            for n in range(8):
                S.op("dve", lambda e, n=n: e.tensor_scalar(out=dw[:, n * 8:(n + 1) * 8], in0=ks[:, 0:8],
                                                           scalar1=ks[:, n:n + 1], scalar2=None,
                                                           op0=ALU.subtract), reads=[gb_], writes=[gb_])
            yield
            for c in (2, 3):
                cs = slice(c * 512, (c + 1) * 512)
                bank = pbank()
                S.group("pe", [lambda e: e.matmul(ps[0:64, bank, :], lhsT=dw[:, :], rhs=qt[0:64, cs],
                                                  start=True, stop=True)],
                        reads=[gb_, qtb], writes=[PB[bank]])
                for hh in range(2):
                    qblk = 2 * c + hh
                    hs = slice(hh * 256, (hh + 1) * 256)
                    S.op("dve", lambda e, hs=hs, qblk=qblk: e.tensor_scalar(
                        out=ind[hx][:, hs], in0=ps[0:64, bank, hs], scalar1=0.0,
                        scalar2=pastmask[:, qblk:qblk + 1], op0=ALU.is_gt, op1=ALU.mult),
                        reads=[PB[bank], cb], writes=[indb[hx]])
                yield
                bank = pbank()
                S.group("pe", [lambda e: e.matmul(ps[0:72, bank, :], lhsT=pairsum[:, :], rhs=ind[hx][:, :],
                                                  start=True, stop=True)],
                        reads=[indb[hx], cb], writes=[PB[bank]])
                for hh in range(2):
                    qblk = 2 * c + hh
                    hs = slice(hh * 256, (hh + 1) * 256)
                    S.op("dve", lambda e, hs=hs, qblk=qblk: e.tensor_scalar(
                        out=qt[64:72, c * 512 + hh * 256:c * 512 + (hh + 1) * 256], in0=ps[64:72, bank, hs],
                        scalar1=2.5, scalar2=notown[64:72, qblk:qblk + 1], op0=ALU.is_gt, op1=ALU.mult),
                        reads=[PB[bank], cb], writes=[qtb])
                yield

        state = {"s": 0, "p": 0, "la": 2, "sb": (0, 1, 2)}
        pending = []

        def defer(n, fn):
            pending.append([n, fn])

        def run_pending(flush=False):
            while True:
                ready = None
                for ent in pending:
                    if flush or ent[0] <= 0:
                        ready = ent
                        break
                if ready is None:
                    break
                pending.remove(ready)
                ready[1]()
            for ent in pending:
                ent[0] -= 1

        def attention(items, finalize, filler, every):
            n = len(items)
            sb_of = {}

            def qk(i):
                it = items[i]
                sbank = state["sb"][state["s"] % len(state["sb"])]
                state["s"] += 1
                sb_of[i] = sbank
                c, kt = it["c"], it["kt"]
                j = kt - 4 * c
                col0 = 128 * j if j > 0 else 0
                it["col0"] = col0
                fns = [lambda e: e.matmul(ps[:, sbank, col0:512], lhsT=it["k"][0:80, kt * 128:(kt + 1) * 128],
                                          rhs=it["q"][0:80, c * 512 + col0:(c + 1) * 512],
                                          start=True, stop=(j < 0))]
                if j >= 0:
                    fns.append(lambda e: e.matmul(ps[:, sbank, col0:col0 + 128], lhsT=ident[:], rhs=negtri[:],
                                                  start=False, stop=True))
                S.group("pe", fns, reads=[it["kb"], it["qb"], cb], writes=[PB[sbank]])

            def ex_pv(i):
                it = items[i]
                sbank = sb_of[i]
                pi = state["p"] % NPB
                state["p"] += 1
                col0 = it["col0"]
                S.op("act", lambda e: e.activation(out=PBF[pi][:, col0:512], in_=ps[:, sbank, col0:512], func=AF.Exp),
                     reads=[PB[sbank]], writes=[PBFb[pi]])
                fns = []
                banks = []
                rd = [PBFb[pi]]
                for (bank, lap, lbuf) in it["pv"]:
                    fns.append(lambda e, bank=bank, lap=lap: e.matmul(
                        ps[:, bank, col0:512], lhsT=lap, rhs=PBF[pi][:, col0:512],
                        start=(it["kt"] == 0), stop=it["last"]))
                    banks.append(PB[bank])
                    rd.append(lbuf)
                S.group("pe", fns, reads=rd, writes=banks)
                if it["last"]:
                    finalize(it)

            la = state["la"]
            for i in range(min(la, n)):
                qk(i)
            for i in range(n):
                if i + la < n:
                    qk(i + la)
                ex_pv(i)
                run_pending()
                if filler is not None and i % every == every - 1:
                    next(filler, None)

        def recip(out_ap, in_ap, scr_ap, reads, wb_, on_act=False):
            if on_act:
                S.op("act", lambda e: e.activation(out=scr_ap, in_=in_ap, func=AF.Ln), reads=reads, writes=[wb_])
                S.op("act", lambda e: e.activation(out=out_ap, in_=scr_ap, func=AF.Exp, scale=-1.0),
                     reads=[wb_], writes=[wb_])
            else:
                S.op("dve", lambda e: e.reciprocal(out=out_ap, in_=in_ap), reads=reads, writes=[wb_])

        def run_moba(g, filler):
            sl = g % 2
            for hx in range(2):
                items = []
                for c in range(4):
                    nk = 4 * c + 4
                    bank = 3 + ((hx * 4 + c) % 2)
                    lap_of = (lambda kt: VV[sl][:, kt, 0:128]) if hx == 0 else (lambda kt: VV[sl][:, kt, 64:192])
                    for kt in range(nk):
                        items.append(dict(q=QK[sl][hx], qb=QKb[sl][hx], k=QK[sl][2 + hx], kb=QKb[sl][2 + hx],
                                          c=c, kt=kt, pv=[(bank, lap_of(kt), VVb[sl])], last=(kt == nk - 1),
                                          bank=bank))

                def fin(it, hx=hx):
                    bank, c = it["bank"], it["c"]
                    cs = slice(c * 512, (c + 1) * 512)
                    rb, rbb = rbuf[c % 2], rbufb[c % 2]
                    rs_ = rscr[c % 2]
                    if hx == 0:
                        recip(rb[0:64, :], ps[64:128, bank, :], rs_[0:64, :], [PB[bank]], rbb, on_act=True)
                        S.op("dve", lambda e: e.tensor_tensor(out=yT[0:64, g, cs], in0=ps[0:64, bank, :],
                                                              in1=rb[0:64, :], op=ALU.mult),
                             reads=[PB[bank], rbb], writes=[yTb])
                    else:
                        recip(rb[64:128, :], ps[0:64, bank, :], rs_[64:128, :], [PB[bank]], rbb, on_act=True)
                        S.op("dve", lambda e: e.tensor_tensor(out=yT[64:128, g, cs], in0=ps[64:128, bank, :],
                                                              in1=rb[64:128, :], op=ALU.mult),
                             reads=[PB[bank], rbb], writes=[yTb])
                attention(items, fin, filler, 1)

        def run_diff(g, filler):
            sl = g % 2
            j = g - 4
            items = []
            for c in range(4):
                nk = 4 * c + 4
                for kt in range(nk):
                    for mp in range(2):
                        items.append(dict(q=QK[sl][mp], qb=QKb[sl][mp], k=QK[sl][2 + mp], kb=QKb[sl][2 + mp],
                                          c=c, kt=kt,
                                          pv=[(3 + mp, VV[sl][:, kt, 192:320], VVb[sl]),
                                              (5 + mp, ones_bf[:], cb)],
                                          last=(kt == nk - 1), mp=mp))

            def fin(it):
                if it["mp"] == 0:
                    return
                run_pending(flush=True)
                c = it["c"]
                cs = slice(c * 512, (c + 1) * 512)
                k2 = c % 2
                L1, L2, O1, O2, SQ, RS, fb = dL1[k2], dL2[k2], dO1[k2], dO2[k2], dSQ[k2], dRS[k2], dfb[k2]
                S.op("act", lambda e: e.activation(out=L1[:], in_=ps[:, 5, :], func=AF.Ln), reads=[PB[5]], writes=[fb])
                S.op("dve", lambda e: e.tensor_copy(out=O1[:], in_=ps[:, 3, :]), reads=[PB[3]], writes=[fb])
                S.op("act", lambda e: e.activation(out=L2[:], in_=ps[:, 6, :], func=AF.Ln), reads=[PB[6]], writes=[fb])
                S.op("dve", lambda e: e.tensor_copy(out=O2[:], in_=ps[:, 4, :]), reads=[PB[4]], writes=[fb])

                def stage_b():
                    S.op("act", lambda e: e.activation(out=L1[:], in_=L1[:], func=AF.Exp, scale=-1.0),
                         reads=[fb], writes=[fb])
                    S.op("act", lambda e: e.activation(out=L2[:], in_=L2[:], func=AF.Exp, scale=-1.0),
                         reads=[fb], writes=[fb])
                    S.op("dve", lambda e: e.tensor_tensor(out=O1[:], in0=O1[:], in1=L1[:], op=ALU.mult),
                         reads=[fb], writes=[fb])
                    S.op("dve", lambda e: e.tensor_tensor(out=O2[:], in0=O2[:], in1=L2[:], op=ALU.mult),
                         reads=[fb], writes=[fb])
                    S.op("dve", lambda e: e.scalar_tensor_tensor(out=O1[:], in0=O2[:], scalar=neglam[:, 0:1],
                                                                 in1=O1[:], op0=ALU.mult, op1=ALU.add),
                         reads=[fb, cb], writes=[fb])
                    S.op("act", lambda e: e.activation(out=SQ[:], in_=O1[:], func=AF.Square), reads=[fb], writes=[fb])
                    defer(2, stage_c)

                def stage_c():
                    bank = pbank()
                    S.group("pe", [lambda e: e.matmul(ps[:, bank, :], lhsT=ones_bf[:], rhs=SQ[:], start=True,
                                                      stop=True)], reads=[fb, cb], writes=[PB[bank]])
                    S.op("dve", lambda e: e.tensor_scalar(out=RS[:], in0=ps[:, bank, :], scalar1=1.0 / 128,
                                                          scalar2=1e-5, op0=ALU.mult, op1=ALU.add),
                         reads=[PB[bank]], writes=[fb])
                    S.op("act", lambda e: e.activation(out=RS[:], in_=RS[:], func=AF.Ln), reads=[fb], writes=[fb])
                    S.op("act", lambda e: e.activation(out=RS[:], in_=RS[:], func=AF.Exp, scale=-0.5),
                         reads=[fb], writes=[fb])
                    S.op("dve", lambda e: e.scalar_tensor_tensor(out=yT[:, 4 + j, cs], in0=O1[:], scalar=gsub[:, 0:1],
                                                                 in1=RS[:], op0=ALU.mult, op1=ALU.mult),
                         reads=[fb, cb], writes=[yTb])
                defer(2, stage_b)
            attention(items, fin, filler, 2)

        for _ in prep_units(0):
            pass
        for g in range(8):
            filler = prep_units(g + 1) if g + 1 < 8 else None
            if g < 4:
                pbk["set"] = (7, 5, 6)
                state["la"], state["sb"] = 2, (0, 1, 2)
                run_moba(g, filler)
            else:
                pbk["set"] = (2, 7)
                state["la"], state["sb"] = 1, (0, 1)
                run_diff(g, filler)
            if filler is not None:
                for _ in filler:
                    pass
            run_pending(flush=True)
        if debug and seq == 0:
            S.dma("sp", dbg["yT"], yT[:], reads=[yTb])

        S.barrier()
        ar.reset(a_mark)
        mT = ar.alloc("mT", [128, 8, T], BF16)
        mTb = Buf("mT")
        woT = ar.alloc("woT", [128, 8, D], BF16)
        woTb = Buf("woT")
        WM = [ar.alloc("wm", [128, 24, 128], BF16) for _ in range(2)]
        WMb = [Buf("wm") for _ in range(2)]
        gpost = ar.alloc("gpost", [128, D], F32)
        gpostb = Buf("gpost")
        SA = [ar.alloc("sa", [128, 512], F32) for _ in range(2)]
        SB_ = [ar.alloc("sb", [128, 512], F32) for _ in range(2)]
        TA = [ar.alloc("ta", [128, 512], F32) for _ in range(2)]
        TB = [ar.alloc("tb", [128, 512], F32) for _ in range(2)]
        MSb = [Buf("ms") for _ in range(2)]

        def load_merge_weights(j):
            sl = j % 2
            js = slice(128 * j, 128 * j + 128)
            S.dma("pool", WM[sl][:, 0:4, :], w_a_r[:, :, js], writes=[WMb[sl]])
            S.dma("pool", WM[sl][:, 4:8, :], w_b_r[:, :, js], writes=[WMb[sl]])
            S.dma("pool", WM[sl][:, 8:16, :], w_in_r[:, :, 3072 + 128 * j:3072 + 128 * j + 128], writes=[WMb[sl]])
            S.dma("pool", WM[sl][:, 16:24, :], w_in_r[:, :, 4096 + 128 * j:4096 + 128 * j + 128], writes=[WMb[sl]])

        S.dma("sp", gpost[:], g_post.partition_broadcast(128), writes=[gpostb])
        load_merge_weights(0)
        load_merge_weights(1)
        for hf in range(2):
            S.dma("pool", woT[:, 4 * hf:4 * hf + 4, :], w_out_r[:, 4 * hf:4 * hf + 4, :], writes=[woTb])
        it_ = 0
        for j in range(8):
            sl = j % 2
            wm, wmb = WM[sl], WMb[sl]
            for c in range(4):
                cs = slice(c * 512, (c + 1) * 512)
                b0 = 4 * (it_ % 2)
                k2 = it_ % 2
                it_ += 1
                S.group("pe", [(lambda e, kc=kc: e.matmul(ps[:, b0, :], lhsT=wm[:, kc, :], rhs=yT[:, kc, cs],
                                                           start=(kc == 0), stop=(kc == 3))) for kc in range(4)],
                        reads=[wmb, yTb], writes=[PB[b0]])
                S.group("pe", [(lambda e, kc=kc: e.matmul(ps[:, b0 + 1, :], lhsT=wm[:, 4 + kc, :],
                                                           rhs=yT[:, 4 + kc, cs],
                                                           start=(kc == 0), stop=(kc == 3))) for kc in range(4)],
                        reads=[wmb, yTb], writes=[PB[b0 + 1]])
                S.group("pe", [(lambda e, kc=kc: e.matmul(ps[:, b0 + 2, :], lhsT=wm[:, 8 + kc, :], rhs=hT[:, kc, cs],
                                                           start=(kc == 0), stop=(kc == 7))) for kc in range(8)],
                        reads=[wmb, hTb], writes=[PB[b0 + 2]])
                S.group("pe", [(lambda e, kc=kc: e.matmul(ps[:, b0 + 3, :], lhsT=wm[:, 16 + kc, :], rhs=hT[:, kc, cs],
                                                           start=(kc == 0), stop=(kc == 7))) for kc in range(8)],
                        reads=[wmb, hTb], writes=[PB[b0 + 3]])
                S.op("act", lambda e: e.activation(out=SA[k2][:], in_=ps[:, b0 + 2, :], func=AF.Sigmoid),
                     reads=[PB[b0 + 2]], writes=[MSb[k2]])
                S.op("act", lambda e: e.activation(out=SB_[k2][:], in_=ps[:, b0 + 3, :], func=AF.Sigmoid),
                     reads=[PB[b0 + 3]], writes=[MSb[k2]])
                S.op("dve", lambda e: e.tensor_tensor(out=TA[k2][:], in0=ps[:, b0, :], in1=SA[k2][:], op=ALU.mult),
                     reads=[PB[b0], MSb[k2]], writes=[MSb[k2]])
                S.op("dve", lambda e: e.tensor_tensor(out=TB[k2][:], in0=ps[:, b0 + 1, :], in1=SB_[k2][:],
                                                      op=ALU.mult),
                     reads=[PB[b0 + 1], MSb[k2]], writes=[MSb[k2]])
                S.op("dve", lambda e: e.tensor_tensor(out=mT[:, j, cs], in0=TA[k2][:], in1=TB[k2][:], op=ALU.add),
                     reads=[MSb[k2]], writes=[mTb])
            if j + 2 < 8:
                load_merge_weights(j + 2)
        if debug and seq == 0:
            S.dma("sp", dbg["mT"], mT[:], reads=[mTb])
        for tt in range(16):
            ts_ = slice(tt * 128, (tt + 1) * 128)
            banks = (2 * (tt % 2), 2 * (tt % 2) + 1)
            xin, xinb = XIN[tt % 2], XINb[tt % 2]
            S.dma("sp", xin[:], x[seq, ts_, :], writes=[xinb])
            for hf in range(2):
                S.group("pe", [(lambda e, kc=kc: e.matmul(ps[:, banks[hf], :], lhsT=mT[:, kc, ts_],
                                                           rhs=woT[:, kc, hf * 512:(hf + 1) * 512],
                                                           start=(kc == 0), stop=(kc == 7))) for kc in range(8)],
                        reads=[mTb, woTb], writes=[PB[banks[hf]]])
            post_A(banks, tt)
            post_B(banks, gpost, gpostb, xin[:], [xinb], tt, x1s[seq, ts_, :])

        S.barrier()
        ar.reset(base_mark)
        X1 = ar.alloc("X1", [128, 8, D], F32)
        X1b = [Buf("x1_%d" % i) for i in range(8)]
        h2T = ar.alloc("h2T", [128, 8, 1024], BF16)
        h2Tb = Buf("h2T")
        actT = ar.alloc("actT", [128, NF, 1024], BF16)
        actTb = Buf("actT")
        Wd = ar.alloc("Wd", [128, NF, D], BF16)
        Wdb = Buf("Wd")
        WGU = [ar.alloc("wgu", [128, 8, 512], BF16) for _ in range(2)]
        WGUb = [Buf("wgu") for _ in range(2)]
        gfpre = ar.alloc("gfpre", [128, D], F32)
        gfpost = ar.alloc("gfpost", [128, D], F32)
        gfb = Buf("gf")
        SG = [ar.alloc("sg", [128, 512], F32) for _ in range(2)]
        SGb = [Buf("sg") for _ in range(2)]

        S.dma("sp", gfpre[:], g_fpre.partition_broadcast(128), writes=[gfb])
        S.dma("sp", gfpost[:], g_fpost.partition_broadcast(128), writes=[gfb])

        def load_gu(fp):
            sl = fp % 2
            S.dma("pool", WGU[sl][:, :, 0:256], w_gate_r[:, :, 256 * fp:256 * fp + 256], writes=[WGUb[sl]])
            S.dma("pool", WGU[sl][:, :, 256:512], w_up_r[:, :, 256 * fp:256 * fp + 256], writes=[WGUb[sl]])

        nfp = NF // 2
        gu_loaded = [0]
        for half in range(2):
            if half == 0:
                load_gu(0)
                load_gu(1)
                for f2 in range(nfp):
                    S.dma("pool", Wd[:, 2 * f2:2 * f2 + 2, :], w_down_r[:, 2 * f2:2 * f2 + 2, :], writes=[Wdb])
            def p4_src(t8, half=half):
                tt = half * 8 + t8
                S.dma("sp", X1[:, t8, :], x1s[seq, tt * 128:(tt + 1) * 128, :], writes=[X1b[t8]])
                return X1[:, t8, :], [X1b[t8]]
            norm_transpose_pipeline(8, p4_src, gfpre, gfb, h2T, h2Tb, (6, 7))
            it_ = 0
            for fp in range(nfp):
                sl = fp % 2
                wg, wgb = WGU[sl], WGUb[sl]
                if half == 1 and fp == 0:
                    load_gu(0)
                    load_gu(1)
                for fi in range(2):
                    f = 2 * fp + fi
                    for c2 in range(2):
                        cs = slice(c2 * 512, (c2 + 1) * 512)
                        b0 = 2 * (it_ % 3)
                        k2 = it_ % 2
                        it_ += 1
                        S.group("pe", [(lambda e, kc=kc: e.matmul(ps[:, b0, :], lhsT=wg[:, kc, fi * 128:(fi + 1) * 128],
                                                                   rhs=h2T[:, kc, cs], start=(kc == 0), stop=(kc == 7)))
                                       for kc in range(8)], reads=[wgb, h2Tb], writes=[PB[b0]])
                        S.group("pe", [(lambda e, kc=kc: e.matmul(ps[:, b0 + 1, :],
                                                                   lhsT=wg[:, kc, 256 + fi * 128:256 + (fi + 1) * 128],
                                                                   rhs=h2T[:, kc, cs], start=(kc == 0), stop=(kc == 7)))
                                       for kc in range(8)], reads=[wgb, h2Tb], writes=[PB[b0 + 1]])
                        S.op("act", lambda e: e.activation(out=SG[k2][:], in_=ps[:, b0, :], func=AF.Silu),
                             reads=[PB[b0]], writes=[SGb[k2]])
                        S.op("dve", lambda e: e.tensor_tensor(out=actT[:, f, cs], in0=ps[:, b0 + 1, :], in1=SG[k2][:],
                                                              op=ALU.mult),
                             reads=[PB[b0 + 1], SGb[k2]], writes=[actTb])
                if fp + 2 < nfp:
                    load_gu(fp + 2)
            for t8 in range(8):
                tt = half * 8 + t8
                banks = (2 * (t8 % 2), 2 * (t8 % 2) + 1)
                for hf in range(2):
                    S.group("pe", [(lambda e, f=f: e.matmul(ps[:, banks[hf], :], lhsT=actT[:, f, t8 * 128:(t8 + 1) * 128],
                                                             rhs=Wd[:, f, hf * 512:(hf + 1) * 512],
                                                             start=(f == 0), stop=(f == NF - 1))) for f in range(NF)],
                            reads=[actTb, Wdb], writes=[PB[banks[hf]]])
                post_A(banks, t8)
                post_B(banks, gfpost, gfb, X1[:, t8, :], [X1b[t8]], t8, y[seq, tt * 128:(tt + 1) * 128, :])
        S.barrier()
    return nc


_NC_CACHE = {}


def kernel(x, norm_mix_pre_g, w_in, w_branch_a, w_branch_b, lam_q1, lam_k1, lam_q2, lam_k2,
           diff_subln_g, w_out, norm_mix_post_g, norm_ffn_pre_g, w_gate, w_up, w_down, norm_ffn_post_g):
    n = 8
    f32 = lambda a: np.ascontiguousarray(np.asarray(a, dtype=np.float32))
    x = f32(x)
    consts = _host_consts()
    shared = {
        "w_in": f32(w_in)[0], "w_branch_a": f32(w_branch_a)[0], "w_branch_b": f32(w_branch_b)[0],
        "w_out": f32(w_out)[0], "w_gate": f32(w_gate)[0], "w_up": f32(w_up)[0], "w_down": f32(w_down)[0],
        "norm_mix_pre_g": f32(norm_mix_pre_g), "norm_mix_post_g": f32(norm_mix_post_g),
        "norm_ffn_pre_g": f32(norm_ffn_pre_g), "norm_ffn_post_g": f32(norm_ffn_post_g),
        "lamv": np.concatenate([f32(lam_q1)[0], f32(lam_k1)[0], f32(lam_q2)[0], f32(lam_k2)[0]])[None, :],
        "diff_subln_g": f32(diff_subln_g)[0][:, None],
    }
    shared.update(consts)
    nc = build(2)
    in_maps = []
    for c in range(n):
        m = dict(shared)
        m["x"] = np.ascontiguousarray(x[2 * c:2 * c + 2])
        in_maps.append(m)
    res = run_bass_kernel_spmd(nc, in_maps, core_ids=list(range(n)))
    return np.concatenate([r["y"] for r in res.results], axis=0).astype(np.float32)
```

```python
import numpy as np
import ml_dtypes
import concourse.bass as bass
import concourse.mybir as mybir
from concourse.bass_utils import run_bass_kernel_spmd

F32 = mybir.dt.float32
BF16 = mybir.dt.bfloat16
AF = mybir.ActivationFunctionType
ALU = mybir.AluOpType
AX = mybir.AxisListType

T = 2048
D = 1024
DFF = 2816
NF = DFF // 128
NEGM = -30000.0


class Buf:
    __slots__ = ("name", "w", "r")

    def __init__(self, name):
        self.name = name
        self.w = None
        self.r = {}


class EngState:
    def __init__(self, name, eng, sem):
        self.name = name
        self.eng = eng
        self.sem = sem
        self.cnt = 0
        self.seen = {}


class Sync:
    def __init__(self, nc, ndma=20):
        self.nc = nc
        self.E = {}
        for name, e in (("pe", nc.tensor), ("act", nc.scalar), ("dve", nc.vector),
                        ("pool", nc.gpsimd), ("sp", nc.sync)):
            self.E[name] = EngState(name, e, nc.alloc_semaphore("s_" + name))
        self.dsem = {}
        self.dnext = {}
        for q in ("sp", "pool"):
            self.dsem[q] = [[nc.alloc_semaphore("d_%s%d" % (q, i)), 0] for i in range(ndma)]
            self.dnext[q] = 0
        self.semof = {}
        for name, st in self.E.items():
            self.semof[("c", name)] = st.sem
        for q in ("sp", "pool"):
            for i, (s, _) in enumerate(self.dsem[q]):
                self.semof[("d", q, i)] = s

    def _wait(self, E, deps):
        for key, n in deps.items():
            if key == ("c", "pe") and E.name == "pe":
                continue
            if E.seen.get(key, 0) >= n:
                continue
            E.eng.wait_ge(self.semof[key], n)
            E.seen[key] = n

    @staticmethod
    def _deps(reads, writes):
        deps = {}

        def add(key, n):
            if deps.get(key, 0) < n:
                deps[key] = n
        for b in reads:
            if b.w is not None:
                add(*b.w)
        for b in writes:
            if b.w is not None:
                add(*b.w)
            for key, n in b.r.items():
                add(key, n)
        return deps

    @staticmethod
    def _mark(ev, reads, writes):
        key, n = ev
        for b in reads:
            if b.r.get(key, 0) < n:
                b.r[key] = n
        for b in writes:
            b.w = ev
            b.r = {}

    def op(self, engname, fn, reads=(), writes=()):
        E = self.E[engname]
        self._wait(E, self._deps(reads, writes))
        ins = fn(E.eng)
        E.cnt += 1
        ins.then_inc(E.sem, 1)
        ev = (("c", engname), E.cnt)
        self._mark(ev, reads, writes)
        return ev

    def group(self, engname, fns, reads=(), writes=()):
        E = self.E[engname]
        self._wait(E, self._deps(reads, writes))
        ins = None
        for fn in fns:
            ins = fn(E.eng)
        E.cnt += 1
        ins.then_inc(E.sem, 1)
        ev = (("c", engname), E.cnt)
        self._mark(ev, reads, writes)
        return ev

    def split_group(self, engname, fns, reads, writes, per):
        E = self.E[engname]
        self._wait(E, self._deps(reads, writes))
        n = len(fns)
        for i, fn in enumerate(fns):
            ins = fn(E.eng)
            if i == n - 1:
                E.cnt += 1
                ins.then_inc(E.sem, 1)
                self._mark((("c", engname), E.cnt), reads, writes)
            elif (i + 1) % per == 0:
                yield

    def dma(self, q, out, in_, reads=(), writes=()):
        E = self.E[q]
        i = self.dnext[q]
        self.dnext[q] = (i + 1) % len(self.dsem[q])
        slot = self.dsem[q][i]
        key = ("d", q, i)
        deps = self._deps(reads, writes)
        if slot[1] > 0 and deps.get(key, 0) < slot[1]:
            deps[key] = slot[1]
        self._wait(E, deps)
        E.eng.dma_start(out=out, in_=in_).then_inc(slot[0], 16)
        slot[1] += 16
        ev = (key, slot[1])
        self._mark(ev, reads, writes)
        return ev

    def barrier(self, engines=("pe", "act", "dve", "pool", "sp")):
        deps = {}
        for name, st in self.E.items():
            if st.cnt > 0:
                deps[("c", name)] = st.cnt
        for q in ("sp", "pool"):
            for i, (s, n) in enumerate(self.dsem[q]):
                if n > 0:
                    deps[("d", q, i)] = n
        for name in engines:
            self._wait(self.E[name], deps)


class Arena:
    def __init__(self, nc, base, top):
        self.nc = nc
        self.ptr = (base + 31) // 32 * 32
        self.top = top
        self.n = 0

    def alloc(self, name, shape, dtype):
        nbytes = 2 if dtype == BF16 else 4
        sz = 1
        for s in shape[1:]:
            sz *= s
        sz *= nbytes
        off = self.ptr
        self.ptr = (off + sz + 31) // 32 * 32
        assert self.ptr <= self.top, ("SBUF overflow", name, self.ptr, self.top)
        self.n += 1
        return self.nc.alloc_sbuf_tensor_at("%s_%d" % (name, self.n), list(shape), dtype, offset=off)

    def mark(self):
        return self.ptr

    def reset(self, m):
        self.ptr = m


def _alibi_slopes():
    n = 12
    slopes = 2.0 ** (-8.0 * np.arange(1, n + 1) / n)
    diff_idx = np.arange(4) * 3 + 2
    moba_idx = np.setdiff1d(np.arange(n), diff_idx)
    return slopes[moba_idx].astype(np.float32), slopes[diff_idx].astype(np.float32)


def _host_consts():
    bf = ml_dtypes.bfloat16
    ms, ds = _alibi_slopes()
    slopes = np.concatenate([ms, ds]).astype(np.float32)
    pos = np.arange(T)
    hi = (256 * (pos // 256)).astype(np.float32)
    lo = (pos % 256).astype(np.float32)
    qx = np.zeros((12, 16, T), np.float32)
    kx = np.zeros((12, 16, T), np.float32)
    for h in range(12):
        c1 = np.float32(slopes[h]).astype(bf).astype(np.float32)
        c2 = np.float32(slopes[h] - c1).astype(bf).astype(np.float32)
        qx[h, 8], qx[h, 9], qx[h, 10], qx[h, 11] = hi, lo, hi, lo
        qx[h, 12], qx[h, 13], qx[h, 14], qx[h, 15] = c1, c1, c2, c2
        kx[h, 8], kx[h, 9], kx[h, 10], kx[h, 11] = -c1, -c1, -c2, -c2
        kx[h, 12], kx[h, 13], kx[h, 14], kx[h, 15] = hi, lo, hi, lo
        if h < 8:
            for n in range(8):
                kx[h, n] = (pos // 256 == n).astype(np.float32)
    ident = np.eye(128, dtype=np.float32)
    kk = np.arange(128)[:, None]
    qq = np.arange(128)[None, :]
    negtri = np.where(kk <= qq, 0.0, NEGM).astype(np.float32)
    pastmask = np.zeros((64, 8), np.float32)
    pairsum = np.zeros((64, 72), np.float32)
    for n in range(8):
        for m in range(8):
            p = n * 8 + m
            pairsum[p, 64 + n] = 1.0
            for qb in range(8):
                pastmask[p, qb] = 1.0 if m < qb else 0.0
    notown = np.zeros((128, 8), np.float32)
    for n in range(8):
        for qb in range(8):
            notown[64 + n, qb] = NEGM if n < qb else 0.0
    return {
        "c_qx": qx.astype(bf), "c_kx": kx.astype(bf), "c_ident": ident.astype(bf),
        "c_negtri": negtri.astype(bf), "c_pastmask": pastmask, "c_pairsum": pairsum.astype(bf),
        "c_notown": notown,
    }


def build(nseq=2, debug=False):
    nc = bass.Bass("TRN2", target_bir_lowering=False)
    S = Sync(nc)

    def din(name, shape, dt=F32):
        return nc.dram_tensor(name, list(shape), dt, kind="ExternalInput").ap()

    x = din("x", [nseq, T, D])
    w_in = din("w_in", [D, 5120])
    w_a = din("w_branch_a", [512, D])
    w_b = din("w_branch_b", [512, D])
    w_out = din("w_out", [D, D])
    w_gate = din("w_gate", [D, DFF])
    w_up = din("w_up", [D, DFF])
    w_down = din("w_down", [DFF, D])
    g_pre = din("norm_mix_pre_g", [1, D])
    g_post = din("norm_mix_post_g", [1, D])
    g_fpre = din("norm_ffn_pre_g", [1, D])
    g_fpost = din("norm_ffn_post_g", [1, D])
    lamv = din("lamv", [1, 256])
    g_sub = din("diff_subln_g", [128, 1])
    c_qx = din("c_qx", [12, 16, T], BF16)
    c_kx = din("c_kx", [12, 16, T], BF16)
    c_ident = din("c_ident", [128, 128], BF16)
    c_negtri = din("c_negtri", [128, 128], BF16)
    c_pastmask = din("c_pastmask", [64, 8])
    c_pairsum = din("c_pairsum", [64, 72], BF16)
    c_notown = din("c_notown", [128, 8])
    y = nc.dram_tensor("y", [nseq, T, D], F32, kind="ExternalOutput").ap()
    x1s = nc.dram_tensor("x1s", [nseq, T, D], F32).ap()
    dbg = {}
    if debug:
        dbg["hT"] = nc.dram_tensor("dbg_hT", [128, 8, T], BF16, kind="ExternalOutput").ap()
        dbg["yT"] = nc.dram_tensor("dbg_yT", [128, 8, T], BF16, kind="ExternalOutput").ap()
        dbg["mT"] = nc.dram_tensor("dbg_mT", [128, 8, T], BF16, kind="ExternalOutput").ap()

    w_in_r = w_in.rearrange("(kc p) n -> p kc n", p=128)
    w_in_r4 = w_in.rearrange("(kc p) (a n) -> p kc a n", p=128, n=128)
    w_a_r = w_a.rearrange("(kc p) n -> p kc n", p=128)
    w_b_r = w_b.rearrange("(kc p) n -> p kc n", p=128)
    w_out_r = w_out.rearrange("(kc p) n -> p kc n", p=128)
    w_gate_r = w_gate.rearrange("(kc p) n -> p kc n", p=128)
    w_up_r = w_up.rearrange("(kc p) n -> p kc n", p=128)
    w_down_r = w_down.rearrange("(f p) n -> p f n", p=128)

    _ms, _ds = _alibi_slopes()
    WIN = [min(15, int((100.0 / float(sl_) + 127.0) // 128)) for sl_ in list(_ms) + list(_ds)]
    ar = Arena(nc, nc.sbuf_base, nc.sbuf_top)
    ps = nc.alloc_psum_tensor("ps", [128, 8, 512], F32)
    PB = [Buf("bank%d" % i) for i in range(8)]

    ident = ar.alloc("ident", [128, 128], BF16)
    negtri = ar.alloc("negtri", [128, 128], BF16)
    ones_bf = ar.alloc("ones", [128, 128], BF16)
    pastmask = ar.alloc("pastmask", [64, 8], F32)
    pairsum = ar.alloc("pairsum", [64, 72], BF16)
    notown = ar.alloc("notown", [128, 8], F32)
    gsub = ar.alloc("gsub", [128, 1], F32)
    neglam = ar.alloc("neglam", [128, 1], F32)
    lamt = ar.alloc("lamt", [128, 256], F32)
    lamp = ar.alloc("lamp", [128, 128], F32)
    lams = ar.alloc("lams", [128, 4], F32)
    XIN = [ar.alloc("xin", [128, D], F32) for _ in range(2)]
    XINb = [Buf("xin%d" % i) for i in range(2)]
    HN = [ar.alloc("hn", [128, D], BF16) for _ in range(2)]
    HNb = [Buf("hn%d" % i) for i in range(2)]
    junk = ar.alloc("junk", [128, D], BF16)
    junkb = Buf("junk")
    SS = [ar.alloc("ss", [128, 4], F32) for _ in range(4)]
    SSb = [Buf("ss%d" % i) for i in range(4)]
    epsD = ar.alloc("epsD", [128, 1], F32)
    TST = [ar.alloc("tst", [128, D], F32) for _ in range(2)]
    TSTb = [Buf("tst%d" % i) for i in range(2)]
    cb = Buf("consts")

    S.dma("sp", ident[:], c_ident, writes=[cb])
    S.dma("sp", negtri[:], c_negtri, writes=[cb])
    S.dma("sp", pastmask[:], c_pastmask, writes=[cb])
    S.dma("sp", pairsum[:], c_pairsum, writes=[cb])
    S.dma("sp", notown[:], c_notown, writes=[cb])
    S.dma("sp", gsub[:], g_sub, writes=[cb])
    S.dma("sp", lamt[:], lamv.partition_broadcast(128), writes=[cb])
    S.op("dve", lambda e: e.memset(ones_bf[:], 1.0), writes=[cb])
    S.op("dve", lambda e: e.memset(epsD[:], 1e-6), writes=[cb])
    lamtv = lamt[:].rearrange("p (a b d) -> p a b d", a=2, b=2)
    S.op("dve", lambda e: e.tensor_tensor(out=lamp[:].rearrange("p (a d) -> p a d", a=2),
                                          in0=lamtv[:, :, 0, :], in1=lamtv[:, :, 1, :], op=ALU.mult),
         reads=[cb], writes=[cb])
    S.op("dve", lambda e: e.tensor_reduce(out=lams[:, 0:2], in_=lamp[:].rearrange("p (a d) -> p a d", a=2),
                                          axis=AX.X, op=ALU.add), reads=[cb], writes=[cb])
    S.op("act", lambda e: e.activation(out=lams[:, 2:4], in_=lams[:, 0:2], func=AF.Exp), reads=[cb], writes=[cb])
    S.op("dve", lambda e: e.scalar_tensor_tensor(out=neglam[:], in0=lams[:, 3:4], scalar=-0.2, in1=lams[:, 2:3],
                                                 op0=ALU.add, op1=ALU.subtract), reads=[cb], writes=[cb])
    S.op("dve", lambda e: e.tensor_scalar(out=gsub[:], in0=gsub[:], scalar1=0.8, scalar2=None, op0=ALU.mult),
         reads=[cb], writes=[cb])

    base_mark = ar.mark()

    NSS = 4

    def norm_A(src_ap, srcb, k):
        ss, ssb = SS[k % NSS], SSb[k % NSS]
        S.op("act", lambda e: e.memzero(ss[:]), writes=[ssb])
        S.op("act", lambda e: e.activation(out=junk[:], in_=src_ap, func=AF.Square, accum_out=ss[:, 0:1]),
             reads=srcb, writes=[junkb, ssb])
        S.op("act", lambda e: e.activation(out=ss[:, 1:2], in_=ss[:, 0:1], func=AF.Ln, scale=1.0 / D,
                                           bias=epsD[:, 0:1]), reads=[ssb, cb], writes=[ssb])
        S.op("act", lambda e: e.activation(out=ss[:, 1:2], in_=ss[:, 1:2], func=AF.Exp, scale=-0.5),
             reads=[ssb], writes=[ssb])

    def norm_B(src_ap, srcb, gt, gtb, k, bank):
        ss, ssb = SS[k % NSS], SSb[k % NSS]
        hn, hnb = HN[k % 2], HNb[k % 2]
        S.op("dve", lambda e: e.scalar_tensor_tensor(out=hn[:], in0=src_ap, scalar=ss[:, 1:2], in1=gt[:],
                                                     op0=ALU.mult, op1=ALU.mult),
             reads=list(srcb) + [ssb, gtb], writes=[hnb])
        psb = ps[:, bank, :].bitcast(BF16)
        S.group("pe", [(lambda e, kc=kc: e.transpose(out=psb[:, kc * 128:(kc + 1) * 128],
                                                      in_=hn[:, kc * 128:(kc + 1) * 128], identity=ident[:]))
                       for kc in range(8)], reads=[hnb, cb], writes=[PB[bank]])

    def norm_C(dstT, dstb, tcol, bank):
        psb = ps[:, bank, :].bitcast(BF16)
        S.op("act", lambda e: e.activation(out=dstT[:, :, tcol:tcol + 128],
                                           in_=psb.rearrange("p (k t) -> p k t", k=8), func=AF.Copy),
             reads=[PB[bank]], writes=[dstb])

    def norm_transpose_pipeline(n, src_of, gt, gtb, dstT, dstb, banks):
        srcs = {}

        def A(k):
            srcs[k] = src_of(k)
            norm_A(srcs[k][0], srcs[k][1], k)

        def B(k):
            norm_B(srcs[k][0], srcs[k][1], gt, gtb, k, banks[k % len(banks)])

        def C(k):
            norm_C(dstT, dstb, k * 128, banks[k % len(banks)])
        A(0)
        for k in range(n):
            if k + 1 < n:
                A(k + 1)
            B(k)
            if k >= 1:
                C(k - 1)
        C(n - 1)

    def post_A(banks, k):
        ss, ssb = SS[k % NSS], SSb[k % NSS]
        S.op("act", lambda e: e.memzero(ss[:]), writes=[ssb])
        S.op("act", lambda e: e.activation(out=junk[:], in_=ps[:, banks[0]:banks[0] + 2, :].rearrange("p b n -> p (b n)"),
                                           func=AF.Square, accum_out=ss[:, 0:1]),
             reads=[PB[banks[0]], PB[banks[1]]], writes=[junkb, ssb])
        S.op("act", lambda e: e.activation(out=ss[:, 1:2], in_=ss[:, 0:1], func=AF.Ln, scale=1.0 / D,
                                           bias=epsD[:, 0:1]), reads=[ssb, cb], writes=[ssb])
        S.op("act", lambda e: e.activation(out=ss[:, 1:2], in_=ss[:, 1:2], func=AF.Exp, scale=-0.5),
             reads=[ssb], writes=[ssb])

    def post_B(banks, gt, gtb, resid_ap, residb, k, out_dram):
        ss, ssb = SS[k % NSS], SSb[k % NSS]
        tst, tstb = TST[k % 2], TSTb[k % 2]
        for hf in range(2):
            S.op("dve", lambda e, hf=hf: e.scalar_tensor_tensor(
                out=tst[:, hf * 512:(hf + 1) * 512], in0=ps[:, banks[hf], :], scalar=ss[:, 1:2],
                in1=gt[:, hf * 512:(hf + 1) * 512], op0=ALU.mult, op1=ALU.mult),
                reads=[PB[banks[hf]], ssb, gtb], writes=[tstb])
        S.op("dve", lambda e: e.tensor_tensor(out=tst[:], in0=tst[:], in1=resid_ap, op=ALU.add),
             reads=[tstb] + list(residb), writes=[tstb])
        S.dma("sp", out_dram, tst[:], reads=[tstb])

    for seq in range(nseq):
        ar.reset(base_mark)
        hT = ar.alloc("hT", [128, 8, T], BF16)
        hTb = Buf("hT")
        yT = ar.alloc("yT", [128, 8, T], BF16)
        yTb = Buf("yT")
        a_mark = ar.mark()
        gpre = ar.alloc("gpre", [128, D], F32)
        gpreb = Buf("gpre")
        QK = [[ar.alloc("qk", [128, T], BF16) for _ in range(4)] for _ in range(2)]
        QKb = [[Buf("qk") for _ in range(4)] for _ in range(2)]
        VV = [ar.alloc("vv", [128, 16, 320], BF16) for _ in range(2)]
        VVb = [Buf("vv") for _ in range(2)]
        WSL = [ar.alloc("wsl", [128, 8, 384], BF16) for _ in range(2)]
        WSLb = [Buf("wsl") for _ in range(2)]
        NPB = 5
        PBF = [ar.alloc("pbf", [128, 512], BF16) for _ in range(NPB)]
        PBFb = [Buf("pbf") for _ in range(NPB)]
        ksum = [ar.alloc("ksum", [64, 8], F32) for _ in range(2)]
        diffw = [ar.alloc("diffw", [64, 64], BF16) for _ in range(2)]
        ind = [ar.alloc("ind", [64, 512], BF16) for _ in range(2)]
        gateb = [Buf("gate") for _ in range(2)]
        indb = [Buf("ind") for _ in range(2)]
        rscr = [ar.alloc("rscr", [128, 512], F32) for _ in range(2)]
        rbuf = [ar.alloc("rbuf", [128, 512], F32) for _ in range(2)]
        rbufb = [Buf("rbuf") for _ in range(2)]
        dL1 = [ar.alloc("dL1", [128, 512], F32) for _ in range(2)]
        dL2 = [ar.alloc("dL2", [128, 512], F32) for _ in range(2)]
        dO1 = [ar.alloc("dO1", [128, 512], F32) for _ in range(2)]
        dO2 = [ar.alloc("dO2", [128, 512], F32) for _ in range(2)]
        dSQ = [ar.alloc("dSQ", [128, 512], BF16) for _ in range(2)]
        dRS = [ar.alloc("dRS", [128, 512], F32) for _ in range(2)]
        dfb = [Buf("dfin") for _ in range(2)]

        S.dma("sp", gpre[:], g_pre.partition_broadcast(128), writes=[gpreb])
        for sl in range(2):
            for i in range(4):
                S.op("dve", lambda e, sl=sl, i=i: e.memset(QK[sl][i][:], 0.0), writes=[QKb[sl][i]])
            S.op("dve", lambda e, sl=sl: e.memset(VV[sl][:], 1.0), writes=[VVb[sl]])

        def p1_src(tt):
            xin, xinb = XIN[tt % 2], XINb[tt % 2]
            S.dma("sp", xin[:], x[seq, tt * 128:(tt + 1) * 128, :], writes=[xinb])
            return xin[:], [xinb]
        norm_transpose_pipeline(16, p1_src, gpre, gpreb, hT, hTb, (6, 7))
        if debug and seq == 0:
            S.dma("sp", dbg["hT"], hT[:], reads=[hTb])

        def load_group_weights(g):
            sl = g % 2
            a0 = g if g < 4 else 12 + (g - 4)
            for i3 in range(3):
                S.dma("pool", WSL[sl][:, :, i3 * 128:(i3 + 1) * 128], w_in_r4[:, :, a0 + 4 * i3, :],
                      writes=[WSLb[sl]])
            hA, hB = (2 * g, 2 * g + 1) if g < 4 else (8 + g - 4, 8 + g - 4)
            q = QK[sl]
            qb_ = QKb[sl]
            S.dma("sp", q[0][64:80, :], c_qx[hA], writes=[qb_[0]])
            S.dma("sp", q[1][64:80, :], c_qx[hB], writes=[qb_[1]])
            S.dma("sp", q[2][64:80, :], c_kx[hA], writes=[qb_[2]])
            S.dma("sp", q[3][64:80, :], c_kx[hB], writes=[qb_[3]])

        pbk = {"i": 0}

        pbk["set"] = (7, 5, 6)

        def pbank():
            pbk["i"] += 1
            st = pbk["set"]
            return st[pbk["i"] % len(st)]

        def prep_units(g):
            sl = g % 2
            q, qb_ = QK[sl], QKb[sl]
            w, wb = WSL[sl], WSLb[sl]
            load_group_weights(g)
            yield
            for c in range(4):
                cs = slice(c * 512, (c + 1) * 512)
                bank = pbank()
                yield from S.split_group(
                    "pe", [(lambda e, kc=kc: e.matmul(ps[:, bank, :], lhsT=w[:, kc, 0:128], rhs=hT[:, kc, cs],
                                                      start=(kc == 0), stop=(kc == 7))) for kc in range(8)],
                    [wb, hTb], [PB[bank]], 2)
                S.op("dve", lambda e: e.tensor_scalar(out=q[0][0:64, cs], in0=ps[0:64, bank, :], scalar1=0.125,
                                                      scalar2=None, op0=ALU.mult),
                     reads=[PB[bank]], writes=[qb_[0]])
                S.op("dve", lambda e: e.tensor_scalar(out=q[1][0:64, cs], in0=ps[64:128, bank, :], scalar1=0.125,
                                                      scalar2=None, op0=ALU.mult),
                     reads=[PB[bank]], writes=[qb_[1]])
                yield
                bank = pbank()
                yield from S.split_group(
                    "pe", [(lambda e, kc=kc: e.matmul(ps[:, bank, :], lhsT=w[:, kc, 128:256], rhs=hT[:, kc, cs],
                                                      start=(kc == 0), stop=(kc == 7))) for kc in range(8)],
                    [wb, hTb], [PB[bank]], 2)
                S.op("dve", lambda e: e.tensor_copy(out=q[2][0:64, cs], in_=ps[0:64, bank, :]),
                     reads=[PB[bank]], writes=[qb_[2]])
                S.op("dve", lambda e: e.tensor_copy(out=q[3][0:64, cs], in_=ps[64:128, bank, :]),
                     reads=[PB[bank]], writes=[qb_[3]])
                yield
            vv, vvb = VV[sl], VVb[sl]
            for tg in range(4):
                bank = pbank()
                fns = []
                for i in range(4):
                    tt = 4 * tg + i
                    for kc in range(8):
                        fns.append(lambda e, i=i, tt=tt, kc=kc: e.matmul(
                            ps[:, bank, i * 128:(i + 1) * 128], lhsT=hT[:, kc, tt * 128:(tt + 1) * 128],
                            rhs=w[:, kc, 256:384], start=(kc == 0), stop=(kc == 7)))
                yield from S.split_group("pe", fns, [wb, hTb], [PB[bank]], 4)
                if g < 4:
                    vq = vv[:].rearrange("p t (a d) -> p t a d", d=64)
                    S.op("dve", lambda e: e.tensor_copy(
                        out=vq[:, 4 * tg:4 * tg + 4, 0:3:2, :],
                        in_=ps[:, bank, :].rearrange("p (t h d) -> p t h d", t=4, h=2)),
                        reads=[PB[bank]], writes=[vvb])
                else:
                    S.op("dve", lambda e: e.tensor_copy(
                        out=vv[:, 4 * tg:4 * tg + 4, 192:320],
                        in_=ps[:, bank, :].rearrange("p (t d) -> p t d", t=4)),
                        reads=[PB[bank]], writes=[vvb])
                yield
            if g < 4:
                for hx in range(2):
                    for _ in moba_gate(g, hx):
                        yield

        def moba_gate(g, hx):
            sl = g % 2
            qt, qtb = QK[sl][hx], QKb[sl][hx]
            kt_, ktb = QK[sl][2 + hx], QKb[sl][2 + hx]
            ks, dw, gb_ = ksum[hx], diffw[hx], gateb[hx]
            S.op("dve", lambda e: e.tensor_reduce(out=ks[:], in_=kt_[0:64, :].rearrange("p (n k) -> p n k", k=256),
                                                  axis=AX.X, op=ALU.add), reads=[ktb], writes=[gb_])
            for n in range(8):
                S.op("dve", lambda e, n=n: e.tensor_scalar(out=dw[:, n * 8:(n + 1) * 8], in0=ks[:, 0:8],
                                                           scalar1=ks[:, n:n + 1], scalar2=None,
                                                           op0=ALU.subtract), reads=[gb_], writes=[gb_])
            yield
            for c in (2, 3):
                cs = slice(c * 512, (c + 1) * 512)
                bank = pbank()
                S.group("pe", [lambda e: e.matmul(ps[0:64, bank, :], lhsT=dw[:, :], rhs=qt[0:64, cs],
                                                  start=True, stop=True)],
                        reads=[gb_, qtb], writes=[PB[bank]])
                for hh in range(2):
                    qblk = 2 * c + hh
                    hs = slice(hh * 256, (hh + 1) * 256)
                    S.op("dve", lambda e, hs=hs, qblk=qblk: e.tensor_scalar(
                        out=ind[hx][:, hs], in0=ps[0:64, bank, hs], scalar1=0.0,
                        scalar2=pastmask[:, qblk:qblk + 1], op0=ALU.is_gt, op1=ALU.mult),
                        reads=[PB[bank], cb], writes=[indb[hx]])
                yield
                bank = pbank()
                S.group("pe", [lambda e: e.matmul(ps[0:72, bank, :], lhsT=pairsum[:, :], rhs=ind[hx][:, :],
                                                  start=True, stop=True)],
                        reads=[indb[hx], cb], writes=[PB[bank]])
                for hh in range(2):
                    qblk = 2 * c + hh
                    hs = slice(hh * 256, (hh + 1) * 256)
                    S.op("dve", lambda e, hs=hs, qblk=qblk: e.tensor_scalar(
                        out=qt[64:72, c * 512 + hh * 256:c * 512 + (hh + 1) * 256], in0=ps[64:72, bank, hs],
                        scalar1=2.5, scalar2=notown[64:72, qblk:qblk + 1], op0=ALU.is_gt, op1=ALU.mult),
                        reads=[PB[bank], cb], writes=[qtb])
                yield

        state = {"s": 0, "p": 0, "la": 2, "sb": (0, 1, 2)}

        def tile_cols(c, W):
            out = []
            for kt in range(max(0, 4 * c - W), 4 * c + 4):
                qlo = max(kt, 4 * c)
                qhi = min(4 * c + 3, kt + W)
                if qhi < qlo:
                    continue
                out.append((kt, (128 * (qlo - 4 * c), 128 * (qhi - 4 * c + 1))))
            return out
        pending = []

        def defer(n, fn):
            pending.append([n, fn])

        def run_pending(flush=False):
            while True:
                ready = None
                for ent in pending:
                    if flush or ent[0] <= 0:
                        ready = ent
                        break
                if ready is None:
                    break
                pending.remove(ready)
                ready[1]()
            for ent in pending:
                ent[0] -= 1

        def attention(items, finalize, filler, every):
            n = len(items)
            sb_of = {}

            def qk(i):
                it = items[i]
                sbank = state["sb"][state["s"] % len(state["sb"])]
                state["s"] += 1
                sb_of[i] = sbank
                c, kt = it["c"], it["kt"]
                j = kt - 4 * c
                col0, col1 = it["cols"]
                fns = [lambda e: e.matmul(ps[:, sbank, col0:col1], lhsT=it["k"][0:80, kt * 128:(kt + 1) * 128],
                                          rhs=it["q"][0:80, c * 512 + col0:c * 512 + col1],
                                          start=True, stop=(j < 0))]
                if j >= 0:
                    fns.append(lambda e: e.matmul(ps[:, sbank, col0:col0 + 128], lhsT=ident[:], rhs=negtri[:],
                                                  start=False, stop=True))
                S.group("pe", fns, reads=[it["kb"], it["qb"], cb], writes=[PB[sbank]])

            def ex_pv(i):
                it = items[i]
                sbank = sb_of[i]
                pi = state["p"] % NPB
                state["p"] += 1
                col0, col1 = it["cols"]
                win = it["win"]
                S.op("act", lambda e: e.activation(out=PBF[pi][:, col0:col1], in_=ps[:, sbank, col0:col1], func=AF.Exp),
                     reads=[PB[sbank]], writes=[PBFb[pi]])
                fns = []
                banks = []
                rd = [PBFb[pi]]
                for (bank, lap, lbuf) in it["pv"]:
                    if win and it["first"]:
                        S.op("dve", lambda e, bank=bank: e.memset(ps[:, bank, :], 0.0), writes=[PB[bank]])
                    if win:
                        fns.append(lambda e, bank=bank, lap=lap: e.matmul(
                            ps[:, bank, col0:col1], lhsT=lap, rhs=PBF[pi][:, col0:col1],
                            start=False, stop=it["last"], skip_group_check=True))
                    else:
                        fns.append(lambda e, bank=bank, lap=lap: e.matmul(
                            ps[:, bank, col0:col1], lhsT=lap, rhs=PBF[pi][:, col0:col1],
                            start=it["first"], stop=it["last"]))
                    banks.append(PB[bank])
                    rd.append(lbuf)
                S.group("pe", fns, reads=rd, writes=banks)
                if it["last"]:
                    finalize(it)

            la = state["la"]
            for i in range(min(la, n)):
                qk(i)
            for i in range(n):
                if i + la < n:
                    qk(i + la)
                ex_pv(i)
                run_pending()
                if filler is not None and i % every == every - 1:
                    next(filler, None)

        def recip(out_ap, in_ap, scr_ap, reads, wb_, on_act=False):
            if on_act:
                S.op("act", lambda e: e.activation(out=scr_ap, in_=in_ap, func=AF.Ln), reads=reads, writes=[wb_])
                S.op("act", lambda e: e.activation(out=out_ap, in_=scr_ap, func=AF.Exp, scale=-1.0),
                     reads=[wb_], writes=[wb_])
            else:
                S.op("dve", lambda e: e.reciprocal(out=out_ap, in_=in_ap), reads=reads, writes=[wb_])

        def run_moba(g, filler):
            sl = g % 2
            for hx in range(2):
                items = []
                W = WIN[2 * g + hx]
                for c in range(4):
                    bank = 3 + ((hx * 4 + c) % 2)
                    lap_of = (lambda kt: VV[sl][:, kt, 0:128]) if hx == 0 else (lambda kt: VV[sl][:, kt, 64:192])
                    first = True
                    for kt, cols in tile_cols(c, W):
                        items.append(dict(q=QK[sl][hx], qb=QKb[sl][hx], k=QK[sl][2 + hx], kb=QKb[sl][2 + hx],
                                          c=c, kt=kt, pv=[(bank, lap_of(kt), VVb[sl])], last=(kt == 4 * c + 3),
                                          bank=bank, cols=cols, first=first, win=(W < 15)))
                        first = False

                def fin(it, hx=hx):
                    bank, c = it["bank"], it["c"]
                    cs = slice(c * 512, (c + 1) * 512)
                    rb, rbb = rbuf[c % 2], rbufb[c % 2]
                    rs_ = rscr[c % 2]
                    if hx == 0:
                        recip(rb[0:64, :], ps[64:128, bank, :], rs_[0:64, :], [PB[bank]], rbb, on_act=True)
                        S.op("dve", lambda e: e.tensor_tensor(out=yT[0:64, g, cs], in0=ps[0:64, bank, :],
                                                              in1=rb[0:64, :], op=ALU.mult),
                             reads=[PB[bank], rbb], writes=[yTb])
                    else:
                        recip(rb[64:128, :], ps[0:64, bank, :], rs_[64:128, :], [PB[bank]], rbb, on_act=True)
                        S.op("dve", lambda e: e.tensor_tensor(out=yT[64:128, g, cs], in0=ps[64:128, bank, :],
                                                              in1=rb[64:128, :], op=ALU.mult),
                             reads=[PB[bank], rbb], writes=[yTb])
                attention(items, fin, filler, 1)

        def run_diff(g, filler):
            sl = g % 2
            j = g - 4
            items = []
            W = WIN[8 + j]
            for c in range(4):
                first = True
                for kt, cols in tile_cols(c, W):
                    for mp in range(2):
                        items.append(dict(q=QK[sl][mp], qb=QKb[sl][mp], k=QK[sl][2 + mp], kb=QKb[sl][2 + mp],
                                          c=c, kt=kt,
                                          pv=[(3 + mp, VV[sl][:, kt, 192:320], VVb[sl]),
                                              (5 + mp, ones_bf[:], cb)],
                                          last=(kt == 4 * c + 3), mp=mp, cols=cols, first=first, win=(W < 15)))
                    first = False

            def fin(it):
                if it["mp"] == 0:
                    return
                run_pending(flush=True)
                c = it["c"]
                cs = slice(c * 512, (c + 1) * 512)
                k2 = c % 2
                L1, L2, O1, O2, SQ, RS, fb = dL1[k2], dL2[k2], dO1[k2], dO2[k2], dSQ[k2], dRS[k2], dfb[k2]
                S.op("act", lambda e: e.activation(out=L1[:], in_=ps[:, 5, :], func=AF.Ln), reads=[PB[5]], writes=[fb])
                S.op("dve", lambda e: e.tensor_copy(out=O1[:], in_=ps[:, 3, :]), reads=[PB[3]], writes=[fb])
                S.op("act", lambda e: e.activation(out=L2[:], in_=ps[:, 6, :], func=AF.Ln), reads=[PB[6]], writes=[fb])
                S.op("dve", lambda e: e.tensor_copy(out=O2[:], in_=ps[:, 4, :]), reads=[PB[4]], writes=[fb])

                def stage_b():
                    S.op("act", lambda e: e.activation(out=L1[:], in_=L1[:], func=AF.Exp, scale=-1.0),
                         reads=[fb], writes=[fb])
                    S.op("act", lambda e: e.activation(out=L2[:], in_=L2[:], func=AF.Exp, scale=-1.0),
                         reads=[fb], writes=[fb])
                    S.op("dve", lambda e: e.tensor_tensor(out=O1[:], in0=O1[:], in1=L1[:], op=ALU.mult),
                         reads=[fb], writes=[fb])
                    S.op("dve", lambda e: e.tensor_tensor(out=O2[:], in0=O2[:], in1=L2[:], op=ALU.mult),
                         reads=[fb], writes=[fb])
                    S.op("dve", lambda e: e.scalar_tensor_tensor(out=O1[:], in0=O2[:], scalar=neglam[:, 0:1],
                                                                 in1=O1[:], op0=ALU.mult, op1=ALU.add),
                         reads=[fb, cb], writes=[fb])
                    S.op("act", lambda e: e.activation(out=SQ[:], in_=O1[:], func=AF.Square), reads=[fb], writes=[fb])
                    defer(2, stage_c)

                def stage_c():
                    bank = pbank()
                    S.group("pe", [lambda e: e.matmul(ps[:, bank, :], lhsT=ones_bf[:], rhs=SQ[:], start=True,
                                                      stop=True)], reads=[fb, cb], writes=[PB[bank]])
                    S.op("dve", lambda e: e.tensor_scalar(out=RS[:], in0=ps[:, bank, :], scalar1=1.0 / 128,
                                                          scalar2=1e-5, op0=ALU.mult, op1=ALU.add),
                         reads=[PB[bank]], writes=[fb])
                    S.op("act", lambda e: e.activation(out=RS[:], in_=RS[:], func=AF.Ln), reads=[fb], writes=[fb])
                    S.op("act", lambda e: e.activation(out=RS[:], in_=RS[:], func=AF.Exp, scale=-0.5),
                         reads=[fb], writes=[fb])
                    S.op("dve", lambda e: e.scalar_tensor_tensor(out=yT[:, 4 + j, cs], in0=O1[:], scalar=gsub[:, 0:1],
                                                                 in1=RS[:], op0=ALU.mult, op1=ALU.mult),
                         reads=[fb, cb], writes=[yTb])
                defer(2, stage_b)
            attention(items, fin, filler, 2)

        for _ in prep_units(0):
            pass
        for g in range(8):
            filler = prep_units(g + 1) if g + 1 < 8 else None
            if g < 4:
                pbk["set"] = (7, 5, 6)
                state["la"], state["sb"] = 2, (0, 1, 2)
                run_moba(g, filler)
            else:
                pbk["set"] = (2, 7)
                state["la"], state["sb"] = 1, (0, 1)
                run_diff(g, filler)
            if filler is not None:
                for _ in filler:
                    pass
            run_pending(flush=True)
        if debug and seq == 0:
            S.dma("sp", dbg["yT"], yT[:], reads=[yTb])

        S.barrier()
        ar.reset(a_mark)
        mT = ar.alloc("mT", [128, 8, T], BF16)
        mTb = Buf("mT")
        woT = ar.alloc("woT", [128, 8, D], BF16)
        woTb = Buf("woT")
        WM = [ar.alloc("wm", [128, 24, 128], BF16) for _ in range(2)]
        WMb = [Buf("wm") for _ in range(2)]
        gpost = ar.alloc("gpost", [128, D], F32)
        gpostb = Buf("gpost")
        SA = [ar.alloc("sa", [128, 512], F32) for _ in range(2)]
        SB_ = [ar.alloc("sb", [128, 512], F32) for _ in range(2)]
        TA = [ar.alloc("ta", [128, 512], F32) for _ in range(2)]
        TB = [ar.alloc("tb", [128, 512], F32) for _ in range(2)]
        MSb = [Buf("ms") for _ in range(2)]

        def load_merge_weights(j):
            sl = j % 2
            js = slice(128 * j, 128 * j + 128)
            S.dma("pool", WM[sl][:, 0:4, :], w_a_r[:, :, js], writes=[WMb[sl]])
            S.dma("pool", WM[sl][:, 4:8, :], w_b_r[:, :, js], writes=[WMb[sl]])
            S.dma("pool", WM[sl][:, 8:16, :], w_in_r[:, :, 3072 + 128 * j:3072 + 128 * j + 128], writes=[WMb[sl]])
            S.dma("pool", WM[sl][:, 16:24, :], w_in_r[:, :, 4096 + 128 * j:4096 + 128 * j + 128], writes=[WMb[sl]])

        S.dma("sp", gpost[:], g_post.partition_broadcast(128), writes=[gpostb])
        load_merge_weights(0)
        load_merge_weights(1)
        for hf in range(2):
            S.dma("pool", woT[:, 4 * hf:4 * hf + 4, :], w_out_r[:, 4 * hf:4 * hf + 4, :], writes=[woTb])
        it_ = 0
        for j in range(8):
            sl = j % 2
            wm, wmb = WM[sl], WMb[sl]
            for c in range(4):
                cs = slice(c * 512, (c + 1) * 512)
                b0 = 4 * (it_ % 2)
                k2 = it_ % 2
                it_ += 1
                S.group("pe", [(lambda e, kc=kc: e.matmul(ps[:, b0, :], lhsT=wm[:, kc, :], rhs=yT[:, kc, cs],
                                                           start=(kc == 0), stop=(kc == 3))) for kc in range(4)],
                        reads=[wmb, yTb], writes=[PB[b0]])
                S.group("pe", [(lambda e, kc=kc: e.matmul(ps[:, b0 + 1, :], lhsT=wm[:, 4 + kc, :],
                                                           rhs=yT[:, 4 + kc, cs],
                                                           start=(kc == 0), stop=(kc == 3))) for kc in range(4)],
                        reads=[wmb, yTb], writes=[PB[b0 + 1]])
                S.group("pe", [(lambda e, kc=kc: e.matmul(ps[:, b0 + 2, :], lhsT=wm[:, 8 + kc, :], rhs=hT[:, kc, cs],
                                                           start=(kc == 0), stop=(kc == 7))) for kc in range(8)],
                        reads=[wmb, hTb], writes=[PB[b0 + 2]])
                S.group("pe", [(lambda e, kc=kc: e.matmul(ps[:, b0 + 3, :], lhsT=wm[:, 16 + kc, :], rhs=hT[:, kc, cs],
                                                           start=(kc == 0), stop=(kc == 7))) for kc in range(8)],
                        reads=[wmb, hTb], writes=[PB[b0 + 3]])
                S.op("act", lambda e: e.activation(out=SA[k2][:], in_=ps[:, b0 + 2, :], func=AF.Sigmoid),
                     reads=[PB[b0 + 2]], writes=[MSb[k2]])
                S.op("act", lambda e: e.activation(out=SB_[k2][:], in_=ps[:, b0 + 3, :], func=AF.Sigmoid),
                     reads=[PB[b0 + 3]], writes=[MSb[k2]])
                S.op("dve", lambda e: e.tensor_tensor(out=TA[k2][:], in0=ps[:, b0, :], in1=SA[k2][:], op=ALU.mult),
                     reads=[PB[b0], MSb[k2]], writes=[MSb[k2]])
                S.op("dve", lambda e: e.tensor_tensor(out=TB[k2][:], in0=ps[:, b0 + 1, :], in1=SB_[k2][:],
                                                      op=ALU.mult),
                     reads=[PB[b0 + 1], MSb[k2]], writes=[MSb[k2]])
                S.op("dve", lambda e: e.tensor_tensor(out=mT[:, j, cs], in0=TA[k2][:], in1=TB[k2][:], op=ALU.add),
                     reads=[MSb[k2]], writes=[mTb])
            if j + 2 < 8:
                load_merge_weights(j + 2)
        if debug and seq == 0:
            S.dma("sp", dbg["mT"], mT[:], reads=[mTb])
        for tt in range(16):
            ts_ = slice(tt * 128, (tt + 1) * 128)
            banks = (2 * (tt % 2), 2 * (tt % 2) + 1)
            xin, xinb = XIN[tt % 2], XINb[tt % 2]
            S.dma("sp", xin[:], x[seq, ts_, :], writes=[xinb])
            for hf in range(2):
                S.group("pe", [(lambda e, kc=kc: e.matmul(ps[:, banks[hf], :], lhsT=mT[:, kc, ts_],
                                                           rhs=woT[:, kc, hf * 512:(hf + 1) * 512],
                                                           start=(kc == 0), stop=(kc == 7))) for kc in range(8)],
                        reads=[mTb, woTb], writes=[PB[banks[hf]]])
            post_A(banks, tt)
            post_B(banks, gpost, gpostb, xin[:], [xinb], tt, x1s[seq, ts_, :])

        S.barrier()
        ar.reset(base_mark)
        X1 = ar.alloc("X1", [128, 8, D], F32)
        X1b = [Buf("x1_%d" % i) for i in range(8)]
        h2T = ar.alloc("h2T", [128, 8, 1024], BF16)
        h2Tb = Buf("h2T")
        actT = ar.alloc("actT", [128, NF, 1024], BF16)
        actTb = Buf("actT")
        Wd = ar.alloc("Wd", [128, NF, D], BF16)
        Wdb = Buf("Wd")
        WGU = [ar.alloc("wgu", [128, 8, 512], BF16) for _ in range(2)]
        WGUb = [Buf("wgu") for _ in range(2)]
        gfpre = ar.alloc("gfpre", [128, D], F32)
        gfpost = ar.alloc("gfpost", [128, D], F32)
        gfb = Buf("gf")
        SG = [ar.alloc("sg", [128, 512], F32) for _ in range(2)]
        SGb = [Buf("sg") for _ in range(2)]

        S.dma("sp", gfpre[:], g_fpre.partition_broadcast(128), writes=[gfb])
        S.dma("sp", gfpost[:], g_fpost.partition_broadcast(128), writes=[gfb])

        def load_gu(fp):
            sl = fp % 2
            S.dma("pool", WGU[sl][:, :, 0:256], w_gate_r[:, :, 256 * fp:256 * fp + 256], writes=[WGUb[sl]])
            S.dma("pool", WGU[sl][:, :, 256:512], w_up_r[:, :, 256 * fp:256 * fp + 256], writes=[WGUb[sl]])

        nfp = NF // 2
        gu_loaded = [0]
        for half in range(2):
            if half == 0:
                load_gu(0)
                load_gu(1)
                for f2 in range(nfp):
                    S.dma("pool", Wd[:, 2 * f2:2 * f2 + 2, :], w_down_r[:, 2 * f2:2 * f2 + 2, :], writes=[Wdb])
            def p4_src(t8, half=half):
                tt = half * 8 + t8
                S.dma("sp", X1[:, t8, :], x1s[seq, tt * 128:(tt + 1) * 128, :], writes=[X1b[t8]])
                return X1[:, t8, :], [X1b[t8]]
            norm_transpose_pipeline(8, p4_src, gfpre, gfb, h2T, h2Tb, (6, 7))
            it_ = 0
            for fp in range(nfp):
                sl = fp % 2
                wg, wgb = WGU[sl], WGUb[sl]
                if half == 1 and fp == 0:
                    load_gu(0)
                    load_gu(1)
                for fi in range(2):
                    f = 2 * fp + fi
                    for c2 in range(2):
                        cs = slice(c2 * 512, (c2 + 1) * 512)
                        b0 = 2 * (it_ % 3)
                        k2 = it_ % 2
                        it_ += 1
                        S.group("pe", [(lambda e, kc=kc: e.matmul(ps[:, b0, :], lhsT=wg[:, kc, fi * 128:(fi + 1) * 128],
                                                                   rhs=h2T[:, kc, cs], start=(kc == 0), stop=(kc == 7)))
                                       for kc in range(8)], reads=[wgb, h2Tb], writes=[PB[b0]])
                        S.group("pe", [(lambda e, kc=kc: e.matmul(ps[:, b0 + 1, :],
                                                                   lhsT=wg[:, kc, 256 + fi * 128:256 + (fi + 1) * 128],
                                                                   rhs=h2T[:, kc, cs], start=(kc == 0), stop=(kc == 7)))
                                       for kc in range(8)], reads=[wgb, h2Tb], writes=[PB[b0 + 1]])
                        S.op("act", lambda e: e.activation(out=SG[k2][:], in_=ps[:, b0, :], func=AF.Silu),
                             reads=[PB[b0]], writes=[SGb[k2]])
                        S.op("dve", lambda e: e.tensor_tensor(out=actT[:, f, cs], in0=ps[:, b0 + 1, :], in1=SG[k2][:],
                                                              op=ALU.mult),
                             reads=[PB[b0 + 1], SGb[k2]], writes=[actTb])
                if fp + 2 < nfp:
                    load_gu(fp + 2)
            for t8 in range(8):
                tt = half * 8 + t8
                banks = (2 * (t8 % 2), 2 * (t8 % 2) + 1)
                for hf in range(2):
                    S.group("pe", [(lambda e, f=f: e.matmul(ps[:, banks[hf], :], lhsT=actT[:, f, t8 * 128:(t8 + 1) * 128],
                                                             rhs=Wd[:, f, hf * 512:(hf + 1) * 512],
                                                             start=(f == 0), stop=(f == NF - 1))) for f in range(NF)],
                            reads=[actTb, Wdb], writes=[PB[banks[hf]]])
                post_A(banks, t8)
                post_B(banks, gfpost, gfb, X1[:, t8, :], [X1b[t8]], t8, y[seq, tt * 128:(tt + 1) * 128, :])
        S.barrier()
    return nc


_NC_CACHE = {}


def kernel(x, norm_mix_pre_g, w_in, w_branch_a, w_branch_b, lam_q1, lam_k1, lam_q2, lam_k2,
           diff_subln_g, w_out, norm_mix_post_g, norm_ffn_pre_g, w_gate, w_up, w_down, norm_ffn_post_g):
    n = 8
    f32 = lambda a: np.ascontiguousarray(np.asarray(a, dtype=np.float32))
    x = f32(x)
    consts = _host_consts()
    shared = {
        "w_in": f32(w_in)[0], "w_branch_a": f32(w_branch_a)[0], "w_branch_b": f32(w_branch_b)[0],
        "w_out": f32(w_out)[0], "w_gate": f32(w_gate)[0], "w_up": f32(w_up)[0], "w_down": f32(w_down)[0],
        "norm_mix_pre_g": f32(norm_mix_pre_g), "norm_mix_post_g": f32(norm_mix_post_g),
        "norm_ffn_pre_g": f32(norm_ffn_pre_g), "norm_ffn_post_g": f32(norm_ffn_post_g),
        "lamv": np.concatenate([f32(lam_q1)[0], f32(lam_k1)[0], f32(lam_q2)[0], f32(lam_k2)[0]])[None, :],
        "diff_subln_g": f32(diff_subln_g)[0][:, None],
    }
    shared.update(consts)
    nc = build(2)
    in_maps = []
    for c in range(n):
        m = dict(shared)
        m["x"] = np.ascontiguousarray(x[2 * c:2 * c + 2])
        in_maps.append(m)
    res = run_bass_kernel_spmd(nc, in_maps, core_ids=list(range(n)))
    return np.concatenate([r["y"] for r in res.results], axis=0).astype(np.float32)
```

```python
import numpy as np
import ml_dtypes
import concourse.bass as bass
import concourse.mybir as mybir
from concourse.bass_utils import run_bass_kernel_spmd

F32 = mybir.dt.float32
BF16 = mybir.dt.bfloat16
AF = mybir.ActivationFunctionType
ALU = mybir.AluOpType
AX = mybir.AxisListType

T = 2048
D = 1024
DFF = 2816
NF = DFF // 128
NEGM = -30000.0


class Buf:
    __slots__ = ("name", "w", "r")

    def __init__(self, name):
        self.name = name
        self.w = None
        self.r = {}


class EngState:
    def __init__(self, name, eng, sem):
        self.name = name
        self.eng = eng
        self.sem = sem
        self.cnt = 0
        self.seen = {}


class Sync:
    def __init__(self, nc, ndma=20):
        self.nc = nc
        self.E = {}
        for name, e in (("pe", nc.tensor), ("act", nc.scalar), ("dve", nc.vector),
                        ("pool", nc.gpsimd), ("sp", nc.sync)):
            self.E[name] = EngState(name, e, nc.alloc_semaphore("s_" + name))
        self.dsem = {}
        self.dnext = {}
        for q in ("sp", "pool"):
            self.dsem[q] = [[nc.alloc_semaphore("d_%s%d" % (q, i)), 0] for i in range(ndma)]
            self.dnext[q] = 0
        self.semof = {}
        for name, st in self.E.items():
            self.semof[("c", name)] = st.sem
        for q in ("sp", "pool"):
            for i, (s, _) in enumerate(self.dsem[q]):
                self.semof[("d", q, i)] = s

    def _wait(self, E, deps):
        for key, n in deps.items():
            if key == ("c", "pe") and E.name == "pe":
                continue
            if E.seen.get(key, 0) >= n:
                continue
            E.eng.wait_ge(self.semof[key], n)
            E.seen[key] = n

    @staticmethod
    def _deps(reads, writes):
        deps = {}

        def add(key, n):
            if deps.get(key, 0) < n:
                deps[key] = n
        for b in reads:
            if b.w is not None:
                add(*b.w)
        for b in writes:
            if b.w is not None:
                add(*b.w)
            for key, n in b.r.items():
                add(key, n)
        return deps

    @staticmethod
    def _mark(ev, reads, writes):
        key, n = ev
        for b in reads:
            if b.r.get(key, 0) < n:
                b.r[key] = n
        for b in writes:
            b.w = ev
            b.r = {}

    def op(self, engname, fn, reads=(), writes=()):
        E = self.E[engname]
        self._wait(E, self._deps(reads, writes))
        ins = fn(E.eng)
        E.cnt += 1
        ins.then_inc(E.sem, 1)
        ev = (("c", engname), E.cnt)
        self._mark(ev, reads, writes)
        return ev

    def group(self, engname, fns, reads=(), writes=()):
        E = self.E[engname]
        self._wait(E, self._deps(reads, writes))
        ins = None
        for fn in fns:
            ins = fn(E.eng)
        E.cnt += 1
        ins.then_inc(E.sem, 1)
        ev = (("c", engname), E.cnt)
        self._mark(ev, reads, writes)
        return ev

    def split_group(self, engname, fns, reads, writes, per):
        E = self.E[engname]
        self._wait(E, self._deps(reads, writes))
        n = len(fns)
        for i, fn in enumerate(fns):
            ins = fn(E.eng)
            if i == n - 1:
                E.cnt += 1
                ins.then_inc(E.sem, 1)
                self._mark((("c", engname), E.cnt), reads, writes)
            elif (i + 1) % per == 0:
                yield

    def dma(self, q, out, in_, reads=(), writes=()):
        E = self.E[q]
        i = self.dnext[q]
        self.dnext[q] = (i + 1) % len(self.dsem[q])
        slot = self.dsem[q][i]
        key = ("d", q, i)
        deps = self._deps(reads, writes)
        if slot[1] > 0 and deps.get(key, 0) < slot[1]:
            deps[key] = slot[1]
        self._wait(E, deps)
        E.eng.dma_start(out=out, in_=in_).then_inc(slot[0], 16)
        slot[1] += 16
        ev = (key, slot[1])
        self._mark(ev, reads, writes)
        return ev

    def barrier(self, engines=("pe", "act", "dve", "pool", "sp")):
        deps = {}
        for name, st in self.E.items():
            if st.cnt > 0:
                deps[("c", name)] = st.cnt
        for q in ("sp", "pool"):
            for i, (s, n) in enumerate(self.dsem[q]):
                if n > 0:
                    deps[("d", q, i)] = n
        for name in engines:
            self._wait(self.E[name], deps)


class Arena:
    def __init__(self, nc, base, top):
        self.nc = nc
        self.ptr = (base + 31) // 32 * 32
        self.top = top
        self.n = 0

    def alloc(self, name, shape, dtype):
        nbytes = 2 if dtype == BF16 else 4
        sz = 1
        for s in shape[1:]:
            sz *= s
        sz *= nbytes
        off = self.ptr
        self.ptr = (off + sz + 31) // 32 * 32
        assert self.ptr <= self.top, ("SBUF overflow", name, self.ptr, self.top)
        self.n += 1
        return self.nc.alloc_sbuf_tensor_at("%s_%d" % (name, self.n), list(shape), dtype, offset=off)

    def mark(self):
        return self.ptr

    def reset(self, m):
        self.ptr = m


def _alibi_slopes():
    n = 12
    slopes = 2.0 ** (-8.0 * np.arange(1, n + 1) / n)
    diff_idx = np.arange(4) * 3 + 2
    moba_idx = np.setdiff1d(np.arange(n), diff_idx)
    return slopes[moba_idx].astype(np.float32), slopes[diff_idx].astype(np.float32)


def _host_consts():
    bf = ml_dtypes.bfloat16
    ms, ds = _alibi_slopes()
    slopes = np.concatenate([ms, ds]).astype(np.float32)
    pos = np.arange(T)
    hi = (256 * (pos // 256)).astype(np.float32)
    lo = (pos % 256).astype(np.float32)
    qx = np.zeros((12, 16, T), np.float32)
    kx = np.zeros((12, 16, T), np.float32)
    for h in range(12):
        c1 = np.float32(slopes[h]).astype(bf).astype(np.float32)
        c2 = np.float32(slopes[h] - c1).astype(bf).astype(np.float32)
        qx[h, 8], qx[h, 9], qx[h, 10], qx[h, 11] = hi, lo, hi, lo
        qx[h, 12], qx[h, 13], qx[h, 14], qx[h, 15] = c1, c1, c2, c2
        kx[h, 8], kx[h, 9], kx[h, 10], kx[h, 11] = -c1, -c1, -c2, -c2
        kx[h, 12], kx[h, 13], kx[h, 14], kx[h, 15] = hi, lo, hi, lo
        if h < 8:
            for n in range(8):
                kx[h, n] = (pos // 256 == n).astype(np.float32)
    ident = np.eye(128, dtype=np.float32)
    kk = np.arange(128)[:, None]
    qq = np.arange(128)[None, :]
    negtri = np.where(kk <= qq, 0.0, NEGM).astype(np.float32)
    pastmask = np.zeros((64, 8), np.float32)
    pairsum = np.zeros((64, 72), np.float32)
    for n in range(8):
        for m in range(8):
            p = n * 8 + m
            pairsum[p, 64 + n] = 1.0
            for qb in range(8):
                pastmask[p, qb] = 1.0 if m < qb else 0.0
    notown = np.zeros((128, 8), np.float32)
    for n in range(8):
        for qb in range(8):
            notown[64 + n, qb] = NEGM if n < qb else 0.0
    return {
        "c_qx": qx.astype(bf), "c_kx": kx.astype(bf), "c_ident": ident.astype(bf),
        "c_negtri": negtri.astype(bf), "c_pastmask": pastmask, "c_pairsum": pairsum.astype(bf),
        "c_notown": notown,
    }


def build(nseq=2, debug=False):
    nc = bass.Bass("TRN2", target_bir_lowering=False)
    S = Sync(nc)

    def din(name, shape, dt=F32):
        return nc.dram_tensor(name, list(shape), dt, kind="ExternalInput").ap()

    x = din("x", [nseq, T, D])
    w_in = din("w_in", [D, 5120])
    w_a = din("w_branch_a", [512, D])
    w_b = din("w_branch_b", [512, D])
    w_out = din("w_out", [D, D])
    w_gate = din("w_gate", [D, DFF])
    w_up = din("w_up", [D, DFF])
    w_down = din("w_down", [DFF, D])
    g_pre = din("norm_mix_pre_g", [1, D])
    g_post = din("norm_mix_post_g", [1, D])
    g_fpre = din("norm_ffn_pre_g", [1, D])
    g_fpost = din("norm_ffn_post_g", [1, D])
    lamv = din("lamv", [1, 256])
    g_sub = din("diff_subln_g", [128, 1])
    c_qx = din("c_qx", [12, 16, T], BF16)
    c_kx = din("c_kx", [12, 16, T], BF16)
    c_ident = din("c_ident", [128, 128], BF16)
    c_negtri = din("c_negtri", [128, 128], BF16)
    c_pastmask = din("c_pastmask", [64, 8])
    c_pairsum = din("c_pairsum", [64, 72], BF16)
    c_notown = din("c_notown", [128, 8])
    y = nc.dram_tensor("y", [nseq, T, D], F32, kind="ExternalOutput").ap()
    x1s = nc.dram_tensor("x1s", [nseq, T, D], F32).ap()
    dbg = {}
    if debug:
        dbg["hT"] = nc.dram_tensor("dbg_hT", [128, 8, T], BF16, kind="ExternalOutput").ap()
        dbg["yT"] = nc.dram_tensor("dbg_yT", [128, 8, T], BF16, kind="ExternalOutput").ap()
        dbg["mT"] = nc.dram_tensor("dbg_mT", [128, 8, T], BF16, kind="ExternalOutput").ap()

    w_in_r = w_in.rearrange("(kc p) n -> p kc n", p=128)
    w_in_r4 = w_in.rearrange("(kc p) (a n) -> p kc a n", p=128, n=128)
    w_a_r = w_a.rearrange("(kc p) n -> p kc n", p=128)
    w_b_r = w_b.rearrange("(kc p) n -> p kc n", p=128)
    w_out_r = w_out.rearrange("(kc p) n -> p kc n", p=128)
    w_gate_r = w_gate.rearrange("(kc p) n -> p kc n", p=128)
    w_up_r = w_up.rearrange("(kc p) n -> p kc n", p=128)
    w_down_r = w_down.rearrange("(f p) n -> p f n", p=128)

    _ms, _ds = _alibi_slopes()
    WIN = [min(15, int((100.0 / float(sl_) + 127.0) // 128)) for sl_ in list(_ms) + list(_ds)]
    ar = Arena(nc, nc.sbuf_base, nc.sbuf_top)
    ps = nc.alloc_psum_tensor("ps", [128, 8, 512], F32)
    PB = [Buf("bank%d" % i) for i in range(8)]

    ident = ar.alloc("ident", [128, 128], BF16)
    negtri = ar.alloc("negtri", [128, 128], BF16)
    ones_bf = ar.alloc("ones", [128, 128], BF16)
    pastmask = ar.alloc("pastmask", [64, 8], F32)
    pairsum = ar.alloc("pairsum", [64, 72], BF16)
    notown = ar.alloc("notown", [128, 8], F32)
    gsub = ar.alloc("gsub", [128, 1], F32)
    neglam = ar.alloc("neglam", [128, 1], F32)
    lamt = ar.alloc("lamt", [128, 256], F32)
    lamp = ar.alloc("lamp", [128, 128], F32)
    lams = ar.alloc("lams", [128, 4], F32)
    XIN = [ar.alloc("xin", [128, D], F32) for _ in range(2)]
    XINb = [Buf("xin%d" % i) for i in range(2)]
    HN = [ar.alloc("hn", [128, D], BF16) for _ in range(2)]
    HNb = [Buf("hn%d" % i) for i in range(2)]
    junk = ar.alloc("junk", [128, D], BF16)
    junkb = Buf("junk")
    SS = [ar.alloc("ss", [128, 4], F32) for _ in range(4)]
    SSb = [Buf("ss%d" % i) for i in range(4)]
    epsD = ar.alloc("epsD", [128, 1], F32)
    eps5 = ar.alloc("eps5", [128, 1], F32)
    TST = [ar.alloc("tst", [128, D], F32) for _ in range(2)]
    TSTb = [Buf("tst%d" % i) for i in range(2)]
    cb = Buf("consts")

    S.dma("sp", ident[:], c_ident, writes=[cb])
    S.dma("sp", negtri[:], c_negtri, writes=[cb])
    S.dma("sp", pastmask[:], c_pastmask, writes=[cb])
    S.dma("sp", pairsum[:], c_pairsum, writes=[cb])
    S.dma("sp", notown[:], c_notown, writes=[cb])
    S.dma("sp", gsub[:], g_sub, writes=[cb])
    S.dma("sp", lamt[:], lamv.partition_broadcast(128), writes=[cb])
    S.op("dve", lambda e: e.memset(ones_bf[:], 1.0), writes=[cb])
    S.op("dve", lambda e: e.memset(epsD[:], 1e-6), writes=[cb])
    S.op("dve", lambda e: e.memset(eps5[:], 1e-5), writes=[cb])
    lamtv = lamt[:].rearrange("p (a b d) -> p a b d", a=2, b=2)
    S.op("dve", lambda e: e.tensor_tensor(out=lamp[:].rearrange("p (a d) -> p a d", a=2),
                                          in0=lamtv[:, :, 0, :], in1=lamtv[:, :, 1, :], op=ALU.mult),
         reads=[cb], writes=[cb])
    S.op("dve", lambda e: e.tensor_reduce(out=lams[:, 0:2], in_=lamp[:].rearrange("p (a d) -> p a d", a=2),
                                          axis=AX.X, op=ALU.add), reads=[cb], writes=[cb])
    S.op("act", lambda e: e.activation(out=lams[:, 2:4], in_=lams[:, 0:2], func=AF.Exp), reads=[cb], writes=[cb])
    S.op("dve", lambda e: e.scalar_tensor_tensor(out=neglam[:], in0=lams[:, 3:4], scalar=-0.2, in1=lams[:, 2:3],
                                                 op0=ALU.add, op1=ALU.subtract), reads=[cb], writes=[cb])
    S.op("dve", lambda e: e.tensor_scalar(out=gsub[:], in0=gsub[:], scalar1=0.8, scalar2=None, op0=ALU.mult),
         reads=[cb], writes=[cb])

    base_mark = ar.mark()

    NSS = 4

    def norm_A(src_ap, srcb, k):
        ss, ssb = SS[k % NSS], SSb[k % NSS]
        S.op("act", lambda e: e.memzero(ss[:]), writes=[ssb])
        S.op("act", lambda e: e.activation(out=junk[:], in_=src_ap, func=AF.Square, accum_out=ss[:, 0:1]),
             reads=srcb, writes=[junkb, ssb])
        S.op("act", lambda e: e.activation(out=ss[:, 1:2], in_=ss[:, 0:1], func=AF.Ln, scale=1.0 / D,
                                           bias=epsD[:, 0:1]), reads=[ssb, cb], writes=[ssb])
        S.op("act", lambda e: e.activation(out=ss[:, 1:2], in_=ss[:, 1:2], func=AF.Exp, scale=-0.5),
             reads=[ssb], writes=[ssb])

    def norm_B(src_ap, srcb, gt, gtb, k, bank):
        ss, ssb = SS[k % NSS], SSb[k % NSS]
        hn, hnb = HN[k % 2], HNb[k % 2]
        S.op("dve", lambda e: e.scalar_tensor_tensor(out=hn[:], in0=src_ap, scalar=ss[:, 1:2], in1=gt[:],
                                                     op0=ALU.mult, op1=ALU.mult),
             reads=list(srcb) + [ssb, gtb], writes=[hnb])
        psb = ps[:, bank, :].bitcast(BF16)
        S.group("pe", [(lambda e, kc=kc: e.transpose(out=psb[:, kc * 128:(kc + 1) * 128],
                                                      in_=hn[:, kc * 128:(kc + 1) * 128], identity=ident[:]))
                       for kc in range(8)], reads=[hnb, cb], writes=[PB[bank]])

    def norm_C(dstT, dstb, tcol, bank):
        psb = ps[:, bank, :].bitcast(BF16)
        S.op("act", lambda e: e.activation(out=dstT[:, :, tcol:tcol + 128],
                                           in_=psb.rearrange("p (k t) -> p k t", k=8), func=AF.Copy),
             reads=[PB[bank]], writes=[dstb])

    def norm_transpose_pipeline(n, src_of, gt, gtb, dstT, dstb, banks):
        srcs = {}

        def A(k):
            srcs[k] = src_of(k)
            norm_A(srcs[k][0], srcs[k][1], k)

        def B(k):
            norm_B(srcs[k][0], srcs[k][1], gt, gtb, k, banks[k % len(banks)])

        def C(k):
            norm_C(dstT, dstb, k * 128, banks[k % len(banks)])
        A(0)
        for k in range(n):
            if k + 1 < n:
                A(k + 1)
            B(k)
            if k >= 1:
                C(k - 1)
        C(n - 1)

    def post_A(banks, k):
        ss, ssb = SS[k % NSS], SSb[k % NSS]
        S.op("act", lambda e: e.memzero(ss[:]), writes=[ssb])
        S.op("act", lambda e: e.activation(out=junk[:], in_=ps[:, banks[0]:banks[0] + 2, :].rearrange("p b n -> p (b n)"),
                                           func=AF.Square, accum_out=ss[:, 0:1]),
             reads=[PB[banks[0]], PB[banks[1]]], writes=[junkb, ssb])
        S.op("act", lambda e: e.activation(out=ss[:, 1:2], in_=ss[:, 0:1], func=AF.Ln, scale=1.0 / D,
                                           bias=epsD[:, 0:1]), reads=[ssb, cb], writes=[ssb])
        S.op("act", lambda e: e.activation(out=ss[:, 1:2], in_=ss[:, 1:2], func=AF.Exp, scale=-0.5),
             reads=[ssb], writes=[ssb])

    def post_B(banks, gt, gtb, resid_ap, residb, k, out_dram):
        ss, ssb = SS[k % NSS], SSb[k % NSS]
        tst, tstb = TST[k % 2], TSTb[k % 2]
        for hf in range(2):
            S.op("dve", lambda e, hf=hf: e.scalar_tensor_tensor(
                out=tst[:, hf * 512:(hf + 1) * 512], in0=ps[:, banks[hf], :], scalar=ss[:, 1:2],
                in1=gt[:, hf * 512:(hf + 1) * 512], op0=ALU.mult, op1=ALU.mult),
                reads=[PB[banks[hf]], ssb, gtb], writes=[tstb])
        S.op("dve", lambda e: e.tensor_tensor(out=tst[:], in0=tst[:], in1=resid_ap, op=ALU.add),
             reads=[tstb] + list(residb), writes=[tstb])
        S.dma("sp", out_dram, tst[:], reads=[tstb])

    for seq in range(nseq):
        ar.reset(base_mark)
        hT = ar.alloc("hT", [128, 8, T], BF16)
        hTb = Buf("hT")
        yT = ar.alloc("yT", [128, 8, T], BF16)
        yTb = Buf("yT")
        a_mark = ar.mark()
        gpre = ar.alloc("gpre", [128, D], F32)
        gpreb = Buf("gpre")
        QK = [[ar.alloc("qk", [128, T], BF16) for _ in range(4)] for _ in range(2)]
        QKb = [[Buf("qk") for _ in range(4)] for _ in range(2)]
        VV = [ar.alloc("vv", [128, 16, 320], BF16) for _ in range(2)]
        VVb = [Buf("vv") for _ in range(2)]
        WSL = [ar.alloc("wsl", [128, 8, 384], BF16) for _ in range(2)]
        WSLb = [Buf("wsl") for _ in range(2)]
        NPB = 5
        PBF = [ar.alloc("pbf", [128, 512], BF16) for _ in range(NPB)]
        PBFb = [Buf("pbf") for _ in range(NPB)]
        ksum = [ar.alloc("ksum", [64, 8], F32) for _ in range(2)]
        diffw = [ar.alloc("diffw", [64, 64], BF16) for _ in range(2)]
        ind = [ar.alloc("ind", [64, 512], BF16) for _ in range(2)]
        gateb = [Buf("gate") for _ in range(2)]
        indb = [Buf("ind") for _ in range(2)]
        rscr = [ar.alloc("rscr", [128, 512], F32) for _ in range(2)]
        rbuf = [ar.alloc("rbuf", [128, 512], F32) for _ in range(2)]
        rbufb = [Buf("rbuf") for _ in range(2)]
        dL1 = [ar.alloc("dL1", [128, 512], F32) for _ in range(2)]
        dL2 = [ar.alloc("dL2", [128, 512], F32) for _ in range(2)]
        dO1 = [ar.alloc("dO1", [128, 512], F32) for _ in range(2)]
        dO2 = [ar.alloc("dO2", [128, 512], F32) for _ in range(2)]
        dSQ = [ar.alloc("dSQ", [128, 512], BF16) for _ in range(2)]
        dRS = [ar.alloc("dRS", [128, 512], F32) for _ in range(2)]
        dfb = [Buf("dfin") for _ in range(2)]

        S.dma("sp", gpre[:], g_pre.partition_broadcast(128), writes=[gpreb])
        for sl in range(2):
            for i in range(4):
                S.op("dve", lambda e, sl=sl, i=i: e.memset(QK[sl][i][:], 0.0), writes=[QKb[sl][i]])
            S.op("dve", lambda e, sl=sl: e.memset(VV[sl][:], 1.0), writes=[VVb[sl]])

        def p1_src(tt):
            xin, xinb = XIN[tt % 2], XINb[tt % 2]
            S.dma("sp", xin[:], x[seq, tt * 128:(tt + 1) * 128, :], writes=[xinb])
            return xin[:], [xinb]
        norm_transpose_pipeline(16, p1_src, gpre, gpreb, hT, hTb, (6, 7))
        if debug and seq == 0:
            S.dma("sp", dbg["hT"], hT[:], reads=[hTb])

        def load_group_weights(g):
            sl = g % 2
            a0 = g if g < 4 else 12 + (g - 4)
            for i3 in range(3):
                S.dma("pool", WSL[sl][:, :, i3 * 128:(i3 + 1) * 128], w_in_r4[:, :, a0 + 4 * i3, :],
                      writes=[WSLb[sl]])
            hA, hB = (2 * g, 2 * g + 1) if g < 4 else (8 + g - 4, 8 + g - 4)
            q = QK[sl]
            qb_ = QKb[sl]
            S.dma("sp", q[0][64:80, :], c_qx[hA], writes=[qb_[0]])
            S.dma("sp", q[1][64:80, :], c_qx[hB], writes=[qb_[1]])
            S.dma("sp", q[2][64:80, :], c_kx[hA], writes=[qb_[2]])
            S.dma("sp", q[3][64:80, :], c_kx[hB], writes=[qb_[3]])

        pbk = {"i": 0}

        pbk["set"] = (7, 5, 6)

        def pbank():
            pbk["i"] += 1
            st = pbk["set"]
            return st[pbk["i"] % len(st)]

        def prep_units(g):
            sl = g % 2
            q, qb_ = QK[sl], QKb[sl]
            w, wb = WSL[sl], WSLb[sl]
            load_group_weights(g)
            yield
            for c in range(4):
                cs = slice(c * 512, (c + 1) * 512)
                bank = pbank()
                yield from S.split_group(
                    "pe", [(lambda e, kc=kc: e.matmul(ps[:, bank, :], lhsT=w[:, kc, 0:128], rhs=hT[:, kc, cs],
                                                      start=(kc == 0), stop=(kc == 7))) for kc in range(8)],
                    [wb, hTb], [PB[bank]], 2)
                S.op("dve", lambda e: e.tensor_scalar(out=q[0][0:64, cs], in0=ps[0:64, bank, :], scalar1=0.125,
                                                      scalar2=None, op0=ALU.mult),
                     reads=[PB[bank]], writes=[qb_[0]])
                S.op("dve", lambda e: e.tensor_scalar(out=q[1][0:64, cs], in0=ps[64:128, bank, :], scalar1=0.125,
                                                      scalar2=None, op0=ALU.mult),
                     reads=[PB[bank]], writes=[qb_[1]])
                yield
                bank = pbank()
                yield from S.split_group(
                    "pe", [(lambda e, kc=kc: e.matmul(ps[:, bank, :], lhsT=w[:, kc, 128:256], rhs=hT[:, kc, cs],
                                                      start=(kc == 0), stop=(kc == 7))) for kc in range(8)],
                    [wb, hTb], [PB[bank]], 2)
                S.op("dve", lambda e: e.tensor_copy(out=q[2][0:64, cs], in_=ps[0:64, bank, :]),
                     reads=[PB[bank]], writes=[qb_[2]])
                S.op("dve", lambda e: e.tensor_copy(out=q[3][0:64, cs], in_=ps[64:128, bank, :]),
                     reads=[PB[bank]], writes=[qb_[3]])
                yield
            vv, vvb = VV[sl], VVb[sl]
            for tg in range(4):
                bank = pbank()
                fns = []
                for i in range(4):
                    tt = 4 * tg + i
                    for kc in range(8):
                        fns.append(lambda e, i=i, tt=tt, kc=kc: e.matmul(
                            ps[:, bank, i * 128:(i + 1) * 128], lhsT=hT[:, kc, tt * 128:(tt + 1) * 128],
                            rhs=w[:, kc, 256:384], start=(kc == 0), stop=(kc == 7)))
                yield from S.split_group("pe", fns, [wb, hTb], [PB[bank]], 4)
                if g < 4:
                    vq = vv[:].rearrange("p t (a d) -> p t a d", d=64)
                    S.op("dve", lambda e: e.tensor_copy(
                        out=vq[:, 4 * tg:4 * tg + 4, 0:3:2, :],
                        in_=ps[:, bank, :].rearrange("p (t h d) -> p t h d", t=4, h=2)),
                        reads=[PB[bank]], writes=[vvb])
                else:
                    S.op("dve", lambda e: e.tensor_copy(
                        out=vv[:, 4 * tg:4 * tg + 4, 192:320],
                        in_=ps[:, bank, :].rearrange("p (t d) -> p t d", t=4)),
                        reads=[PB[bank]], writes=[vvb])
                yield
            if g < 4:
                for hx in range(2):
                    for _ in moba_gate(g, hx):
                        yield

        def moba_gate(g, hx):
            sl = g % 2
            qt, qtb = QK[sl][hx], QKb[sl][hx]
            kt_, ktb = QK[sl][2 + hx], QKb[sl][2 + hx]
            ks, dw, gb_ = ksum[hx], diffw[hx], gateb[hx]
            S.op("dve", lambda e: e.tensor_reduce(out=ks[:], in_=kt_[0:64, :].rearrange("p (n k) -> p n k", k=256),
                                                  axis=AX.X, op=ALU.add), reads=[ktb], writes=[gb_])
            for n in range(8):
                S.op("dve", lambda e, n=n: e.tensor_scalar(out=dw[:, n * 8:(n + 1) * 8], in0=ks[:, 0:8],
                                                           scalar1=ks[:, n:n + 1], scalar2=None,
                                                           op0=ALU.subtract), reads=[gb_], writes=[gb_])
            yield
            for c in (2, 3):
                cs = slice(c * 512, (c + 1) * 512)
                bank = pbank()
                S.group("pe", [lambda e: e.matmul(ps[0:64, bank, :], lhsT=dw[:, :], rhs=qt[0:64, cs],
                                                  start=True, stop=True)],
                        reads=[gb_, qtb], writes=[PB[bank]])
                for hh in range(2):
                    qblk = 2 * c + hh
                    hs = slice(hh * 256, (hh + 1) * 256)
                    S.op("dve", lambda e, hs=hs, qblk=qblk: e.tensor_scalar(
                        out=ind[hx][:, hs], in0=ps[0:64, bank, hs], scalar1=0.0,
                        scalar2=pastmask[:, qblk:qblk + 1], op0=ALU.is_gt, op1=ALU.mult),
                        reads=[PB[bank], cb], writes=[indb[hx]])
                yield
                bank = pbank()
                S.group("pe", [lambda e: e.matmul(ps[0:72, bank, :], lhsT=pairsum[:, :], rhs=ind[hx][:, :],
                                                  start=True, stop=True)],
                        reads=[indb[hx], cb], writes=[PB[bank]])
                for hh in range(2):
                    qblk = 2 * c + hh
                    hs = slice(hh * 256, (hh + 1) * 256)
                    S.op("dve", lambda e, hs=hs, qblk=qblk: e.tensor_scalar(
                        out=qt[64:72, c * 512 + hh * 256:c * 512 + (hh + 1) * 256], in0=ps[64:72, bank, hs],
                        scalar1=2.5, scalar2=notown[64:72, qblk:qblk + 1], op0=ALU.is_gt, op1=ALU.mult),
                        reads=[PB[bank], cb], writes=[qtb])
                yield

        state = {"s": 0, "p": 0, "la": 2, "sb": (0, 1, 2), "facc": 0.0}

        def tile_cols(c, W):
            out = []
            for kt in range(max(0, 4 * c - W), 4 * c + 4):
                qlo = max(kt, 4 * c)
                qhi = min(4 * c + 3, kt + W)
                if qhi < qlo:
                    continue
                out.append((kt, (128 * (qlo - 4 * c), 128 * (qhi - 4 * c + 1))))
            return out
        pending = []

        def defer(n, fn):
            pending.append([n, fn])

        def run_pending(flush=False):
            while True:
                ready = None
                for ent in pending:
                    if flush or ent[0] <= 0:
                        ready = ent
                        break
                if ready is None:
                    break
                pending.remove(ready)
                ready[1]()
            for ent in pending:
                ent[0] -= 1

        def attention(items, finalize, filler, every):
            n = len(items)
            sb_of = {}

            def qk(i):
                it = items[i]
                sbank = state["sb"][state["s"] % len(state["sb"])]
                state["s"] += 1
                sb_of[i] = sbank
                c, kt = it["c"], it["kt"]
                j = kt - 4 * c
                col0, col1 = it["cols"]
                fns = [lambda e: e.matmul(ps[:, sbank, col0:col1], lhsT=it["k"][0:80, kt * 128:(kt + 1) * 128],
                                          rhs=it["q"][0:80, c * 512 + col0:c * 512 + col1],
                                          start=True, stop=(j < 0))]
                if j >= 0:
                    fns.append(lambda e: e.matmul(ps[:, sbank, col0:col0 + 128], lhsT=ident[:], rhs=negtri[:],
                                                  start=False, stop=True))
                S.group("pe", fns, reads=[it["kb"], it["qb"], cb], writes=[PB[sbank]])

            def ex_pv(i):
                it = items[i]
                sbank = sb_of[i]
                pi = state["p"] % NPB
                state["p"] += 1
                col0, col1 = it["cols"]
                win = it["win"]
                S.op("act", lambda e: e.activation(out=PBF[pi][:, col0:col1], in_=ps[:, sbank, col0:col1], func=AF.Exp),
                     reads=[PB[sbank]], writes=[PBFb[pi]])
                fns = []
                banks = []
                rd = [PBFb[pi]]
                for (bank, lap, lbuf) in it["pv"]:
                    if win and it["first"]:
                        S.op("dve", lambda e, bank=bank: e.memset(ps[:, bank, :], 0.0), writes=[PB[bank]])
                    if win:
                        fns.append(lambda e, bank=bank, lap=lap: e.matmul(
                            ps[:, bank, col0:col1], lhsT=lap, rhs=PBF[pi][:, col0:col1],
                            start=False, stop=it["last"], skip_group_check=True))
                    else:
                        fns.append(lambda e, bank=bank, lap=lap: e.matmul(
                            ps[:, bank, col0:col1], lhsT=lap, rhs=PBF[pi][:, col0:col1],
                            start=it["first"], stop=it["last"]))
                    banks.append(PB[bank])
                    rd.append(lbuf)
                S.group("pe", fns, reads=rd, writes=banks)
                if it["last"]:
                    finalize(it)

            la = state["la"]
            for i in range(min(la, n)):
                qk(i)
            for i in range(n):
                if i + la < n:
                    qk(i + la)
                ex_pv(i)
                run_pending()
                if filler is not None:
                    state["facc"] += every
                    while state["facc"] >= 1.0:
                        state["facc"] -= 1.0
                        next(filler, None)

        def recip(out_ap, in_ap, scr_ap, reads, wb_, on_act=False):
            if on_act:
                S.op("act", lambda e: e.activation(out=scr_ap, in_=in_ap, func=AF.Ln), reads=reads, writes=[wb_])
                S.op("act", lambda e: e.activation(out=out_ap, in_=scr_ap, func=AF.Exp, scale=-1.0),
                     reads=[wb_], writes=[wb_])
            else:
                S.op("dve", lambda e: e.reciprocal(out=out_ap, in_=in_ap), reads=reads, writes=[wb_])

        def run_moba(g, filler):
            sl = g % 2
            nit = sum(len(tile_cols(c, WIN[2 * g + hx])) for hx in range(2) for c in range(4))
            rate = 82.0 / nit
            for hx in range(2):
                items = []
                W = WIN[2 * g + hx]
                for c in range(4):
                    bank = 3 + ((hx * 4 + c) % 2)
                    lap_of = (lambda kt: VV[sl][:, kt, 0:128]) if hx == 0 else (lambda kt: VV[sl][:, kt, 64:192])
                    first = True
                    for kt, cols in tile_cols(c, W):
                        items.append(dict(q=QK[sl][hx], qb=QKb[sl][hx], k=QK[sl][2 + hx], kb=QKb[sl][2 + hx],
                                          c=c, kt=kt, pv=[(bank, lap_of(kt), VVb[sl])], last=(kt == 4 * c + 3),
                                          bank=bank, cols=cols, first=first, win=(W < 15)))
                        first = False

                def fin(it, hx=hx):
                    defer(3, lambda: fin_body(it, hx))

                def fin_body(it, hx):
                    bank, c = it["bank"], it["c"]
                    cs = slice(c * 512, (c + 1) * 512)
                    rb, rbb = rbuf[c % 2], rbufb[c % 2]
                    rs_ = rscr[c % 2]
                    if hx == 0:
                        recip(rb[0:64, :], ps[64:128, bank, :], rs_[0:64, :], [PB[bank]], rbb, on_act=True)
                        S.op("dve", lambda e: e.tensor_tensor(out=yT[0:64, g, cs], in0=ps[0:64, bank, :],
                                                              in1=rb[0:64, :], op=ALU.mult),
                             reads=[PB[bank], rbb], writes=[yTb])
                    else:
                        recip(rb[64:128, :], ps[0:64, bank, :], rs_[64:128, :], [PB[bank]], rbb, on_act=True)
                        S.op("dve", lambda e: e.tensor_tensor(out=yT[64:128, g, cs], in0=ps[64:128, bank, :],
                                                              in1=rb[64:128, :], op=ALU.mult),
                             reads=[PB[bank], rbb], writes=[yTb])
                attention(items, fin, filler, rate)

        def run_diff(g, filler):
            sl = g % 2
            j = g - 4
            items = []
            W = WIN[8 + j]
            for c in range(4):
                first = True
                for kt, cols in tile_cols(c, W):
                    for mp in range(2):
                        items.append(dict(q=QK[sl][mp], qb=QKb[sl][mp], k=QK[sl][2 + mp], kb=QKb[sl][2 + mp],
                                          c=c, kt=kt,
                                          pv=[(3 + mp, VV[sl][:, kt, 192:320], VVb[sl]),
                                              (5 + mp, ones_bf[:], cb)],
                                          last=(kt == 4 * c + 3), mp=mp, cols=cols, first=first, win=(W < 15)))
                    first = False

            def fin(it):
                if it["mp"] == 0:
                    return
                run_pending(flush=True)
                c = it["c"]
                cs = slice(c * 512, (c + 1) * 512)
                k2 = c % 2
                L1, L2, O1, O2, SQ, RS, fb = dL1[k2], dL2[k2], dO1[k2], dO2[k2], dSQ[k2], dRS[k2], dfb[k2]
                S.op("dve", lambda e: e.tensor_copy(out=L1[:], in_=ps[:, 5, :]), reads=[PB[5]], writes=[fb])
                S.op("dve", lambda e: e.tensor_copy(out=O1[:], in_=ps[:, 3, :]), reads=[PB[3]], writes=[fb])
                S.op("dve", lambda e: e.tensor_copy(out=L2[:], in_=ps[:, 6, :]), reads=[PB[6]], writes=[fb])
                S.op("dve", lambda e: e.tensor_copy(out=O2[:], in_=ps[:, 4, :]), reads=[PB[4]], writes=[fb])

                def stage_b():
                    S.op("act", lambda e: e.activation(out=L1[:], in_=L1[:], func=AF.Ln), reads=[fb], writes=[fb])
                    S.op("act", lambda e: e.activation(out=L1[:], in_=L1[:], func=AF.Exp, scale=-1.0),
                         reads=[fb], writes=[fb])
                    S.op("act", lambda e: e.activation(out=L2[:], in_=L2[:], func=AF.Ln), reads=[fb], writes=[fb])
                    S.op("act", lambda e: e.activation(out=L2[:], in_=L2[:], func=AF.Exp, scale=-1.0),
                         reads=[fb], writes=[fb])
                    S.op("dve", lambda e: e.tensor_tensor(out=O1[:], in0=O1[:], in1=L1[:], op=ALU.mult),
                         reads=[fb], writes=[fb])
                    S.op("dve", lambda e: e.tensor_tensor(out=O2[:], in0=O2[:], in1=L2[:], op=ALU.mult),
                         reads=[fb], writes=[fb])
                    S.op("dve", lambda e: e.scalar_tensor_tensor(out=O1[:], in0=O2[:], scalar=neglam[:, 0:1],
                                                                 in1=O1[:], op0=ALU.mult, op1=ALU.add),
                         reads=[fb, cb], writes=[fb])
                    S.op("dve", lambda e: e.tensor_tensor(out=SQ[:], in0=O1[:], in1=O1[:], op=ALU.mult),
                         reads=[fb], writes=[fb])
                    defer(4, stage_c)

                def stage_c():
                    bank = pbank()
                    S.group("pe", [lambda e: e.matmul(ps[:, bank, :], lhsT=ones_bf[:], rhs=SQ[:], start=True,
                                                      stop=True)], reads=[fb, cb], writes=[PB[bank]])
                    S.op("act", lambda e: e.activation(out=RS[:], in_=ps[:, bank, :], func=AF.Ln, scale=1.0 / 128,
                                                       bias=eps5[:, 0:1]), reads=[PB[bank], cb], writes=[fb])
                    S.op("act", lambda e: e.activation(out=RS[:], in_=RS[:], func=AF.Exp, scale=-0.5),
                         reads=[fb], writes=[fb])
                    S.op("dve", lambda e: e.scalar_tensor_tensor(out=yT[:, 4 + j, cs], in0=O1[:], scalar=gsub[:, 0:1],
                                                                 in1=RS[:], op0=ALU.mult, op1=ALU.mult),
                         reads=[fb, cb], writes=[yTb])
                defer(3, stage_b)
            attention(items, fin, filler, 82.0 / len(items))

        for _ in prep_units(0):
            pass
        for g in range(8):
            filler = prep_units(g + 1) if g + 1 < 8 else None
            if g < 4:
                pbk["set"] = (7, 5, 6)
                state["la"], state["sb"] = 2, (0, 1, 2)
                run_moba(g, filler)
            else:
                pbk["set"] = (2, 7)
                state["la"], state["sb"] = 1, (0, 1)
                run_diff(g, filler)
            if filler is not None:
                for _ in filler:
                    pass
            run_pending(flush=True)
        if debug and seq == 0:
            S.dma("sp", dbg["yT"], yT[:], reads=[yTb])

        S.barrier()
        ar.reset(a_mark)
        mT = ar.alloc("mT", [128, 8, T], BF16)
        mTb = Buf("mT")
        woT = ar.alloc("woT", [128, 8, D], BF16)
        woTb = Buf("woT")
        WM = [ar.alloc("wm", [128, 24, 128], BF16) for _ in range(2)]
        WMb = [Buf("wm") for _ in range(2)]
        gpost = ar.alloc("gpost", [128, D], F32)
        gpostb = Buf("gpost")
        SA = [ar.alloc("sa", [128, 512], F32) for _ in range(2)]
        SB_ = [ar.alloc("sb", [128, 512], F32) for _ in range(2)]
        TA = [ar.alloc("ta", [128, 512], F32) for _ in range(2)]
        TB = [ar.alloc("tb", [128, 512], F32) for _ in range(2)]
        MSb = [Buf("ms") for _ in range(2)]

        def load_merge_weights(j):
            sl = j % 2
            js = slice(128 * j, 128 * j + 128)
            S.dma("pool", WM[sl][:, 0:4, :], w_a_r[:, :, js], writes=[WMb[sl]])
            S.dma("pool", WM[sl][:, 4:8, :], w_b_r[:, :, js], writes=[WMb[sl]])
            S.dma("pool", WM[sl][:, 8:16, :], w_in_r[:, :, 3072 + 128 * j:3072 + 128 * j + 128], writes=[WMb[sl]])
            S.dma("pool", WM[sl][:, 16:24, :], w_in_r[:, :, 4096 + 128 * j:4096 + 128 * j + 128], writes=[WMb[sl]])

        S.dma("sp", gpost[:], g_post.partition_broadcast(128), writes=[gpostb])
        load_merge_weights(0)
        load_merge_weights(1)
        for hf in range(2):
            S.dma("pool", woT[:, 4 * hf:4 * hf + 4, :], w_out_r[:, 4 * hf:4 * hf + 4, :], writes=[woTb])
        it_ = 0
        for j in range(8):
            sl = j % 2
            wm, wmb = WM[sl], WMb[sl]
            for c in range(4):
                cs = slice(c * 512, (c + 1) * 512)
                b0 = 4 * (it_ % 2)
                k2 = it_ % 2
                it_ += 1
                S.group("pe", [(lambda e, kc=kc: e.matmul(ps[:, b0, :], lhsT=wm[:, kc, :], rhs=yT[:, kc, cs],
                                                           start=(kc == 0), stop=(kc == 3))) for kc in range(4)],
                        reads=[wmb, yTb], writes=[PB[b0]])
                S.group("pe", [(lambda e, kc=kc: e.matmul(ps[:, b0 + 1, :], lhsT=wm[:, 4 + kc, :],
                                                           rhs=yT[:, 4 + kc, cs],
                                                           start=(kc == 0), stop=(kc == 3))) for kc in range(4)],
                        reads=[wmb, yTb], writes=[PB[b0 + 1]])
                S.group("pe", [(lambda e, kc=kc: e.matmul(ps[:, b0 + 2, :], lhsT=wm[:, 8 + kc, :], rhs=hT[:, kc, cs],
                                                           start=(kc == 0), stop=(kc == 7))) for kc in range(8)],
                        reads=[wmb, hTb], writes=[PB[b0 + 2]])
                S.group("pe", [(lambda e, kc=kc: e.matmul(ps[:, b0 + 3, :], lhsT=wm[:, 16 + kc, :], rhs=hT[:, kc, cs],
                                                           start=(kc == 0), stop=(kc == 7))) for kc in range(8)],
                        reads=[wmb, hTb], writes=[PB[b0 + 3]])
                S.op("act", lambda e: e.activation(out=SA[k2][:], in_=ps[:, b0 + 2, :], func=AF.Sigmoid),
                     reads=[PB[b0 + 2]], writes=[MSb[k2]])
                S.op("act", lambda e: e.activation(out=SB_[k2][:], in_=ps[:, b0 + 3, :], func=AF.Sigmoid),
                     reads=[PB[b0 + 3]], writes=[MSb[k2]])
                S.op("dve", lambda e: e.tensor_tensor(out=TA[k2][:], in0=ps[:, b0, :], in1=SA[k2][:], op=ALU.mult),
                     reads=[PB[b0], MSb[k2]], writes=[MSb[k2]])
                S.op("dve", lambda e: e.tensor_tensor(out=TB[k2][:], in0=ps[:, b0 + 1, :], in1=SB_[k2][:],
                                                      op=ALU.mult),
                     reads=[PB[b0 + 1], MSb[k2]], writes=[MSb[k2]])
                S.op("dve", lambda e: e.tensor_tensor(out=mT[:, j, cs], in0=TA[k2][:], in1=TB[k2][:], op=ALU.add),
                     reads=[MSb[k2]], writes=[mTb])
            if j + 2 < 8:
                load_merge_weights(j + 2)
        if debug and seq == 0:
            S.dma("sp", dbg["mT"], mT[:], reads=[mTb])
        for tt in range(16):
            ts_ = slice(tt * 128, (tt + 1) * 128)
            banks = (2 * (tt % 2), 2 * (tt % 2) + 1)
            xin, xinb = XIN[tt % 2], XINb[tt % 2]
            S.dma("sp", xin[:], x[seq, ts_, :], writes=[xinb])
            for hf in range(2):
                S.group("pe", [(lambda e, kc=kc: e.matmul(ps[:, banks[hf], :], lhsT=mT[:, kc, ts_],
                                                           rhs=woT[:, kc, hf * 512:(hf + 1) * 512],
                                                           start=(kc == 0), stop=(kc == 7))) for kc in range(8)],
                        reads=[mTb, woTb], writes=[PB[banks[hf]]])
            post_A(banks, tt)
            post_B(banks, gpost, gpostb, xin[:], [xinb], tt, x1s[seq, ts_, :])

        S.barrier()
        ar.reset(base_mark)
        X1 = ar.alloc("X1", [128, 8, D], F32)
        X1b = [Buf("x1_%d" % i) for i in range(8)]
        h2T = ar.alloc("h2T", [128, 8, 1024], BF16)
        h2Tb = Buf("h2T")
        actT = ar.alloc("actT", [128, NF, 1024], BF16)
        actTb = Buf("actT")
        Wd = ar.alloc("Wd", [128, NF, D], BF16)
        Wdb = Buf("Wd")
        WGU = [ar.alloc("wgu", [128, 8, 512], BF16) for _ in range(2)]
        WGUb = [Buf("wgu") for _ in range(2)]
        gfpre = ar.alloc("gfpre", [128, D], F32)
        gfpost = ar.alloc("gfpost", [128, D], F32)
        gfb = Buf("gf")
        SG = [ar.alloc("sg", [128, 512], F32) for _ in range(2)]
        SGb = [Buf("sg") for _ in range(2)]

        S.dma("sp", gfpre[:], g_fpre.partition_broadcast(128), writes=[gfb])
        S.dma("sp", gfpost[:], g_fpost.partition_broadcast(128), writes=[gfb])

        def load_gu(fp):
            sl = fp % 2
            S.dma("pool", WGU[sl][:, :, 0:256], w_gate_r[:, :, 256 * fp:256 * fp + 256], writes=[WGUb[sl]])
            S.dma("pool", WGU[sl][:, :, 256:512], w_up_r[:, :, 256 * fp:256 * fp + 256], writes=[WGUb[sl]])

        nfp = NF // 2
        gu_loaded = [0]
        for half in range(2):
            if half == 0:
                load_gu(0)
                load_gu(1)
                for f2 in range(nfp):
                    S.dma("pool", Wd[:, 2 * f2:2 * f2 + 2, :], w_down_r[:, 2 * f2:2 * f2 + 2, :], writes=[Wdb])
            def p4_src(t8, half=half):
                tt = half * 8 + t8
                S.dma("sp", X1[:, t8, :], x1s[seq, tt * 128:(tt + 1) * 128, :], writes=[X1b[t8]])
                return X1[:, t8, :], [X1b[t8]]
            norm_transpose_pipeline(8, p4_src, gfpre, gfb, h2T, h2Tb, (6, 7))
            it_ = 0
            for fp in range(nfp):
                sl = fp % 2
                wg, wgb = WGU[sl], WGUb[sl]
                if half == 1 and fp == 0:
                    load_gu(0)
                    load_gu(1)
                for fi in range(2):
                    f = 2 * fp + fi
                    for c2 in range(2):
                        cs = slice(c2 * 512, (c2 + 1) * 512)
                        b0 = 2 * (it_ % 3)
                        k2 = it_ % 2
                        it_ += 1
                        S.group("pe", [(lambda e, kc=kc: e.matmul(ps[:, b0, :], lhsT=wg[:, kc, fi * 128:(fi + 1) * 128],
                                                                   rhs=h2T[:, kc, cs], start=(kc == 0), stop=(kc == 7)))
                                       for kc in range(8)], reads=[wgb, h2Tb], writes=[PB[b0]])
                        S.group("pe", [(lambda e, kc=kc: e.matmul(ps[:, b0 + 1, :],
                                                                   lhsT=wg[:, kc, 256 + fi * 128:256 + (fi + 1) * 128],
                                                                   rhs=h2T[:, kc, cs], start=(kc == 0), stop=(kc == 7)))
                                       for kc in range(8)], reads=[wgb, h2Tb], writes=[PB[b0 + 1]])
                        S.op("act", lambda e: e.activation(out=SG[k2][:], in_=ps[:, b0, :], func=AF.Silu),
                             reads=[PB[b0]], writes=[SGb[k2]])
                        S.op("dve", lambda e: e.tensor_tensor(out=actT[:, f, cs], in0=ps[:, b0 + 1, :], in1=SG[k2][:],
                                                              op=ALU.mult),
                             reads=[PB[b0 + 1], SGb[k2]], writes=[actTb])
                if fp + 2 < nfp:
                    load_gu(fp + 2)
            for t8 in range(8):
                tt = half * 8 + t8
                banks = (2 * (t8 % 2), 2 * (t8 % 2) + 1)
                for hf in range(2):
                    S.group("pe", [(lambda e, f=f: e.matmul(ps[:, banks[hf], :], lhsT=actT[:, f, t8 * 128:(t8 + 1) * 128],
                                                             rhs=Wd[:, f, hf * 512:(hf + 1) * 512],
                                                             start=(f == 0), stop=(f == NF - 1))) for f in range(NF)],
                            reads=[actTb, Wdb], writes=[PB[banks[hf]]])
                post_A(banks, t8)
                post_B(banks, gfpost, gfb, X1[:, t8, :], [X1b[t8]], t8, y[seq, tt * 128:(tt + 1) * 128, :])
        S.barrier()
    return nc


_NC_CACHE = {}


def kernel(x, norm_mix_pre_g, w_in, w_branch_a, w_branch_b, lam_q1, lam_k1, lam_q2, lam_k2,
           diff_subln_g, w_out, norm_mix_post_g, norm_ffn_pre_g, w_gate, w_up, w_down, norm_ffn_post_g):
    n = 8
    f32 = lambda a: np.ascontiguousarray(np.asarray(a, dtype=np.float32))
    x = f32(x)
    consts = _host_consts()
    shared = {
        "w_in": f32(w_in)[0], "w_branch_a": f32(w_branch_a)[0], "w_branch_b": f32(w_branch_b)[0],
        "w_out": f32(w_out)[0], "w_gate": f32(w_gate)[0], "w_up": f32(w_up)[0], "w_down": f32(w_down)[0],
        "norm_mix_pre_g": f32(norm_mix_pre_g), "norm_mix_post_g": f32(norm_mix_post_g),
        "norm_ffn_pre_g": f32(norm_ffn_pre_g), "norm_ffn_post_g": f32(norm_ffn_post_g),
        "lamv": np.concatenate([f32(lam_q1)[0], f32(lam_k1)[0], f32(lam_q2)[0], f32(lam_k2)[0]])[None, :],
        "diff_subln_g": f32(diff_subln_g)[0][:, None],
    }
    shared.update(consts)
    nc = build(2)
    in_maps = []
    for c in range(n):
        m = dict(shared)
        m["x"] = np.ascontiguousarray(x[2 * c:2 * c + 2])
        in_maps.append(m)
    res = run_bass_kernel_spmd(nc, in_maps, core_ids=list(range(n)))
    return np.concatenate([r["y"] for r in res.results], axis=0).astype(np.float32)
```

```python
import numpy as np
import ml_dtypes
import concourse.bass as bass
import concourse.mybir as mybir
from concourse.bass_utils import run_bass_kernel_spmd

F32 = mybir.dt.float32
BF16 = mybir.dt.bfloat16
AF = mybir.ActivationFunctionType
ALU = mybir.AluOpType
AX = mybir.AxisListType

T = 2048
D = 1024
DFF = 2816
NF = DFF // 128
NEGM = -30000.0


class Buf:
    __slots__ = ("name", "w", "r")

    def __init__(self, name):
        self.name = name
        self.w = None
        self.r = {}


class EngState:
    def __init__(self, name, eng, sem):
        self.name = name
        self.eng = eng
        self.sem = sem
        self.cnt = 0
        self.seen = {}


class Sync:
    def __init__(self, nc, ndma=20):
        self.nc = nc
        self.E = {}
        for name, e in (("pe", nc.tensor), ("act", nc.scalar), ("dve", nc.vector),
                        ("pool", nc.gpsimd), ("sp", nc.sync)):
            self.E[name] = EngState(name, e, nc.alloc_semaphore("s_" + name))
        self.dsem = {}
        self.dnext = {}
        for q in ("sp", "pool"):
            self.dsem[q] = [[nc.alloc_semaphore("d_%s%d" % (q, i)), 0] for i in range(ndma)]
            self.dnext[q] = 0
        self.semof = {}
        for name, st in self.E.items():
            self.semof[("c", name)] = st.sem
        for q in ("sp", "pool"):
            for i, (s, _) in enumerate(self.dsem[q]):
                self.semof[("d", q, i)] = s

    def _wait(self, E, deps):
        for key, n in deps.items():
            if key == ("c", "pe") and E.name == "pe":
                continue
            if E.seen.get(key, 0) >= n:
                continue
            E.eng.wait_ge(self.semof[key], n)
            E.seen[key] = n

    @staticmethod
    def _deps(reads, writes):
        deps = {}

        def add(key, n):
            if deps.get(key, 0) < n:
                deps[key] = n
        for b in reads:
            if b.w is not None:
                add(*b.w)
        for b in writes:
            if b.w is not None:
                add(*b.w)
            for key, n in b.r.items():
                add(key, n)
        return deps

    @staticmethod
    def _mark(ev, reads, writes):
        key, n = ev
        for b in reads:
            if b.r.get(key, 0) < n:
                b.r[key] = n
        for b in writes:
            b.w = ev
            b.r = {}

    def op(self, engname, fn, reads=(), writes=()):
        E = self.E[engname]
        self._wait(E, self._deps(reads, writes))
        ins = fn(E.eng)
        E.cnt += 1
        ins.then_inc(E.sem, 1)
        ev = (("c", engname), E.cnt)
        self._mark(ev, reads, writes)
        return ev

    def group(self, engname, fns, reads=(), writes=()):
        E = self.E[engname]
        self._wait(E, self._deps(reads, writes))
        ins = None
        for fn in fns:
            ins = fn(E.eng)
        E.cnt += 1
        ins.then_inc(E.sem, 1)
        ev = (("c", engname), E.cnt)
        self._mark(ev, reads, writes)
        return ev

    def split_group(self, engname, fns, reads, writes, per):
        E = self.E[engname]
        self._wait(E, self._deps(reads, writes))
        n = len(fns)
        for i, fn in enumerate(fns):
            ins = fn(E.eng)
            if i == n - 1:
                E.cnt += 1
                ins.then_inc(E.sem, 1)
                self._mark((("c", engname), E.cnt), reads, writes)
            elif (i + 1) % per == 0:
                yield

    def dma(self, q, out, in_, reads=(), writes=()):
        E = self.E[q]
        i = self.dnext[q]
        self.dnext[q] = (i + 1) % len(self.dsem[q])
        slot = self.dsem[q][i]
        key = ("d", q, i)
        deps = self._deps(reads, writes)
        if slot[1] > 0 and deps.get(key, 0) < slot[1]:
            deps[key] = slot[1]
        self._wait(E, deps)
        E.eng.dma_start(out=out, in_=in_).then_inc(slot[0], 16)
        slot[1] += 16
        ev = (key, slot[1])
        self._mark(ev, reads, writes)
        return ev

    def barrier(self, engines=("pe", "act", "dve", "pool", "sp")):
        deps = {}
        for name, st in self.E.items():
            if st.cnt > 0:
                deps[("c", name)] = st.cnt
        for q in ("sp", "pool"):
            for i, (s, n) in enumerate(self.dsem[q]):
                if n > 0:
                    deps[("d", q, i)] = n
        for name in engines:
            self._wait(self.E[name], deps)


class Arena:
    def __init__(self, nc, base, top):
        self.nc = nc
        self.ptr = (base + 31) // 32 * 32
        self.top = top
        self.n = 0

    def alloc(self, name, shape, dtype):
        nbytes = 2 if dtype == BF16 else 4
        sz = 1
        for s in shape[1:]:
            sz *= s
        sz *= nbytes
        off = self.ptr
        self.ptr = (off + sz + 31) // 32 * 32
        assert self.ptr <= self.top, ("SBUF overflow", name, self.ptr, self.top)
        self.n += 1
        return self.nc.alloc_sbuf_tensor_at("%s_%d" % (name, self.n), list(shape), dtype, offset=off)

    def mark(self):
        return self.ptr

    def reset(self, m):
        self.ptr = m


def _alibi_slopes():
    n = 12
    slopes = 2.0 ** (-8.0 * np.arange(1, n + 1) / n)
    diff_idx = np.arange(4) * 3 + 2
    moba_idx = np.setdiff1d(np.arange(n), diff_idx)
    return slopes[moba_idx].astype(np.float32), slopes[diff_idx].astype(np.float32)


def _host_consts():
    bf = ml_dtypes.bfloat16
    ms, ds = _alibi_slopes()
    slopes = np.concatenate([ms, ds]).astype(np.float32)
    pos = np.arange(T)
    hi = (256 * (pos // 256)).astype(np.float32)
    lo = (pos % 256).astype(np.float32)
    qx = np.zeros((12, 16, T), np.float32)
    kx = np.zeros((12, 16, T), np.float32)
    for h in range(12):
        c1 = np.float32(slopes[h]).astype(bf).astype(np.float32)
        c2 = np.float32(slopes[h] - c1).astype(bf).astype(np.float32)
        qx[h, 8], qx[h, 9], qx[h, 10], qx[h, 11] = hi, lo, hi, lo
        qx[h, 12], qx[h, 13], qx[h, 14], qx[h, 15] = c1, c1, c2, c2
        kx[h, 8], kx[h, 9], kx[h, 10], kx[h, 11] = -c1, -c1, -c2, -c2
        kx[h, 12], kx[h, 13], kx[h, 14], kx[h, 15] = hi, lo, hi, lo
        if h < 8:
            for n in range(8):
                kx[h, n] = (pos // 256 == n).astype(np.float32)
    ident = np.eye(128, dtype=np.float32)
    kk = np.arange(128)[:, None]
    qq = np.arange(128)[None, :]
    negtri = np.where(kk <= qq, 0.0, NEGM).astype(np.float32)
    pastmask = np.zeros((64, 8), np.float32)
    pairsum = np.zeros((64, 72), np.float32)
    for n in range(8):
        for m in range(8):
            p = n * 8 + m
            pairsum[p, 64 + n] = 1.0
            for qb in range(8):
                pastmask[p, qb] = 1.0 if m < qb else 0.0
    notown = np.zeros((128, 8), np.float32)
    for n in range(8):
        for qb in range(8):
            notown[64 + n, qb] = NEGM if n < qb else 0.0
    return {
        "c_qx": qx.astype(bf), "c_kx": kx.astype(bf), "c_ident": ident.astype(bf),
        "c_negtri": negtri.astype(bf), "c_pastmask": pastmask, "c_pairsum": pairsum.astype(bf),
        "c_notown": notown,
    }


def build(nseq=2, debug=False):
    nc = bass.Bass("TRN2", target_bir_lowering=False)
    S = Sync(nc)

    def din(name, shape, dt=F32):
        return nc.dram_tensor(name, list(shape), dt, kind="ExternalInput").ap()

    x = din("x", [nseq, T, D])
    w_in = din("w_in", [D, 5120])
    w_a = din("w_branch_a", [512, D])
    w_b = din("w_branch_b", [512, D])
    w_out = din("w_out", [D, D])
    w_gate = din("w_gate", [D, DFF])
    w_up = din("w_up", [D, DFF])
    w_down = din("w_down", [DFF, D])
    g_pre = din("norm_mix_pre_g", [1, D])
    g_post = din("norm_mix_post_g", [1, D])
    g_fpre = din("norm_ffn_pre_g", [1, D])
    g_fpost = din("norm_ffn_post_g", [1, D])
    lamv = din("lamv", [1, 256])
    g_sub = din("diff_subln_g", [128, 1])
    c_qx = din("c_qx", [12, 16, T], BF16)
    c_kx = din("c_kx", [12, 16, T], BF16)
    c_ident = din("c_ident", [128, 128], BF16)
    c_negtri = din("c_negtri", [128, 128], BF16)
    c_pastmask = din("c_pastmask", [64, 8])
    c_pairsum = din("c_pairsum", [64, 72], BF16)
    c_notown = din("c_notown", [128, 8])
    y = nc.dram_tensor("y", [nseq, T, D], F32, kind="ExternalOutput").ap()
    x1s = nc.dram_tensor("x1s", [nseq, T, D], F32).ap()
    dbg = {}
    if debug:
        dbg["hT"] = nc.dram_tensor("dbg_hT", [128, 8, T], BF16, kind="ExternalOutput").ap()
        dbg["yT"] = nc.dram_tensor("dbg_yT", [128, 8, T], BF16, kind="ExternalOutput").ap()
        dbg["mT"] = nc.dram_tensor("dbg_mT", [128, 8, T], BF16, kind="ExternalOutput").ap()

    w_in_r = w_in.rearrange("(kc p) n -> p kc n", p=128)
    w_in_r4 = w_in.rearrange("(kc p) (a n) -> p kc a n", p=128, n=128)
    w_a_r = w_a.rearrange("(kc p) n -> p kc n", p=128)
    w_b_r = w_b.rearrange("(kc p) n -> p kc n", p=128)
    w_out_r = w_out.rearrange("(kc p) n -> p kc n", p=128)
    w_gate_r = w_gate.rearrange("(kc p) n -> p kc n", p=128)
    w_up_r = w_up.rearrange("(kc p) n -> p kc n", p=128)
    w_down_r = w_down.rearrange("(f p) n -> p f n", p=128)

    _ms, _ds = _alibi_slopes()
    WIN = [min(15, int((100.0 / float(sl_) + 127.0) // 128)) for sl_ in list(_ms) + list(_ds)]
    wgu_off = (nc.sbuf_top - 2 * 8192) // 32 * 32
    ar = Arena(nc, nc.sbuf_base, wgu_off)
    WGU = [nc.alloc_sbuf_tensor_at("wgu%d" % i, [128, 8, 512], BF16, offset=wgu_off + 8192 * i) for i in range(2)]
    WGUb = [Buf("wgu") for _ in range(2)]
    ps = nc.alloc_psum_tensor("ps", [128, 8, 512], F32)
    PB = [Buf("bank%d" % i) for i in range(8)]

    ident = ar.alloc("ident", [128, 128], BF16)
    negtri = ar.alloc("negtri", [128, 128], BF16)
    ones_bf = ar.alloc("ones", [128, 128], BF16)
    pastmask = ar.alloc("pastmask", [64, 8], F32)
    pairsum = ar.alloc("pairsum", [64, 72], BF16)
    notown = ar.alloc("notown", [128, 8], F32)
    gsub = ar.alloc("gsub", [128, 1], F32)
    neglam = ar.alloc("neglam", [128, 1], F32)
    lamt = ar.alloc("lamt", [128, 256], F32)
    lamp = ar.alloc("lamp", [128, 128], F32)
    lams = ar.alloc("lams", [128, 4], F32)
    XIN = [ar.alloc("xin", [128, D], F32) for _ in range(2)]
    XINb = [Buf("xin%d" % i) for i in range(2)]
    HN = [ar.alloc("hn", [128, D], BF16) for _ in range(2)]
    HNb = [Buf("hn%d" % i) for i in range(2)]
    junk = ar.alloc("junk", [128, D], BF16)
    junkb = Buf("junk")
    SS = [ar.alloc("ss", [128, 4], F32) for _ in range(4)]
    SSb = [Buf("ss%d" % i) for i in range(4)]
    epsD = ar.alloc("epsD", [128, 1], F32)
    eps5 = ar.alloc("eps5", [128, 1], F32)
    TST = [ar.alloc("tst", [128, D], F32) for _ in range(2)]
    TSTb = [Buf("tst%d" % i) for i in range(2)]
    cb = Buf("consts")

    S.dma("sp", ident[:], c_ident, writes=[cb])
    S.dma("sp", negtri[:], c_negtri, writes=[cb])
    S.dma("sp", pastmask[:], c_pastmask, writes=[cb])
    S.dma("sp", pairsum[:], c_pairsum, writes=[cb])
    S.dma("sp", notown[:], c_notown, writes=[cb])
    S.dma("sp", gsub[:], g_sub, writes=[cb])
    S.dma("sp", lamt[:], lamv.partition_broadcast(128), writes=[cb])
    S.op("dve", lambda e: e.memset(ones_bf[:], 1.0), writes=[cb])
    S.op("dve", lambda e: e.memset(epsD[:], 1e-6), writes=[cb])
    S.op("dve", lambda e: e.memset(eps5[:], 1e-5), writes=[cb])
    lamtv = lamt[:].rearrange("p (a b d) -> p a b d", a=2, b=2)
    S.op("dve", lambda e: e.tensor_tensor(out=lamp[:].rearrange("p (a d) -> p a d", a=2),
                                          in0=lamtv[:, :, 0, :], in1=lamtv[:, :, 1, :], op=ALU.mult),
         reads=[cb], writes=[cb])
    S.op("dve", lambda e: e.tensor_reduce(out=lams[:, 0:2], in_=lamp[:].rearrange("p (a d) -> p a d", a=2),
                                          axis=AX.X, op=ALU.add), reads=[cb], writes=[cb])
    S.op("act", lambda e: e.activation(out=lams[:, 2:4], in_=lams[:, 0:2], func=AF.Exp), reads=[cb], writes=[cb])
    S.op("dve", lambda e: e.scalar_tensor_tensor(out=neglam[:], in0=lams[:, 3:4], scalar=-0.2, in1=lams[:, 2:3],
                                                 op0=ALU.add, op1=ALU.subtract), reads=[cb], writes=[cb])
    S.op("dve", lambda e: e.tensor_scalar(out=gsub[:], in0=gsub[:], scalar1=0.8, scalar2=None, op0=ALU.mult),
         reads=[cb], writes=[cb])

    base_mark = ar.mark()

    NSS = 4

    def norm_A(src_ap, srcb, k):
        ss, ssb = SS[k % NSS], SSb[k % NSS]
        S.op("act", lambda e: e.memzero(ss[:]), writes=[ssb])
        S.op("act", lambda e: e.activation(out=junk[:], in_=src_ap, func=AF.Square, accum_out=ss[:, 0:1]),
             reads=srcb, writes=[junkb, ssb])
        S.op("act", lambda e: e.activation(out=ss[:, 1:2], in_=ss[:, 0:1], func=AF.Ln, scale=1.0 / D,
                                           bias=epsD[:, 0:1]), reads=[ssb, cb], writes=[ssb])
        S.op("act", lambda e: e.activation(out=ss[:, 1:2], in_=ss[:, 1:2], func=AF.Exp, scale=-0.5),
             reads=[ssb], writes=[ssb])

    def norm_B(src_ap, srcb, gt, gtb, k, bank):
        ss, ssb = SS[k % NSS], SSb[k % NSS]
        hn, hnb = HN[k % 2], HNb[k % 2]
        S.op("dve", lambda e: e.scalar_tensor_tensor(out=hn[:], in0=src_ap, scalar=ss[:, 1:2], in1=gt[:],
                                                     op0=ALU.mult, op1=ALU.mult),
             reads=list(srcb) + [ssb, gtb], writes=[hnb])
        psb = ps[:, bank, :].bitcast(BF16)
        S.group("pe", [(lambda e, kc=kc: e.transpose(out=psb[:, kc * 128:(kc + 1) * 128],
                                                      in_=hn[:, kc * 128:(kc + 1) * 128], identity=ident[:]))
                       for kc in range(8)], reads=[hnb, cb], writes=[PB[bank]])

    def norm_C(dstT, dstb, tcol, bank):
        psb = ps[:, bank, :].bitcast(BF16)
        S.op("act", lambda e: e.activation(out=dstT[:, :, tcol:tcol + 128],
                                           in_=psb.rearrange("p (k t) -> p k t", k=8), func=AF.Copy),
             reads=[PB[bank]], writes=[dstb])

    def norm_transpose_pipeline(n, src_of, gt, gtb, dstT, dstb, banks):
        srcs = {}

        def A(k):
            srcs[k] = src_of(k)
            norm_A(srcs[k][0], srcs[k][1], k)

        def B(k):
            norm_B(srcs[k][0], srcs[k][1], gt, gtb, k, banks[k % len(banks)])

        def C(k):
            norm_C(dstT, dstb, k * 128, banks[k % len(banks)])
        A(0)
        for k in range(n):
            if k + 1 < n:
                A(k + 1)
            B(k)
            if k >= 1:
                C(k - 1)
        C(n - 1)

    def post_A(banks, k):
        ss, ssb = SS[k % NSS], SSb[k % NSS]
        S.op("act", lambda e: e.memzero(ss[:]), writes=[ssb])
        S.op("act", lambda e: e.activation(out=junk[:], in_=ps[:, banks[0]:banks[0] + 2, :].rearrange("p b n -> p (b n)"),
                                           func=AF.Square, accum_out=ss[:, 0:1]),
             reads=[PB[banks[0]], PB[banks[1]]], writes=[junkb, ssb])
        S.op("act", lambda e: e.activation(out=ss[:, 1:2], in_=ss[:, 0:1], func=AF.Ln, scale=1.0 / D,
                                           bias=epsD[:, 0:1]), reads=[ssb, cb], writes=[ssb])
        S.op("act", lambda e: e.activation(out=ss[:, 1:2], in_=ss[:, 1:2], func=AF.Exp, scale=-0.5),
             reads=[ssb], writes=[ssb])

    def post_B(banks, gt, gtb, resid_ap, residb, k, out_dram):
        ss, ssb = SS[k % NSS], SSb[k % NSS]
        tst, tstb = TST[k % 2], TSTb[k % 2]
        for hf in range(2):
            S.op("dve", lambda e, hf=hf: e.scalar_tensor_tensor(
                out=tst[:, hf * 512:(hf + 1) * 512], in0=ps[:, banks[hf], :], scalar=ss[:, 1:2],
                in1=gt[:, hf * 512:(hf + 1) * 512], op0=ALU.mult, op1=ALU.mult),
                reads=[PB[banks[hf]], ssb, gtb], writes=[tstb])
        S.op("dve", lambda e: e.tensor_tensor(out=tst[:], in0=tst[:], in1=resid_ap, op=ALU.add),
             reads=[tstb] + list(residb), writes=[tstb])
        S.dma("sp", out_dram, tst[:], reads=[tstb])

    def load_gu(fp):
        sl = fp % 2
        S.dma("pool", WGU[sl][:, :, 0:256], w_gate_r[:, :, 256 * fp:256 * fp + 256], writes=[WGUb[sl]])
        S.dma("pool", WGU[sl][:, :, 256:512], w_up_r[:, :, 256 * fp:256 * fp + 256], writes=[WGUb[sl]])

    for seq in range(nseq):
        ar.reset(base_mark)
        hT = ar.alloc("hT", [128, 8, T], BF16)
        hTb = Buf("hT")
        yT = ar.alloc("yT", [128, 8, T], BF16)
        yTb = Buf("yT")
        a_mark = ar.mark()
        gpre = ar.alloc("gpre", [128, D], F32)
        gpreb = Buf("gpre")
        QK = [[ar.alloc("qk", [128, T], BF16) for _ in range(4)] for _ in range(2)]
        QKb = [[Buf("qk") for _ in range(4)] for _ in range(2)]
        VV = [ar.alloc("vv", [128, 16, 320], BF16) for _ in range(2)]
        VVb = [Buf("vv") for _ in range(2)]
        WSL = [ar.alloc("wsl", [128, 8, 384], BF16) for _ in range(2)]
        WSLb = [Buf("wsl") for _ in range(2)]
        NPB = 5
        PBF = [ar.alloc("pbf", [128, 512], BF16) for _ in range(NPB)]
        PBFb = [Buf("pbf") for _ in range(NPB)]
        ksum = [ar.alloc("ksum", [64, 8], F32) for _ in range(2)]
        diffw = [ar.alloc("diffw", [64, 64], BF16) for _ in range(2)]
        ind = [ar.alloc("ind", [64, 512], BF16) for _ in range(2)]
        gateb = [Buf("gate") for _ in range(2)]
        indb = [Buf("ind") for _ in range(2)]
        rbuf = [ar.alloc("rbuf", [128, 512], F32) for _ in range(2)]
        rbufb = [Buf("rbuf") for _ in range(2)]
        dL1 = [ar.alloc("dL1", [128, 512], F32) for _ in range(2)]
        dL2 = [ar.alloc("dL2", [128, 512], F32) for _ in range(2)]
        dO1 = [ar.alloc("dO1", [128, 512], F32) for _ in range(2)]
        dO2 = [ar.alloc("dO2", [128, 512], F32) for _ in range(2)]
        dSQ = [ar.alloc("dSQ", [128, 512], BF16) for _ in range(2)]
        dRS = [ar.alloc("dRS", [128, 512], F32) for _ in range(2)]
        dfb = [Buf("dfin") for _ in range(2)]

        S.dma("sp", gpre[:], g_pre.partition_broadcast(128), writes=[gpreb])
        for sl in range(2):
            for i in range(4):
                S.op("dve", lambda e, sl=sl, i=i: e.memset(QK[sl][i][:], 0.0), writes=[QKb[sl][i]])
            S.op("dve", lambda e, sl=sl: e.memset(VV[sl][:], 1.0), writes=[VVb[sl]])

        def p1_src(tt):
            xin, xinb = XIN[tt % 2], XINb[tt % 2]
            S.dma("sp", xin[:], x[seq, tt * 128:(tt + 1) * 128, :], writes=[xinb])
            return xin[:], [xinb]
        norm_transpose_pipeline(16, p1_src, gpre, gpreb, hT, hTb, (6, 7))
        if debug and seq == 0:
            S.dma("sp", dbg["hT"], hT[:], reads=[hTb])

        def load_group_weights(g):
            sl = g % 2
            a0 = g if g < 4 else 12 + (g - 4)
            for i3 in range(3):
                S.dma("pool", WSL[sl][:, :, i3 * 128:(i3 + 1) * 128], w_in_r4[:, :, a0 + 4 * i3, :],
                      writes=[WSLb[sl]])
            hA, hB = (2 * g, 2 * g + 1) if g < 4 else (8 + g - 4, 8 + g - 4)
            q = QK[sl]
            qb_ = QKb[sl]
            S.dma("sp", q[0][64:80, :], c_qx[hA], writes=[qb_[0]])
            S.dma("sp", q[1][64:80, :], c_qx[hB], writes=[qb_[1]])
            S.dma("sp", q[2][64:80, :], c_kx[hA], writes=[qb_[2]])
            S.dma("sp", q[3][64:80, :], c_kx[hB], writes=[qb_[3]])

        pbk = {"i": 0}

        pbk["set"] = (7, 5, 6)

        def pbank():
            pbk["i"] += 1
            st = pbk["set"]
            return st[pbk["i"] % len(st)]

        def prep_units(g):
            sl = g % 2
            q, qb_ = QK[sl], QKb[sl]
            w, wb = WSL[sl], WSLb[sl]
            load_group_weights(g)
            yield
            for c in range(4):
                cs = slice(c * 512, (c + 1) * 512)
                bank = pbank()
                yield from S.split_group(
                    "pe", [(lambda e, kc=kc: e.matmul(ps[:, bank, :], lhsT=w[:, kc, 0:128], rhs=hT[:, kc, cs],
                                                      start=(kc == 0), stop=(kc == 7))) for kc in range(8)],
                    [wb, hTb], [PB[bank]], 2)
                S.op("dve", lambda e: e.tensor_scalar(out=q[0][0:64, cs], in0=ps[0:64, bank, :], scalar1=0.125,
                                                      scalar2=None, op0=ALU.mult),
                     reads=[PB[bank]], writes=[qb_[0]])
                S.op("dve", lambda e: e.tensor_scalar(out=q[1][0:64, cs], in0=ps[64:128, bank, :], scalar1=0.125,
                                                      scalar2=None, op0=ALU.mult),
                     reads=[PB[bank]], writes=[qb_[1]])
                yield
                bank = pbank()
                yield from S.split_group(
                    "pe", [(lambda e, kc=kc: e.matmul(ps[:, bank, :], lhsT=w[:, kc, 128:256], rhs=hT[:, kc, cs],
                                                      start=(kc == 0), stop=(kc == 7))) for kc in range(8)],
                    [wb, hTb], [PB[bank]], 2)
                S.op("dve", lambda e: e.tensor_copy(out=q[2][0:64, cs], in_=ps[0:64, bank, :]),
                     reads=[PB[bank]], writes=[qb_[2]])
                S.op("dve", lambda e: e.tensor_copy(out=q[3][0:64, cs], in_=ps[64:128, bank, :]),
                     reads=[PB[bank]], writes=[qb_[3]])
                yield
            vv, vvb = VV[sl], VVb[sl]
            for tg in range(4):
                bank = pbank()
                fns = []
                for i in range(4):
                    tt = 4 * tg + i
                    for kc in range(8):
                        fns.append(lambda e, i=i, tt=tt, kc=kc: e.matmul(
                            ps[:, bank, i * 128:(i + 1) * 128], lhsT=hT[:, kc, tt * 128:(tt + 1) * 128],
                            rhs=w[:, kc, 256:384], start=(kc == 0), stop=(kc == 7)))
                yield from S.split_group("pe", fns, [wb, hTb], [PB[bank]], 4)
                if g < 4:
                    vq = vv[:].rearrange("p t (a d) -> p t a d", d=64)
                    S.op("dve", lambda e: e.tensor_copy(
                        out=vq[:, 4 * tg:4 * tg + 4, 0:3:2, :],
                        in_=ps[:, bank, :].rearrange("p (t h d) -> p t h d", t=4, h=2)),
                        reads=[PB[bank]], writes=[vvb])
                else:
                    S.op("dve", lambda e: e.tensor_copy(
                        out=vv[:, 4 * tg:4 * tg + 4, 192:320],
                        in_=ps[:, bank, :].rearrange("p (t d) -> p t d", t=4)),
                        reads=[PB[bank]], writes=[vvb])
                yield
            if g < 4:
                for hx in range(2):
                    for _ in moba_gate(g, hx):
                        yield

        def moba_gate(g, hx):
            sl = g % 2
            qt, qtb = QK[sl][hx], QKb[sl][hx]
            kt_, ktb = QK[sl][2 + hx], QKb[sl][2 + hx]
            ks, dw, gb_ = ksum[hx], diffw[hx], gateb[hx]
            S.op("dve", lambda e: e.tensor_reduce(out=ks[:], in_=kt_[0:64, :].rearrange("p (n k) -> p n k", k=256),
                                                  axis=AX.X, op=ALU.add), reads=[ktb], writes=[gb_])
            for n in range(8):
                S.op("dve", lambda e, n=n: e.tensor_scalar(out=dw[:, n * 8:(n + 1) * 8], in0=ks[:, 0:8],
                                                           scalar1=ks[:, n:n + 1], scalar2=None,
                                                           op0=ALU.subtract), reads=[gb_], writes=[gb_])
            yield
            for c in (2, 3):
                cs = slice(c * 512, (c + 1) * 512)
                bank = pbank()
                S.group("pe", [lambda e: e.matmul(ps[0:64, bank, :], lhsT=dw[:, :], rhs=qt[0:64, cs],
                                                  start=True, stop=True)],
                        reads=[gb_, qtb], writes=[PB[bank]])
                for hh in range(2):
                    qblk = 2 * c + hh
                    hs = slice(hh * 256, (hh + 1) * 256)
                    S.op("dve", lambda e, hs=hs, qblk=qblk: e.tensor_scalar(
                        out=ind[hx][:, hs], in0=ps[0:64, bank, hs], scalar1=0.0,
                        scalar2=pastmask[:, qblk:qblk + 1], op0=ALU.is_gt, op1=ALU.mult),
                        reads=[PB[bank], cb], writes=[indb[hx]])
                yield
                bank = pbank()
                S.group("pe", [lambda e: e.matmul(ps[0:72, bank, :], lhsT=pairsum[:, :], rhs=ind[hx][:, :],
                                                  start=True, stop=True)],
                        reads=[indb[hx], cb], writes=[PB[bank]])
                for hh in range(2):
                    qblk = 2 * c + hh
                    hs = slice(hh * 256, (hh + 1) * 256)
                    S.op("dve", lambda e, hs=hs, qblk=qblk: e.tensor_scalar(
                        out=qt[64:72, c * 512 + hh * 256:c * 512 + (hh + 1) * 256], in0=ps[64:72, bank, hs],
                        scalar1=2.5, scalar2=notown[64:72, qblk:qblk + 1], op0=ALU.is_gt, op1=ALU.mult),
                        reads=[PB[bank], cb], writes=[qtb])
                yield

        state = {"s": 0, "p": 0, "la": 2, "sb": (0, 1, 2), "facc": 0.0}

        def tile_cols(c, W):
            out = []
            for kt in range(max(0, 4 * c - W), 4 * c + 4):
                qlo = max(kt, 4 * c)
                qhi = min(4 * c + 3, kt + W)
                if qhi < qlo:
                    continue
                out.append((kt, (128 * (qlo - 4 * c), 128 * (qhi - 4 * c + 1))))
            return out
        pending = []

        def defer(n, fn):
            pending.append([n, fn])

        def run_pending(flush=False):
            while True:
                ready = None
                for ent in pending:
                    if flush or ent[0] <= 0:
                        ready = ent
                        break
                if ready is None:
                    break
                pending.remove(ready)
                ready[1]()
            for ent in pending:
                ent[0] -= 1

        def attention(items, finalize, filler, every):
            n = len(items)
            sb_of = {}

            def qk(i):
                it = items[i]
                sbank = state["sb"][state["s"] % len(state["sb"])]
                state["s"] += 1
                sb_of[i] = sbank
                c, kt = it["c"], it["kt"]
                j = kt - 4 * c
                col0, col1 = it["cols"]
                fns = [lambda e: e.matmul(ps[:, sbank, col0:col1], lhsT=it["k"][0:80, kt * 128:(kt + 1) * 128],
                                          rhs=it["q"][0:80, c * 512 + col0:c * 512 + col1],
                                          start=True, stop=(j < 0))]
                if j >= 0:
                    fns.append(lambda e: e.matmul(ps[:, sbank, col0:col0 + 128], lhsT=ident[:], rhs=negtri[:],
                                                  start=False, stop=True))
                S.group("pe", fns, reads=[it["kb"], it["qb"], cb], writes=[PB[sbank]])

            def ex_pv(i):
                it = items[i]
                sbank = sb_of[i]
                pi = state["p"] % NPB
                state["p"] += 1
                col0, col1 = it["cols"]
                win = it["win"]
                S.op("act", lambda e: e.activation(out=PBF[pi][:, col0:col1], in_=ps[:, sbank, col0:col1], func=AF.Exp),
                     reads=[PB[sbank]], writes=[PBFb[pi]])
                fns = []
                banks = []
                rd = [PBFb[pi]]
                for (bank, lap, lbuf) in it["pv"]:
                    if win and it["first"]:
                        S.op("dve", lambda e, bank=bank: e.memset(ps[:, bank, :], 0.0), writes=[PB[bank]])
                    if win:
                        fns.append(lambda e, bank=bank, lap=lap: e.matmul(
                            ps[:, bank, col0:col1], lhsT=lap, rhs=PBF[pi][:, col0:col1],
                            start=False, stop=it["last"], skip_group_check=True))
                    else:
                        fns.append(lambda e, bank=bank, lap=lap: e.matmul(
                            ps[:, bank, col0:col1], lhsT=lap, rhs=PBF[pi][:, col0:col1],
                            start=it["first"], stop=it["last"]))
                    banks.append(PB[bank])
                    rd.append(lbuf)
                S.group("pe", fns, reads=rd, writes=banks)
                if it["last"]:
                    finalize(it)

            la = state["la"]
            for i in range(min(la, n)):
                qk(i)
            for i in range(n):
                if i + la < n:
                    qk(i + la)
                ex_pv(i)
                run_pending()
                if filler is not None:
                    state["facc"] += every
                    while state["facc"] >= 1.0:
                        state["facc"] -= 1.0
                        next(filler, None)

        def recip(out_ap, in_ap, scr_ap, reads, wb_, on_act=False):
            if on_act:
                S.op("act", lambda e: e.activation(out=scr_ap, in_=in_ap, func=AF.Ln), reads=reads, writes=[wb_])
                S.op("act", lambda e: e.activation(out=out_ap, in_=scr_ap, func=AF.Exp, scale=-1.0),
                     reads=[wb_], writes=[wb_])
            else:
                S.op("dve", lambda e: e.reciprocal(out=out_ap, in_=in_ap), reads=reads, writes=[wb_])

        def run_moba(g, filler):
            sl = g % 2
            nit = sum(len(tile_cols(c, WIN[2 * g + hx])) for hx in range(2) for c in range(4))
            rate = 82.0 / nit
            for hx in range(2):
                items = []
                W = WIN[2 * g + hx]
                for c in range(4):
                    bank = 3 + ((hx * 4 + c) % 2)
                    lap_of = (lambda kt: VV[sl][:, kt, 0:128]) if hx == 0 else (lambda kt: VV[sl][:, kt, 64:192])
                    first = True
                    for kt, cols in tile_cols(c, W):
                        items.append(dict(q=QK[sl][hx], qb=QKb[sl][hx], k=QK[sl][2 + hx], kb=QKb[sl][2 + hx],
                                          c=c, kt=kt, pv=[(bank, lap_of(kt), VVb[sl])], last=(kt == 4 * c + 3),
                                          bank=bank, cols=cols, first=first, win=(W < 15)))
                        first = False

                def fin(it, hx=hx):
                    defer(3, lambda: fin_body(it, hx))

                def fin_body(it, hx):
                    bank, c = it["bank"], it["c"]
                    cs = slice(c * 512, (c + 1) * 512)
                    rb, rbb = rbuf[c % 2], rbufb[c % 2]
                    if hx == 0:
                        recip(rb[0:64, :], ps[64:128, bank, :], rb[0:64, :], [PB[bank]], rbb, on_act=True)
                        S.op("dve", lambda e: e.tensor_tensor(out=yT[0:64, g, cs], in0=ps[0:64, bank, :],
                                                              in1=rb[0:64, :], op=ALU.mult),
                             reads=[PB[bank], rbb], writes=[yTb])
                    else:
                        recip(rb[64:128, :], ps[0:64, bank, :], rb[64:128, :], [PB[bank]], rbb, on_act=True)
                        S.op("dve", lambda e: e.tensor_tensor(out=yT[64:128, g, cs], in0=ps[64:128, bank, :],
                                                              in1=rb[64:128, :], op=ALU.mult),
                             reads=[PB[bank], rbb], writes=[yTb])
                attention(items, fin, filler, rate)

        def run_diff(g, filler):
            sl = g % 2
            j = g - 4
            items = []
            W = WIN[8 + j]
            for c in range(4):
                first = True
                for kt, cols in tile_cols(c, W):
                    for mp in range(2):
                        items.append(dict(q=QK[sl][mp], qb=QKb[sl][mp], k=QK[sl][2 + mp], kb=QKb[sl][2 + mp],
                                          c=c, kt=kt,
                                          pv=[(3 + mp, VV[sl][:, kt, 192:320], VVb[sl]),
                                              (5 + mp, ones_bf[:], cb)],
                                          last=(kt == 4 * c + 3), mp=mp, cols=cols, first=first, win=(W < 15)))
                    first = False

            def fin(it):
                if it["mp"] == 0:
                    return
                run_pending(flush=True)
                c = it["c"]
                cs = slice(c * 512, (c + 1) * 512)
                k2 = c % 2
                L1, L2, O1, O2, SQ, RS, fb = dL1[k2], dL2[k2], dO1[k2], dO2[k2], dSQ[k2], dRS[k2], dfb[k2]
                S.op("dve", lambda e: e.tensor_copy(out=L1[:], in_=ps[:, 5, :]), reads=[PB[5]], writes=[fb])
                S.op("dve", lambda e: e.tensor_copy(out=O1[:], in_=ps[:, 3, :]), reads=[PB[3]], writes=[fb])
                S.op("dve", lambda e: e.tensor_copy(out=L2[:], in_=ps[:, 6, :]), reads=[PB[6]], writes=[fb])
                S.op("dve", lambda e: e.tensor_copy(out=O2[:], in_=ps[:, 4, :]), reads=[PB[4]], writes=[fb])

                def stage_b():
                    S.op("act", lambda e: e.activation(out=L1[:], in_=L1[:], func=AF.Ln), reads=[fb], writes=[fb])
                    S.op("act", lambda e: e.activation(out=L1[:], in_=L1[:], func=AF.Exp, scale=-1.0),
                         reads=[fb], writes=[fb])
                    S.op("act", lambda e: e.activation(out=L2[:], in_=L2[:], func=AF.Ln), reads=[fb], writes=[fb])
                    S.op("act", lambda e: e.activation(out=L2[:], in_=L2[:], func=AF.Exp, scale=-1.0),
                         reads=[fb], writes=[fb])
                    S.op("dve", lambda e: e.tensor_tensor(out=O1[:], in0=O1[:], in1=L1[:], op=ALU.mult),
                         reads=[fb], writes=[fb])
                    S.op("dve", lambda e: e.tensor_tensor(out=O2[:], in0=O2[:], in1=L2[:], op=ALU.mult),
                         reads=[fb], writes=[fb])
                    S.op("dve", lambda e: e.scalar_tensor_tensor(out=O1[:], in0=O2[:], scalar=neglam[:, 0:1],
                                                                 in1=O1[:], op0=ALU.mult, op1=ALU.add),
                         reads=[fb, cb], writes=[fb])
                    S.op("dve", lambda e: e.tensor_tensor(out=SQ[:], in0=O1[:], in1=O1[:], op=ALU.mult),
                         reads=[fb], writes=[fb])
                    defer(4, stage_c)

                def stage_c():
                    bank = pbank()
                    S.group("pe", [lambda e: e.matmul(ps[:, bank, :], lhsT=ones_bf[:], rhs=SQ[:], start=True,
                                                      stop=True)], reads=[fb, cb], writes=[PB[bank]])
                    S.op("act", lambda e: e.activation(out=RS[:], in_=ps[:, bank, :], func=AF.Ln, scale=1.0 / 128,
                                                       bias=eps5[:, 0:1]), reads=[PB[bank], cb], writes=[fb])
                    S.op("act", lambda e: e.activation(out=RS[:], in_=RS[:], func=AF.Exp, scale=-0.5),
                         reads=[fb], writes=[fb])
                    S.op("dve", lambda e: e.scalar_tensor_tensor(out=yT[:, 4 + j, cs], in0=O1[:], scalar=gsub[:, 0:1],
                                                                 in1=RS[:], op0=ALU.mult, op1=ALU.mult),
                         reads=[fb, cb], writes=[yTb])
                defer(3, stage_b)
            attention(items, fin, filler, 82.0 / len(items))

        for _ in prep_units(0):
            pass
        for g in range(8):
            filler = prep_units(g + 1) if g + 1 < 8 else None
            if g < 4:
                pbk["set"] = (7, 5, 6)
                state["la"], state["sb"] = 2, (0, 1, 2)
                run_moba(g, filler)
            else:
                pbk["set"] = (2, 7)
                state["la"], state["sb"] = 1, (0, 1)
                run_diff(g, filler)
            if filler is not None:
                for _ in filler:
                    pass
            run_pending(flush=True)
        if debug and seq == 0:
            S.dma("sp", dbg["yT"], yT[:], reads=[yTb])

        S.barrier()
        ar.reset(a_mark)
        mT = ar.alloc("mT", [128, 8, T], BF16)
        mTb = Buf("mT")
        woT = ar.alloc("woT", [128, 8, D], BF16)
        woTb = Buf("woT")
        WM = [ar.alloc("wm", [128, 24, 128], BF16) for _ in range(2)]
        WMb = [Buf("wm") for _ in range(2)]
        gpost = ar.alloc("gpost", [128, D], F32)
        gpostb = Buf("gpost")
        SA = [ar.alloc("sa", [128, 512], F32) for _ in range(2)]
        SB_ = [ar.alloc("sb", [128, 512], F32) for _ in range(2)]
        TA = [ar.alloc("ta", [128, 512], F32) for _ in range(2)]
        TB = [ar.alloc("tb", [128, 512], F32) for _ in range(2)]
        MSb = [Buf("ms") for _ in range(2)]

        def load_merge_weights(j):
            sl = j % 2
            js = slice(128 * j, 128 * j + 128)
            S.dma("pool", WM[sl][:, 0:4, :], w_a_r[:, :, js], writes=[WMb[sl]])
            S.dma("pool", WM[sl][:, 4:8, :], w_b_r[:, :, js], writes=[WMb[sl]])
            S.dma("pool", WM[sl][:, 8:16, :], w_in_r[:, :, 3072 + 128 * j:3072 + 128 * j + 128], writes=[WMb[sl]])
            S.dma("pool", WM[sl][:, 16:24, :], w_in_r[:, :, 4096 + 128 * j:4096 + 128 * j + 128], writes=[WMb[sl]])

        S.dma("sp", gpost[:], g_post.partition_broadcast(128), writes=[gpostb])
        load_merge_weights(0)
        load_merge_weights(1)
        for hf in range(2):
            S.dma("pool", woT[:, 4 * hf:4 * hf + 4, :], w_out_r[:, 4 * hf:4 * hf + 4, :], writes=[woTb])
        it_ = 0
        for j in range(8):
            sl = j % 2
            wm, wmb = WM[sl], WMb[sl]
            for c in range(4):
                cs = slice(c * 512, (c + 1) * 512)
                b0 = 4 * (it_ % 2)
                k2 = it_ % 2
                it_ += 1
                S.group("pe", [(lambda e, kc=kc: e.matmul(ps[:, b0, :], lhsT=wm[:, kc, :], rhs=yT[:, kc, cs],
                                                           start=(kc == 0), stop=(kc == 3))) for kc in range(4)],
                        reads=[wmb, yTb], writes=[PB[b0]])
                S.group("pe", [(lambda e, kc=kc: e.matmul(ps[:, b0 + 1, :], lhsT=wm[:, 4 + kc, :],
                                                           rhs=yT[:, 4 + kc, cs],
                                                           start=(kc == 0), stop=(kc == 3))) for kc in range(4)],
                        reads=[wmb, yTb], writes=[PB[b0 + 1]])
                S.group("pe", [(lambda e, kc=kc: e.matmul(ps[:, b0 + 2, :], lhsT=wm[:, 8 + kc, :], rhs=hT[:, kc, cs],
                                                           start=(kc == 0), stop=(kc == 7))) for kc in range(8)],
                        reads=[wmb, hTb], writes=[PB[b0 + 2]])
                S.group("pe", [(lambda e, kc=kc: e.matmul(ps[:, b0 + 3, :], lhsT=wm[:, 16 + kc, :], rhs=hT[:, kc, cs],
                                                           start=(kc == 0), stop=(kc == 7))) for kc in range(8)],
                        reads=[wmb, hTb], writes=[PB[b0 + 3]])
                S.op("act", lambda e: e.activation(out=SA[k2][:], in_=ps[:, b0 + 2, :], func=AF.Sigmoid),
                     reads=[PB[b0 + 2]], writes=[MSb[k2]])
                S.op("act", lambda e: e.activation(out=SB_[k2][:], in_=ps[:, b0 + 3, :], func=AF.Sigmoid),
                     reads=[PB[b0 + 3]], writes=[MSb[k2]])
                S.op("dve", lambda e: e.tensor_tensor(out=TA[k2][:], in0=ps[:, b0, :], in1=SA[k2][:], op=ALU.mult),
                     reads=[PB[b0], MSb[k2]], writes=[MSb[k2]])
                S.op("dve", lambda e: e.tensor_tensor(out=TB[k2][:], in0=ps[:, b0 + 1, :], in1=SB_[k2][:],
                                                      op=ALU.mult),
                     reads=[PB[b0 + 1], MSb[k2]], writes=[MSb[k2]])
                S.op("dve", lambda e: e.tensor_tensor(out=mT[:, j, cs], in0=TA[k2][:], in1=TB[k2][:], op=ALU.add),
                     reads=[MSb[k2]], writes=[mTb])
            if j + 2 < 8:
                load_merge_weights(j + 2)
        if debug and seq == 0:
            S.dma("sp", dbg["mT"], mT[:], reads=[mTb])
        load_gu(0)
        load_gu(1)
        for tt in range(16):
            ts_ = slice(tt * 128, (tt + 1) * 128)
            banks = (2 * (tt % 2), 2 * (tt % 2) + 1)
            xin, xinb = XIN[tt % 2], XINb[tt % 2]
            S.dma("sp", xin[:], x[seq, ts_, :], writes=[xinb])
            for hf in range(2):
                S.group("pe", [(lambda e, kc=kc: e.matmul(ps[:, banks[hf], :], lhsT=mT[:, kc, ts_],
                                                           rhs=woT[:, kc, hf * 512:(hf + 1) * 512],
                                                           start=(kc == 0), stop=(kc == 7))) for kc in range(8)],
                        reads=[mTb, woTb], writes=[PB[banks[hf]]])
            post_A(banks, tt)
            post_B(banks, gpost, gpostb, xin[:], [xinb], tt, x1s[seq, ts_, :])

        S.barrier()
        ar.reset(base_mark)
        X1 = ar.alloc("X1", [128, 8, D], F32)
        X1b = [Buf("x1_%d" % i) for i in range(8)]
        h2T = ar.alloc("h2T", [128, 8, 1024], BF16)
        h2Tb = Buf("h2T")
        actT = ar.alloc("actT", [128, NF, 1024], BF16)
        actTb = Buf("actT")
        Wd = ar.alloc("Wd", [128, NF, D], BF16)
        Wdb = Buf("Wd")
        gfpre = ar.alloc("gfpre", [128, D], F32)
        gfpost = ar.alloc("gfpost", [128, D], F32)
        gfb = Buf("gf")
        SG = [ar.alloc("sg", [128, 512], F32) for _ in range(2)]
        SGb = [Buf("sg") for _ in range(2)]

        S.dma("sp", gfpre[:], g_fpre.partition_broadcast(128), writes=[gfb])
        S.dma("sp", gfpost[:], g_fpost.partition_broadcast(128), writes=[gfb])

        nfp = NF // 2
        gu_loaded = [0]
        for half in range(2):
            if half == 0:
                for f2 in range(nfp):
                    S.dma("pool", Wd[:, 2 * f2:2 * f2 + 2, :], w_down_r[:, 2 * f2:2 * f2 + 2, :], writes=[Wdb])
            def p4_src(t8, half=half):
                tt = half * 8 + t8
                S.dma("sp", X1[:, t8, :], x1s[seq, tt * 128:(tt + 1) * 128, :], writes=[X1b[t8]])
                return X1[:, t8, :], [X1b[t8]]
            norm_transpose_pipeline(8, p4_src, gfpre, gfb, h2T, h2Tb, (6, 7))
            it_ = 0
            for fp in range(nfp):
                sl = fp % 2
                wg, wgb = WGU[sl], WGUb[sl]
                if half == 1 and fp == 0:
                    load_gu(0)
                    load_gu(1)
                for fi in range(2):
                    f = 2 * fp + fi
                    for c2 in range(2):
                        cs = slice(c2 * 512, (c2 + 1) * 512)
                        b0 = 2 * (it_ % 3)
                        k2 = it_ % 2
                        it_ += 1
                        S.group("pe", [(lambda e, kc=kc: e.matmul(ps[:, b0, :], lhsT=wg[:, kc, fi * 128:(fi + 1) * 128],
                                                                   rhs=h2T[:, kc, cs], start=(kc == 0), stop=(kc == 7)))
                                       for kc in range(8)], reads=[wgb, h2Tb], writes=[PB[b0]])
                        S.group("pe", [(lambda e, kc=kc: e.matmul(ps[:, b0 + 1, :],
                                                                   lhsT=wg[:, kc, 256 + fi * 128:256 + (fi + 1) * 128],
                                                                   rhs=h2T[:, kc, cs], start=(kc == 0), stop=(kc == 7)))
                                       for kc in range(8)], reads=[wgb, h2Tb], writes=[PB[b0 + 1]])
                        S.op("act", lambda e: e.activation(out=SG[k2][:], in_=ps[:, b0, :], func=AF.Silu),
                             reads=[PB[b0]], writes=[SGb[k2]])
                        S.op("dve", lambda e: e.tensor_tensor(out=actT[:, f, cs], in0=ps[:, b0 + 1, :], in1=SG[k2][:],
                                                              op=ALU.mult),
                             reads=[PB[b0 + 1], SGb[k2]], writes=[actTb])
                if fp + 2 < nfp:
                    load_gu(fp + 2)
            for t8 in range(8):
                tt = half * 8 + t8
                banks = (2 * (t8 % 2), 2 * (t8 % 2) + 1)
                for hf in range(2):
                    S.group("pe", [(lambda e, f=f: e.matmul(ps[:, banks[hf], :], lhsT=actT[:, f, t8 * 128:(t8 + 1) * 128],
                                                             rhs=Wd[:, f, hf * 512:(hf + 1) * 512],
                                                             start=(f == 0), stop=(f == NF - 1))) for f in range(NF)],
                            reads=[actTb, Wdb], writes=[PB[banks[hf]]])
                post_A(banks, t8)
                post_B(banks, gfpost, gfb, X1[:, t8, :], [X1b[t8]], t8, y[seq, tt * 128:(tt + 1) * 128, :])
        S.barrier()
    return nc


_NC_CACHE = {}


def kernel(x, norm_mix_pre_g, w_in, w_branch_a, w_branch_b, lam_q1, lam_k1, lam_q2, lam_k2,
           diff_subln_g, w_out, norm_mix_post_g, norm_ffn_pre_g, w_gate, w_up, w_down, norm_ffn_post_g):
    n = 8
    f32 = lambda a: np.ascontiguousarray(np.asarray(a, dtype=np.float32))
    x = f32(x)
    consts = _host_consts()
    shared = {
        "w_in": f32(w_in)[0], "w_branch_a": f32(w_branch_a)[0], "w_branch_b": f32(w_branch_b)[0],
        "w_out": f32(w_out)[0], "w_gate": f32(w_gate)[0], "w_up": f32(w_up)[0], "w_down": f32(w_down)[0],
        "norm_mix_pre_g": f32(norm_mix_pre_g), "norm_mix_post_g": f32(norm_mix_post_g),
        "norm_ffn_pre_g": f32(norm_ffn_pre_g), "norm_ffn_post_g": f32(norm_ffn_post_g),
        "lamv": np.concatenate([f32(lam_q1)[0], f32(lam_k1)[0], f32(lam_q2)[0], f32(lam_k2)[0]])[None, :],
        "diff_subln_g": f32(diff_subln_g)[0][:, None],
    }
    shared.update(consts)
    nc = build(2)
    in_maps = []
    for c in range(n):
        m = dict(shared)
        m["x"] = np.ascontiguousarray(x[2 * c:2 * c + 2])
        in_maps.append(m)
    res = run_bass_kernel_spmd(nc, in_maps, core_ids=list(range(n)))
    return np.concatenate([r["y"] for r in res.results], axis=0).astype(np.float32)
```
